# Optimizing a Trainium2 kernel written in Bass

```python
import math
import jax
import jax.numpy as jnp
from jax import lax
import numpy as np

D_MODEL = 2048
BATCH = 8
SEQ = 2048
DEPTH = 4

N_META = 16
N_A_LAYERS = DEPTH // 2
N_B_LAYERS = DEPTH - N_A_LAYERS
N_DENSE = (DEPTH + 1) // 2
N_MOE = DEPTH // 2
DN_ALPHA = (2 * DEPTH) ** 0.25
DN_BETA = (8 * DEPTH) ** -0.25
LN_EPS = 1e-5

GLA_HEADS = 4
GLA_DK = D_MODEL // 2
GLA_DV = D_MODEL
GLA_DK_HEAD = GLA_DK // GLA_HEADS
GLA_DV_HEAD = GLA_DV // GLA_HEADS
GLA_GATE_RANK = 16
GLA_GATE_TAU = 16.0
GLA_CHUNK = 64
GLA_IN = 2 * GLA_DK + 2 * GLA_DV + GLA_GATE_RANK

SWA_HEAD_DIM = 64
SWA_Q_HEADS = D_MODEL // SWA_HEAD_DIM
SWA_GROUP = 8
SWA_KV_HEADS = SWA_Q_HEADS // SWA_GROUP
SWA_WINDOW = 128
SWA_BLOCK = 128
SWA_KEYS = N_META + 2 * SWA_BLOCK

REL_BUCKETS = 32
REL_MAX_DIST = 128

FFN_DIM = 7 * D_MODEL // 2
N_EXPERTS = 8
TOP_K = 2
MOE_BLOCK = 512

NEG_INF = -1e9

kernel_name = 'yoco_gla_swa_sink_moe_deepnorm'


def layer_norm(x, g, b):
    xf = x.astype(jnp.float32)
    mu = jnp.mean(xf, axis=-1, keepdims=True)
    var = jnp.mean(jnp.square(xf - mu), axis=-1, keepdims=True)
    y = (xf - mu) * lax.rsqrt(var + LN_EPS)
    return (y * g.astype(jnp.float32) + b.astype(jnp.float32)).astype(x.dtype)


def swiglu(x, w_gu, w_down):
    a, u = jnp.split(x @ w_gu, 2, axis=-1)
    return (jax.nn.silu(a) * u) @ w_down


def gla_mixer(x, w_in, w_gate2, b_gate, norm_gain, w_out):
    bsz, L, _ = x.shape
    H, dk, dv, C = GLA_HEADS, GLA_DK_HEAD, GLA_DV_HEAD, GLA_CHUNK
    offs = [GLA_DK, 2 * GLA_DK, 2 * GLA_DK + GLA_DV, 2 * GLA_DK + 2 * GLA_DV]
    q, k, v, r, g_low = jnp.split(x @ w_in, offs, axis=-1)
    log_a = jax.nn.log_sigmoid((g_low @ w_gate2 + b_gate).astype(jnp.float32)) / GLA_GATE_TAU
    pad = C - N_META

    def to_chunks(t, d):
        t = jnp.pad(t.astype(jnp.float32), ((0, 0), (pad, 0), (0, 0)))
        n = t.shape[1] // C
        return t.reshape(bsz, n, C, H, d).transpose(0, 3, 1, 2, 4)

    qc = to_chunks(q, dk) * (dk ** -0.5)
    kc = to_chunks(k, dk)
    vc = to_chunks(v, dv)
    bcum = jnp.cumsum(to_chunks(log_a, dk), axis=3)
    q_dec = qc * jnp.exp(bcum)
    k_inv = kc * jnp.exp(-bcum)
    causal = jnp.tril(jnp.ones((C, C), dtype=bool))
    att = jnp.where(causal, jnp.einsum('bhncd,bhnsd->bhncs', q_dec, k_inv), 0.0)
    o_intra = jnp.einsum('bhncs,bhnsv->bhncv', att, vc)
    b_last = bcum[:, :, :, -1:, :]
    k_state = kc * jnp.exp(b_last - bcum)
    decay = jnp.exp(b_last[:, :, :, 0, :])

    def step(S, inp):
        qd, ks, vv, dec = inp
        o = jnp.einsum('bhcd,bhdv->bhcv', qd, S)
        S = dec[..., None] * S + jnp.einsum('bhcd,bhcv->bhdv', ks, vv)
        return S, o

    S0 = jnp.zeros((bsz, H, dk, dv), jnp.float32)
    xs = (jnp.moveaxis(q_dec, 2, 0), jnp.moveaxis(k_state, 2, 0),
          jnp.moveaxis(vc, 2, 0), jnp.moveaxis(decay, 2, 0))
    _, o_inter = lax.scan(step, S0, xs)
    o = o_intra + jnp.moveaxis(o_inter, 0, 2)
    o = o.transpose(0, 2, 3, 1, 4).reshape(bsz, -1, H, dv)[:, pad:]
    mu = jnp.mean(o, axis=-1, keepdims=True)
    var = jnp.mean(jnp.square(o - mu), axis=-1, keepdims=True)
    o = ((o - mu) * lax.rsqrt(var + LN_EPS)).reshape(bsz, L, GLA_DV) * norm_gain.astype(jnp.float32)
    return (o.astype(x.dtype) * jax.nn.silu(r)) @ w_out


def t5_bucket(dist):
    exact = REL_BUCKETS // 2
    d = jnp.maximum(dist, 0)
    df = jnp.maximum(d, 1).astype(jnp.float32)
    large = exact + (jnp.log(df / exact) / math.log(REL_MAX_DIST / exact)
                     * (REL_BUCKETS - exact)).astype(jnp.int32)
    large = jnp.minimum(large, REL_BUCKETS - 1)
    return jnp.where(d < exact, d, large)


def relative_bias(table, n_blocks):
    tab = table.astype(jnp.float32)
    qpos = N_META + jnp.arange(n_blocks * SWA_BLOCK, dtype=jnp.int32).reshape(n_blocks, SWA_BLOCK)
    blk = jnp.arange(n_blocks, dtype=jnp.int32)[:, None]
    j = jnp.arange(SWA_BLOCK, dtype=jnp.int32)[None, :]
    kpos = jnp.concatenate([
        jnp.broadcast_to(jnp.arange(N_META, dtype=jnp.int32)[None, :], (n_blocks, N_META)),
        N_META + (blk - 1) * SWA_BLOCK + j,
        N_META + blk * SWA_BLOCK + j], axis=1)
    is_meta = jnp.arange(SWA_KEYS) < N_META
    dist = qpos[:, :, None] - kpos[:, None, :]
    valid = is_meta[None, None, :] | ((dist >= 0) & (dist < SWA_WINDOW) & (kpos[:, None, :] >= N_META))
    band = jnp.where(valid[..., None], tab[t5_bucket(dist)], NEG_INF).transpose(0, 3, 1, 2)
    mdist = jnp.arange(N_META)[:, None] - jnp.arange(N_META)[None, :]
    meta = jnp.where((mdist >= 0)[..., None], tab[t5_bucket(mdist)], NEG_INF).transpose(2, 0, 1)
    return band, meta


def shared_kv(h, w_kv, n_blocks):
    bsz, L, _ = h.shape
    kv = (h @ w_kv).reshape(bsz, L, 2, SWA_KV_HEADS, SWA_HEAD_DIM)
    k, v = kv[:, :, 0], kv[:, :, 1]

    def band(t):
        meta = t[:, :N_META]
        real = t[:, N_META:].reshape(bsz, n_blocks, SWA_BLOCK, SWA_KV_HEADS, SWA_HEAD_DIM)
        prev = jnp.concatenate([jnp.zeros_like(real[:, :1]), real[:, :-1]], axis=1)
        meta_b = jnp.broadcast_to(meta[:, None], (bsz, n_blocks, N_META, SWA_KV_HEADS, SWA_HEAD_DIM))
        return jnp.concatenate([meta_b, prev, real], axis=2)

    return k[:, :N_META], v[:, :N_META], band(k), band(v)


def swa_mixer(x, k_meta, v_meta, k_band, v_band, band_bias, meta_bias, w_q, sinks, w_out):
    bsz, L, _ = x.shape
    n_blocks = k_band.shape[1]
    KV, G, hd = SWA_KV_HEADS, SWA_GROUP, SWA_HEAD_DIM
    q = (x @ w_q).reshape(bsz, L, KV, G, hd) * (hd ** -0.5)
    sink = sinks.astype(jnp.float32).reshape(KV, G)

    def attend(qb, kb, vb, bias):
        nq, ns = qb.shape[1], kb.shape[1]
        s = jnp.einsum('bqkgd,bskd->bkgqs', qb, kb).astype(jnp.float32) + bias.reshape(KV, G, nq, ns)
        sink_col = jnp.broadcast_to(sink[None, :, :, None, None], s.shape[:-1] + (1,))
        p = jax.nn.softmax(jnp.concatenate([s, sink_col], axis=-1), axis=-1)[..., :-1]
        return jnp.einsum('bkgqs,bskd->bqkgd', p.astype(vb.dtype), vb)

    o_meta = attend(q[:, :N_META], k_meta, v_meta, meta_bias)
    q_real = q[:, N_META:].reshape(bsz, n_blocks, SWA_BLOCK, KV, G, hd)
    o_real = lax.map(lambda a: attend(a[0], a[1], a[2], a[3]),
                     (jnp.moveaxis(q_real, 1, 0), jnp.moveaxis(k_band, 1, 0),
                      jnp.moveaxis(v_band, 1, 0), band_bias))
    o_real = jnp.moveaxis(o_real, 0, 1).reshape(bsz, n_blocks * SWA_BLOCK, KV, G, hd)
    o = jnp.concatenate([o_meta, o_real], axis=1).reshape(bsz, L, SWA_Q_HEADS * hd)
    return o @ w_out


def moe_swiglu(x2d, w_router, w_gu, w_down):
    T, d = x2d.shape
    logits = (x2d @ w_router).astype(jnp.float32)
    top_val, top_idx = lax.top_k(logits, TOP_K)
    gates = jax.nn.softmax(top_val, axis=-1).astype(x2d.dtype)
    flat_e = top_idx.reshape(-1)
    flat_tok = jnp.repeat(jnp.arange(T, dtype=jnp.int32), TOP_K)
    flat_g = gates.reshape(-1)
    order = jnp.argsort(flat_e)
    s_e, s_tok, s_g = flat_e[order], flat_tok[order], flat_g[order]
    counts = jnp.bincount(flat_e, length=N_EXPERTS)
    padded = ((counts + MOE_BLOCK - 1) // MOE_BLOCK) * MOE_BLOCK
    start_sorted = jnp.cumsum(counts) - counts
    padded_end = jnp.cumsum(padded)
    start_padded = padded_end - padded
    dest = start_padded[s_e] + (jnp.arange(T * TOP_K, dtype=jnp.int32) - start_sorted[s_e])
    n_blk = -(-(T * TOP_K) // MOE_BLOCK) + N_EXPERTS
    P = n_blk * MOE_BLOCK
    buf_tok = jnp.zeros((P,), jnp.int32).at[dest].set(s_tok)
    buf_gate = jnp.zeros((P,), x2d.dtype).at[dest].set(s_g)
    blk_e = jnp.minimum(jnp.searchsorted(padded_end, jnp.arange(n_blk) * MOE_BLOCK, side='right'),
                        N_EXPERTS - 1).astype(jnp.int32)
    x_blk = x2d[buf_tok].reshape(n_blk, MOE_BLOCK, d)

    def expert_block(a):
        xb, e = a
        return swiglu(xb, w_gu[e], w_down[e])

    y = lax.map(expert_block, (x_blk, blk_e)).reshape(P, d) * buf_gate[:, None]
    return jnp.zeros((T, d), x2d.dtype).at[buf_tok].add(y)


def setup_inputs(seed: int = 0) -> dict:
    key = jax.random.key(seed)
    ks = jax.random.split(key, 20)
    f32 = jnp.float32
    D = D_MODEL

    def nrm(k, shape, scale):
        return jax.random.normal(k, shape, f32) * scale

    return {
        'x': nrm(ks[0], (BATCH, SEQ, D), 1.0),
        'meta_tokens': nrm(ks[1], (N_META, D), 1.0),
        'rel_bias_table': nrm(ks[2], (REL_BUCKETS, SWA_Q_HEADS), 0.5),
        'ln_gain': 1.0 + nrm(ks[3], (DEPTH, 2, D), 0.02),
        'ln_bias': nrm(ks[4], (DEPTH, 2, D), 0.02),
        'gla_w_in': nrm(ks[5], (N_A_LAYERS, D, GLA_IN), D ** -0.5),
        'gla_w_gate2': nrm(ks[6], (N_A_LAYERS, GLA_GATE_RANK, GLA_DK), GLA_GATE_RANK ** -0.5),
        'gla_b_gate': nrm(ks[7], (N_A_LAYERS, GLA_DK), 0.1),
        'gla_norm_gain': 1.0 + nrm(ks[8], (N_A_LAYERS, GLA_DV), 0.02),
        'gla_w_out': nrm(ks[9], (N_A_LAYERS, GLA_DV, D), DN_BETA * GLA_DV ** -0.5),
        'kv_w_shared': nrm(ks[10], (D, 2 * SWA_KV_HEADS * SWA_HEAD_DIM), D ** -0.5),
        'swa_w_q': nrm(ks[11], (N_B_LAYERS, D, SWA_Q_HEADS * SWA_HEAD_DIM), D ** -0.5),
        'swa_sinks': nrm(ks[12], (N_B_LAYERS, SWA_Q_HEADS), 1.0),
        'swa_w_out': nrm(ks[13], (N_B_LAYERS, SWA_Q_HEADS * SWA_HEAD_DIM, D), DN_BETA * D ** -0.5),
        'ffn_w_gate_up': nrm(ks[14], (N_DENSE, D, 2 * FFN_DIM), D ** -0.5),
        'ffn_w_down': nrm(ks[15], (N_DENSE, FFN_DIM, D), DN_BETA * FFN_DIM ** -0.5),
        'moe_w_router': nrm(ks[16], (N_MOE, D, N_EXPERTS), D ** -0.5),
        'moe_w_gate_up': nrm(ks[17], (N_MOE, N_EXPERTS, D, 2 * FFN_DIM), D ** -0.5),
        'moe_w_down': nrm(ks[18], (N_MOE, N_EXPERTS, FFN_DIM, D), DN_BETA * FFN_DIM ** -0.5),
    }


def reference(x, meta_tokens, rel_bias_table, ln_gain, ln_bias, gla_w_in, gla_w_gate2, gla_b_gate,
              gla_norm_gain, gla_w_out, kv_w_shared, swa_w_q, swa_sinks, swa_w_out,
              ffn_w_gate_up, ffn_w_down, moe_w_router, moe_w_gate_up, moe_w_down):
    bsz, seq, d = x.shape
    n_blocks = seq // SWA_BLOCK
    h = jnp.concatenate([jnp.broadcast_to(meta_tokens.astype(x.dtype)[None], (bsz, N_META, d)), x], axis=1)
    band_bias, meta_bias = relative_bias(rel_bias_table, n_blocks)
    shared = None
    for li in range(DEPTH):
        if li < N_A_LAYERS:
            mix = gla_mixer(h, gla_w_in[li], gla_w_gate2[li], gla_b_gate[li], gla_norm_gain[li], gla_w_out[li])
        else:
            jb = li - N_A_LAYERS
            k_meta, v_meta, k_band, v_band = shared
            mix = swa_mixer(h, k_meta, v_meta, k_band, v_band, band_bias, meta_bias,
                            swa_w_q[jb], swa_sinks[jb], swa_w_out[jb])
        h = layer_norm(DN_ALPHA * h + mix, ln_gain[li, 0], ln_bias[li, 0])
        if li % 2 == 0:
            f = swiglu(h, ffn_w_gate_up[li // 2], ffn_w_down[li // 2])
        else:
            f = moe_swiglu(h.reshape(-1, d), moe_w_router[li // 2], moe_w_gate_up[li // 2],
                           moe_w_down[li // 2]).reshape(h.shape)
        h = layer_norm(DN_ALPHA * h + f, ln_gain[li, 1], ln_bias[li, 1])
        if li == N_A_LAYERS - 1:
            shared = shared_kv(h, kv_w_shared, n_blocks)
    return h[:, N_META:]
```

```python
import contextlib
import numpy as np
import concourse.bass as bass
import concourse.mybir as mybir
from concourse.bass_utils import run_bass_kernel_spmd

F32 = mybir.dt.float32
BF16 = mybir.dt.bfloat16
AF = mybir.ActivationFunctionType
ALU = mybir.AluOpType

D = 2048
NT = 17
TP = NT * 128
NMETA = 16
ALPHA = float(8 ** 0.25)
EPS = 1e-5
FF = 7168
NE = 8
CAPT = 5
CAP = CAPT * 128
KC = 16


class Buf:
    __slots__ = ("name", "w", "r")

    def __init__(self, name):
        self.name = name
        self.w = None
        self.r = []


class Op:
    __slots__ = ("eng", "fn", "deps", "needed", "sem", "val", "inc", "dma")


class Sched:
    ENG = ("pe", "act", "dve", "pool", "sp")

    def __init__(self, nc, es, n_dma_sems=20):
        self.nc = nc
        self.streams = {e: [] for e in self.ENG}
        self.csem = {e: es.enter_context(nc.semaphore("c_" + e)) for e in ("pe", "act", "dve", "pool")}
        self.dsem = {"sp": [es.enter_context(nc.semaphore("d_sp%d" % i)) for i in range(n_dma_sems)],
                     "act": [es.enter_context(nc.semaphore("d_act%d" % i)) for i in range(8)]}
        self.dctr = {"sp": 0, "act": 0}
        self.dlast = {}
        self.last = {e: None for e in self.ENG}

    def op(self, eng, fn, reads=(), writes=(), dma=False):
        o = Op()
        o.eng, o.fn, o.dma, o.needed = eng, fn, dma, False
        o.sem = None
        o.val = 0
        o.inc = 0
        deps = []
        for b in reads:
            if b.w is not None:
                deps.append(b.w)
        for b in writes:
            if b.w is not None:
                deps.append(b.w)
            deps.extend(b.r)
        if dma:
            pool = self.dsem[eng]
            i = self.dctr[eng] % len(pool)
            self.dctr[eng] += 1
            o.sem = pool[i]
            prev = self.dlast.get((eng, i))
            if prev is not None:
                deps.append(prev)
            self.dlast[(eng, i)] = o
            o.needed = True
        seen = set()
        od = []
        for d in deps:
            if id(d) in seen or d is o:
                continue
            seen.add(id(d))
            if d.eng == "pe" and eng == "pe" and not d.dma and not dma:
                continue
            od.append(d)
        o.deps = od
        for d in od:
            d.needed = True
        for b in reads:
            if not dma:
                b.r = [x for x in b.r if x.dma or x.eng != eng]
            b.r.append(o)
        for b in writes:
            b.w = o
            b.r = []
        self.streams[eng].append(o)
        self.last[eng] = o
        return o

    def barrier(self):
        tails = [o for o in self.last.values() if o is not None]
        tails += [o for o in self.dlast.values()]
        bb = Buf("barrier")
        for e in self.ENG:
            o = Op()
            o.eng, o.fn, o.dma, o.needed = e, None, False, False
            o.sem, o.val, o.inc = None, 0, 0
            o.deps = [t for t in tails if not (t.eng == e and not t.dma and e == "pe")]
            for d in o.deps:
                d.needed = True
            self.streams[e].append(o)

    def assign(self):
        for e, ops in self.streams.items():
            c = 0
            dc = {}
            for o in ops:
                if o.fn is None:
                    continue
                if o.dma:
                    k = id(o.sem)
                    dc[k] = dc.get(k, 0) + 16
                    o.val, o.inc = dc[k], 16
                elif o.needed:
                    c += 1
                    o.val, o.inc, o.sem = c, 1, self.csem[e]

    def emit(self, name, e):
        waited = {}
        for o in self.streams[name]:
            for d in o.deps:
                k = id(d.sem)
                if waited.get(k, 0) < d.val:
                    e.wait_ge(d.sem, d.val)
                    waited[k] = d.val
            if o.fn is None:
                continue
            ins = o.fn(e)
            if o.needed:
                ins.then_inc(o.sem, o.inc)


class Builder:
    def __init__(self, cfg):
        self.cfg = cfg
        self.nc = bass.Bass("TRN2", target_bir_lowering=False)
        self.es = contextlib.ExitStack()
        self.bufs = {}

    def buf(self, name):
        b = self.bufs.get(name)
        if b is None:
            b = self.bufs[name] = Buf(name)
        return b

    def din(self, name, shape, dt=F32):
        return self.nc.dram_tensor(name, list(shape), dt, kind="ExternalInput").ap()

    def dout(self, name, shape, dt=F32):
        return self.nc.dram_tensor(name, list(shape), dt, kind="ExternalOutput").ap()

    def dscr(self, name, shape, dt=F32):
        kind = "ExternalOutput" if name in self.cfg.get("expose", ()) else "Internal"
        return self.nc.dram_tensor(name, list(shape), dt, kind=kind).ap()

    def sb_reset(self, base=None):
        self.sb_off = self.sb_persist if base is None else base

    def sb(self, words, dt=F32, shape=None):
        words = int(words)
        a = self.sb_off
        self.sb_off += words
        assert self.sb_off <= self.SBW, ("SBUF overflow", self.sb_off, self.SBW)
        v = self.SB[:, a:a + words]
        if dt != F32:
            v = v.bitcast(dt)
        if shape is not None:
            names = " ".join("d%d" % i for i in range(len(shape)))
            kw = {"d%d" % i: int(s) for i, s in enumerate(shape)}
            v = v.rearrange("p (%s) -> p %s" % (names, names), **kw)
        return v

    def build(self):
        nc, es, cfg = self.nc, self.es, self.cfg
        self.SBW = cfg.get("sbw", 53000)
        self.SB = es.enter_context(nc.sbuf_tensor("SB", [128, self.SBW], F32))
        self.PS = es.enter_context(nc.psum_tensor("PS", [128, 8, 512], F32))
        self.S = Sched(nc, es)
        self.pbank = [self.buf("psum%d" % i) for i in range(8)]
        S = self.S

        self.x = self.din("x", [2048, D])
        self.meta = self.din("meta_tokens", [NMETA, D])
        self.ln_gain = self.din("ln_gain", [4, 2, D])
        self.ln_bias = self.din("ln_bias", [4, 2, D])
        self.out = self.dout("out", [2048, D])
        self.hres = self.dscr("hres", [TP, D])
        self.hT = self.dscr("hT", [NT, 128, KC * 128], BF16)
        self.fd = self.dscr("fd", [TP, D])
        need = cfg["phases"]
        if any(p[0] == "ffn" for p in need):
            self.w_gu = self.din("ffn_w_gate_up", [2, D, 2 * FF])
            self.w_dn = self.din("ffn_w_down", [2, FF, D])
        if any(p[0] == "gla" for p in need):
            self.g_win = self.din("gla_w_in", [2, D, 6160])
            self.g_wg2 = self.din("gla_w_gate2", [2, 16, 1024])
            self.g_bg = self.din("gla_b_gate", [2, 1024])
            self.g_ng = self.din("gla_norm_gain", [2, D])
            self.g_wout = self.din("gla_w_out", [2, D, D])
        if any(p[0] == "swa" for p in need):
            self.w_q = self.din("swa_w_q", [2, D, D])
            self.w_o = self.din("swa_w_out", [2, D, D])
            self.sinks = self.din("swa_sinks", [2, 32])
        if any(p[0] == "moe" for p in need):
            self.m_rt = self.din("moe_w_router", [2, D, NE])
            self.m_gu = self.din("moe_w_gate_up", [2, NE, D, 2 * FF])
            self.m_dn = self.din("moe_w_down", [2, NE, FF, D])
            self.XS = self.dscr("XS", [NE, CAPT, 128, KC * 128], BF16)
            self.FA = self.dscr("FA", [NE * CAP, D], BF16)
            self.SELT = self.dscr("SELT", [NT, 128, NE * CAPT * 128], BF16)

        self.sb_off = 0
        self.identb = self.sb(64, BF16, [128])
        self.identf = self.sb(128, F32, [128])
        self.gs = self.sb(NE * CAPT, F32, [NE * CAPT])
        self.eps_ap = self.sb(1)
        self.sb_persist = self.sb_off
        cb = self.buf("consts")

        S.op("pool", lambda e: e.memset(self.identf, 0.0), writes=[cb])
        S.op("pool", lambda e: e.affine_select(out=self.identf, in_=self.identf, pattern=[[-1, 128]], base=0,
                                               channel_multiplier=1, compare_op=ALU.not_equal, fill=1.0),
             reads=[cb], writes=[cb])
        S.op("pool", lambda e: e.memset(self.eps_ap, EPS), writes=[self.buf("eps")])
        S.op("pool", lambda e: e.tensor_copy(self.identb, self.identf), reads=[cb], writes=[self.buf("identb")])

        for ph in need:
            S.barrier()
            self.sb_reset()
            getattr(self, "ph_" + ph[0])(*ph[1:])
        S.barrier()

        S.assign()
        with nc.Block() as block:
            @block.tensor
            def _(e):
                S.emit("pe", e)

            @block.scalar
            def _(e):
                S.emit("act", e)

            @block.vector
            def _(e):
                S.emit("dve", e)

            @block.gpsimd
            def _(e):
                S.emit("pool", e)

            @block.sync
            def _(e):
                S.emit("sp", e)
        return nc

    def ph_init(self):
        S = self.S
        hb = [self.buf("hres%d" % i) for i in range(NT)]
        for i in range(16):
            S.op("sp", lambda e, i=i: e.dma_start(out=self.hres[i * 128:(i + 1) * 128, :],
                                                   in_=self.x[i * 128:(i + 1) * 128, :]),
                 writes=[hb[i]], dma=True)
        z = self.sb(D, F32)
        zb = self.buf("ztile")
        S.op("pool", lambda e: e.memset(z, 0.0), writes=[zb])
        S.op("sp", lambda e: e.dma_start(out=z[0:NMETA, :], in_=self.meta[:, :]), reads=[zb], writes=[zb], dma=True)
        S.op("sp", lambda e: e.dma_start(out=self.hres[2048:2176, :], in_=z), reads=[zb], writes=[hb[16]], dma=True)

    def ph_ln(self, li, which, mode):
        S = self.S
        NB = 2
        A = [self.sb(D) for _ in range(NB)]
        Fq = [self.sb(D) for _ in range(NB)]
        Yb = [self.sb(D // 2, BF16) for _ in range(NB)]
        HT = [self.sb(KC * 64, BF16, [KC, 128]) for _ in range(NB)]
        Gb = self.sb(D)
        Bb = self.sb(D)
        st = [self.sb(24, F32, [4, 6]) for _ in range(NB)]
        mv = [self.sb(2) for _ in range(NB)]
        sd = [self.sb(1) for _ in range(NB)]
        rs = [self.sb(1) for _ in range(NB)]
        bA = [self.buf("lnA%d" % j) for j in range(NB)]
        bF = [self.buf("lnF%d" % j) for j in range(NB)]
        bY = [self.buf("lnY%d" % j) for j in range(NB)]
        bH = [self.buf("lnH%d" % j) for j in range(NB)]
        bs = [self.buf("lnS%d" % j) for j in range(NB)]
        bg = self.buf("lnG")
        if mode != "plain":
            S.op("sp", lambda e: e.dma_start(out=Gb, in_=self.ln_gain[li, which:which + 1, :].partition_broadcast(128)),
                 writes=[bg], dma=True)
            S.op("sp", lambda e: e.dma_start(out=Bb, in_=self.ln_bias[li, which:which + 1, :].partition_broadcast(128)),
                 writes=[bg], dma=True)
        hb = [self.buf("hres%d" % i) for i in range(NT)]
        htb = [self.buf("hT%d" % i) for i in range(NT)]
        fb = [self.buf("fd%d" % i) for i in range(NT)]
        for i in range(NT):
            j = i % NB
            a, f, yb, ht = A[j], Fq[j], Yb[j], HT[j]
            rows = slice(i * 128, (i + 1) * 128)
            S.op("sp", lambda e, a=a, rows=rows: e.dma_start(out=a, in_=self.hres[rows, :]),
                 reads=[hb[i]], writes=[bA[j]], dma=True)
            if mode != "plain":
                S.op("sp", lambda e, f=f, rows=rows: e.dma_start(out=f, in_=self.fd[rows, :]),
                     reads=[fb[i]], writes=[bF[j]], dma=True)
                S.op("dve", lambda e, a=a, f=f: e.scalar_tensor_tensor(out=a, in0=a, scalar=ALPHA, in1=f,
                                                                       op0=ALU.mult, op1=ALU.add),
                     reads=[bF[j], bA[j]], writes=[bA[j]])
                for q in range(4):
                    S.op("dve", lambda e, a=a, q=q, s=st[j]: e.bn_stats(out=s[:, q, :], in_=a[:, q * 512:(q + 1) * 512]),
                         reads=[bA[j]], writes=[bs[j]])
                S.op("dve", lambda e, s=st[j], m=mv[j]: e.bn_aggr(out=m, in_=s), reads=[bs[j]], writes=[bs[j]])
                S.op("act", lambda e, m=mv[j], d=sd[j]: e.activation(out=d, in_=m[:, 1:2], func=AF.Sqrt, bias=self.eps_ap, scale=1.0),
                     reads=[bs[j], self.buf("eps")], writes=[bs[j]])
                S.op("dve", lambda e, d=sd[j], r=rs[j]: e.reciprocal(out=r, in_=d), reads=[bs[j]], writes=[bs[j]])
                S.op("dve", lambda e, a=a, m=mv[j], r=rs[j]: e.tensor_scalar(out=a, in0=a, scalar1=m[:, 0:1], scalar2=r,
                                                                              op0=ALU.subtract, op1=ALU.mult),
                     reads=[bs[j], bA[j]], writes=[bA[j]])
                S.op("pool", lambda e, a=a: e.tensor_tensor(out=a, in0=a, in1=Gb, op=ALU.mult),
                     reads=[bA[j], bg], writes=[bA[j]])
                S.op("pool", lambda e, a=a: e.tensor_tensor(out=a, in0=a, in1=Bb, op=ALU.add),
                     reads=[bA[j], bg], writes=[bA[j]])
                if mode == "final":
                    if i < 16:
                        S.op("sp", lambda e, a=a, rows=rows: e.dma_start(out=self.out[rows, :], in_=a),
                             reads=[bA[j]], writes=[self.buf("out%d" % i)], dma=True)
                    continue
                S.op("sp", lambda e, a=a, rows=rows: e.dma_start(out=self.hres[rows, :], in_=a),
                     reads=[bA[j]], writes=[hb[i]], dma=True)
            S.op("act", lambda e, a=a, yb=yb: e.activation(out=yb, in_=a, func=AF.Copy), reads=[bA[j]], writes=[bY[j]])
            for half in range(2):
                bank = self.pbank[(2 * i + half) % 8]
                pv = self.PS[:, (2 * i + half) % 8, :].bitcast(BF16).rearrange("p (a b) -> p a b", a=8)

                def tr(e, yb=yb, pv=pv, half=half):
                    for q in range(8):
                        kc = half * 8 + q
                        ins = e.transpose(out=pv[:, q, :], in_=yb[:, kc * 128:(kc + 1) * 128], identity=self.identb)
                    return ins
                S.op("pe", tr, reads=[bY[j], self.buf("identb")], writes=[bank])
                eng = "dve" if half == 0 else "act"
                if eng == "dve":
                    S.op("dve", lambda e, ht=ht, pv=pv, half=half: e.tensor_copy(ht[:, half * 8:(half + 1) * 8, :], pv),
                         reads=[bank], writes=[bH[j]])
                else:
                    S.op("act", lambda e, ht=ht, pv=pv, half=half: e.activation(out=ht[:, half * 8:(half + 1) * 8, :], in_=pv, func=AF.Copy),
                         reads=[bank], writes=[bH[j]])
            S.op("sp", lambda e, ht=ht, i=i: e.dma_start(out=self.hT[i], in_=ht.rearrange("p a b -> p (a b)")),
                 reads=[bH[j]], writes=[htb[i]], dma=True)

    def ffn_pass(self, XT, xbs, nt, wgu, wdn, sink, R):
        S = self.S
        GT, gtb = R["GT"], R["gtb"]
        groups = [(0, min(3, nt))] + ([(3, nt)] if nt > 3 else [])
        wguv = wgu.rearrange("(kc p) f -> p kc f", p=128)
        wdnv = wdn.rearrange("(hc p) f -> p hc f", p=128)
        ctr = R["ctr"]
        tiles = []
        for hp in range(FF // 256):
            tiles.append((wguv[:, :, hp * 256:(hp + 1) * 256], (KC, 256)))
            tiles.append((wguv[:, :, FF + hp * 256:FF + (hp + 1) * 256], (KC, 256)))
        for fp in range(D // 256):
            for q in range(4):
                tiles.append((wdnv[:, q * 14:(q + 1) * 14, fp * 256:(fp + 1) * 256], (14, 256)))
        issued = [0]
        handles = {}

        def load_cast(j):
            src_ap, shape3 = tiles[j]
            k = ctr[0] % 2
            ctr[0] += 1
            stg = R["stg"][k][:, 0:shape3[0] * shape3[1]].rearrange("p (a b) -> p a b", a=shape3[0])
            sbf = R["stgb"][k]
            k2 = ctr[1] % 3
            ctr[1] += 1
            wb = R["wbf"][k2][:, 0:shape3[0] * shape3[1]].rearrange("p (a b) -> p a b", a=shape3[0])
            wbb = R["wbfb"][k2]
            S.op("sp", lambda e: e.dma_start(out=stg, in_=src_ap), writes=[sbf], dma=True)
            S.op("pool", lambda e: e.tensor_copy(wb, stg), reads=[sbf], writes=[wbb])
            handles[j] = (wb, wbb)

        def get(j):
            while issued[0] <= min(j + 2, len(tiles) - 1):
                load_cast(issued[0])
                issued[0] += 1
            return handles.pop(j)

        def mm(e, w, p, t0, t1, h2):
            for kc in range(KC):
                ins = e.matmul(p, w[:, kc, h2 * 128:(h2 + 1) * 128], XT[:, t0:t1, kc, :],
                               start=(kc == 0), stop=(kc == KC - 1))
            return ins
        for hp in range(FF // 256):
            for part in range(2):
                w, wbb = get(hp * 2 + part)
                for h2 in range(2):
                    for gi, (t0, t1) in enumerate(groups):
                        n = (t1 - t0) * 128
                        bi = part * 4 + h2 * 2 + gi
                        p = self.PS[:, bi, 0:n]
                        S.op("pe", lambda e, w=w, p=p, t0=t0, t1=t1, h2=h2: mm(e, w, p, t0, t1, h2),
                             reads=[wbb] + xbs[t0:t1], writes=[self.pbank[bi]])
            for h2 in range(2):
                hc = hp * 2 + h2
                for gi, (t0, t1) in enumerate(groups):
                    n = (t1 - t0) * 128
                    bG = h2 * 2 + gi
                    bU = 4 + bG
                    pG = self.PS[:, bG, 0:n]
                    pU = self.PS[:, bU, 0:n]
                    k = ctr[2] % 2
                    ctr[2] += 1
                    sg = R["sil"][k][:, 0:n]
                    sgb = R["silb"][k]
                    S.op("act", lambda e, sg=sg, pG=pG: e.activation(out=sg, in_=pG, func=AF.Silu),
                         reads=[self.pbank[bG]], writes=[sgb])
                    S.op("dve", lambda e, sg=sg, pU=pU, hc=hc, t0=t0, t1=t1: e.tensor_tensor(
                        out=GT[:, hc, t0 * 128:t1 * 128], in0=sg, in1=pU, op=ALU.mult),
                        reads=[sgb, self.pbank[bU]], writes=[gtb])
        base = 2 * (FF // 256)
        for fp in range(D // 256):
            for q in range(4):
                w, wb_ = get(base + fp * 4 + q)
                for f2 in range(2):
                    for gi, (t0, t1) in enumerate(groups):
                        n = (t1 - t0) * 128
                        bi = f2 * 2 + gi
                        p = self.PS[:, bi, 0:n]

                        def mmb(e, w=w, p=p, t0=t0, t1=t1, f2=f2, q=q):
                            for h in range(14):
                                ins = e.matmul(p, w[:, h, f2 * 128:(f2 + 1) * 128], GT[:, q * 14 + h, t0 * 128:t1 * 128],
                                               start=(q == 0 and h == 0), stop=(q == 3 and h == 13))
                            return ins
                        S.op("pe", mmb, reads=[wb_, gtb], writes=[self.pbank[bi]])
            k = ctr[3] % 2
            ctr[3] += 1
            FT = R["FT"][k]
            ftb = R["ftb"][k]
            for f2 in range(2):
                for gi, (t0, t1) in enumerate(groups):
                    n = (t1 - t0) * 128
                    bi = f2 * 2 + gi
                    p = self.PS[:, bi, 0:n]
                    if (f2 + gi) % 2 == 0:
                        S.op("act", lambda e, p=p, f2=f2, t0=t0, t1=t1, FT=FT: e.activation(
                            out=FT[:, f2, t0 * 128:t1 * 128], in_=p, func=AF.Copy),
                            reads=[self.pbank[bi]], writes=[ftb])
                    else:
                        S.op("dve", lambda e, p=p, f2=f2, t0=t0, t1=t1, FT=FT: e.tensor_copy(FT[:, f2, t0 * 128:t1 * 128], p),
                             reads=[self.pbank[bi]], writes=[ftb])
            for t in range(nt):
                bi = 4 + (ctr[4] % 4)
                ctr[4] += 1
                pv = self.PS[:, bi, 0:256]

                def trb(e, pv=pv, FT=FT, t=t):
                    for f2 in range(2):
                        ins = e.transpose(out=pv[:, f2 * 128:(f2 + 1) * 128], in_=FT[:, f2, t * 128:(t + 1) * 128],
                                          identity=self.identf)
                    return ins
                S.op("pe", trb, reads=[ftb, self.buf("consts")], writes=[self.pbank[bi]])
                sink(t, fp, pv, self.pbank[bi])

    def ffn_resources(self, ntmax):
        R = {}
        R["GT"] = self.sb(56 * ntmax * 64, BF16, [56, ntmax * 128])
        R["gtb"] = self.buf("GT")
        R["stg"] = [self.sb(4096) for _ in range(2)]
        R["stgb"] = [self.buf("stg%d" % i) for i in range(2)]
        R["wbf"] = [self.sb(2048, BF16) for _ in range(3)]
        R["wbfb"] = [self.buf("wbf%d" % i) for i in range(3)]
        R["sil"] = [self.sb(384) for _ in range(2)]
        R["silb"] = [self.buf("sil%d" % i) for i in range(2)]
        R["FT"] = [self.sb(2 * ntmax * 128, F32, [2, ntmax * 128]) for _ in range(2)]
        R["ftb"] = [self.buf("FT%d" % i) for i in range(2)]
        R["OT"] = [self.sb(256) for _ in range(4)]
        R["otb"] = [self.buf("OT%d" % i) for i in range(4)]
        R["ctr"] = [0] * 8
        return R

    def ph_ffn(self, wi):
        S = self.S
        passes = [(0, 6), (6, 12), (12, 17)]
        R = self.ffn_resources(6)
        XT = self.sb(6 * KC * 64, BF16, [6, KC, 128])
        xbs = [self.buf("XT%d" % t) for t in range(6)]
        htb = [self.buf("hT%d" % i) for i in range(NT)]
        fb = [self.buf("fd%d" % i) for i in range(NT)]
        for (a, b) in passes:
            nt = b - a
            for t in range(nt):
                S.op("sp", lambda e, t=t, a=a: e.dma_start(out=XT[:, t, :, :].rearrange("p a b -> p (a b)"), in_=self.hT[a + t]),
                     reads=[htb[a + t]], writes=[xbs[t]], dma=True)

            def sink(t, fp, pv, bankbuf, a=a):
                k = R["ctr"][5] % 4
                R["ctr"][5] += 1
                ot, otb = R["OT"][k], R["otb"][k]
                S.op("dve", lambda e: e.tensor_copy(ot, pv), reads=[bankbuf], writes=[otb])
                S.op("sp", lambda e: e.dma_start(out=self.fd[(a + t) * 128:(a + t + 1) * 128, fp * 256:(fp + 1) * 256], in_=ot),
                     reads=[otb], writes=[fb[a + t]], dma=True)
            self.ffn_pass(XT, xbs, nt, self.w_gu[wi], self.w_dn[wi], sink, R)


    def nextbank(self):
        c = getattr(self, "_bankctr", 0)
        self._bankctr = c + 1
        return c % 8

    def ph_moe(self, wi):
        S = self.S
        htb = [self.buf("hT%d" % i) for i in range(NT)]
        hb = [self.buf("hres%d" % i) for i in range(NT)]
        fb = [self.buf("fd%d" % i) for i in range(NT)]
        WRf = self.sb(128, F32, [KC, 8])
        WRb = self.sb(64, BF16, [KC, 8])
        LG = self.sb(136, F32, [NT, 8])
        M8 = self.sb(136, F32, [NT, 8])
        MASK = self.sb(136, F32, [NT, 8])
        MASK1 = self.sb(136, F32, [NT, 8])
        GATE = self.sb(136, F32, [NT, 8])
        MASKb = self.sb(68, BF16, [NT, 8])
        GATEb = self.sb(68, BF16, [NT, 8])
        RANK = self.sb(136, F32, [NT, 8])
        D1, E1, DEN, G1, G2, DG = [self.sb(NT) for _ in range(6)]
        VAL = self.sb(1)
        ONESf = self.sb(128)
        TRIf = self.sb(128)
        ONESb = self.sb(64, BF16)
        TRIb = self.sb(64, BF16)
        IOTA = self.sb(CAP)
        bw, bl, bm = self.buf("moeW"), self.buf("moeLG"), self.buf("moeM")
        bc = self.buf("moeC")
        S.op("sp", lambda e: e.dma_start(out=WRf, in_=self.m_rt[wi].rearrange("(kc p) e -> p kc e", p=128)), writes=[bw], dma=True)
        S.op("act", lambda e: e.activation(out=WRb, in_=WRf, func=AF.Copy), reads=[bw], writes=[bw])
        S.op("pool", lambda e: e.memset(ONESf, 1.0), writes=[bc])
        S.op("pool", lambda e: e.affine_select(out=TRIf, in_=ONESf, pattern=[[1, 128]], base=-1, channel_multiplier=-1,
                                               compare_op=ALU.is_ge, fill=0.0), reads=[bc], writes=[bc])
        S.op("pool", lambda e: e.tensor_copy(ONESb, ONESf), reads=[bc], writes=[bc])
        S.op("pool", lambda e: e.tensor_copy(TRIb, TRIf), reads=[bc], writes=[bc])
        S.op("pool", lambda e: e.iota(IOTA, pattern=[[1, CAP]], base=0, channel_multiplier=0,
                                      allow_small_or_imprecise_dtypes=True), writes=[bc])
        S.op("pool", lambda e: e.memset(VAL, 0.0), writes=[bc])
        S.op("pool", lambda e: e.memset(VAL[0:NMETA, :], 1.0), reads=[bc], writes=[bc])
        HTi = [self.sb(1024, BF16, [KC, 128]) for _ in range(2)]
        hbuf = [self.buf("moeHT%d" % j) for j in range(2)]
        for i in range(NT):
            j = i % 2
            S.op("sp", lambda e, i=i, j=j: e.dma_start(out=HTi[j].rearrange("p a b -> p (a b)"), in_=self.hT[i]),
                 reads=[htb[i]], writes=[hbuf[j]], dma=True)
            bi = self.nextbank()
            ps = self.PS[:, bi, 0:8]

            def mml(e, j=j, ps=ps):
                for kc in range(KC):
                    ins = e.matmul(ps, HTi[j][:, kc, :], WRb[:, kc, :], start=(kc == 0), stop=(kc == KC - 1))
                return ins
            S.op("pe", mml, reads=[hbuf[j], bw], writes=[self.pbank[bi]])
            S.op("act", lambda e, i=i, ps=ps: e.activation(out=LG[:, i, :], in_=ps, func=AF.Copy),
                 reads=[self.pbank[bi]], writes=[bl])
        for i in range(NT):
            S.op("dve", lambda e, i=i: e.max(out=M8[:, i, :], in_=LG[:, i, :]), reads=[bl], writes=[bm])
        for i in range(NT):
            S.op("dve", lambda e, i=i: e.tensor_scalar(out=MASK[:, i, :], in0=LG[:, i, :], scalar1=M8[:, i, 1:2], scalar2=None,
                                                        op0=ALU.is_ge), reads=[bl, bm], writes=[self.buf("moeMASK")])
            S.op("dve", lambda e, i=i: e.tensor_scalar(out=MASK1[:, i, :], in0=LG[:, i, :], scalar1=M8[:, i, 0:1], scalar2=None,
                                                        op0=ALU.is_equal), reads=[bl, bm], writes=[self.buf("moeMASK1")])
        bk, bk1, bgt = self.buf("moeMASK"), self.buf("moeMASK1"), self.buf("moeG")
        S.op("dve", lambda e: e.tensor_scalar(out=MASK[:, NT - 1, :], in0=MASK[:, NT - 1, :], scalar1=VAL, scalar2=None, op0=ALU.mult),
             reads=[bk, bc], writes=[bk])
        S.op("dve", lambda e: e.tensor_tensor(out=D1, in0=M8[:, :, 1], in1=M8[:, :, 0], op=ALU.subtract), reads=[bm], writes=[bgt])
        S.op("act", lambda e: e.activation(out=E1, in_=D1, func=AF.Exp), reads=[bgt], writes=[bgt])
        S.op("dve", lambda e: e.tensor_scalar(out=DEN, in0=E1, scalar1=1.0, scalar2=None, op0=ALU.add), reads=[bgt], writes=[bgt])
        S.op("dve", lambda e: e.reciprocal(out=G1, in_=DEN), reads=[bgt], writes=[bgt])
        S.op("dve", lambda e: e.tensor_tensor(out=G2, in0=E1, in1=G1, op=ALU.mult), reads=[bgt], writes=[bgt])
        S.op("dve", lambda e: e.tensor_tensor(out=DG, in0=G1, in1=G2, op=ALU.subtract), reads=[bgt], writes=[bgt])
        bga = self.buf("moeGATE")
        for i in range(NT):
            S.op("dve", lambda e, i=i: e.tensor_scalar(out=GATE[:, i, :], in0=MASK[:, i, :], scalar1=G2[:, i:i + 1], scalar2=None,
                                                        op0=ALU.mult), reads=[bk, bgt], writes=[bga])
            S.op("dve", lambda e, i=i: e.scalar_tensor_tensor(out=GATE[:, i, :], in0=MASK1[:, i, :], scalar=DG[:, i:i + 1],
                                                               in1=GATE[:, i, :], op0=ALU.mult, op1=ALU.add),
                 reads=[bk1, bgt, bga], writes=[bga])
        S.op("act", lambda e: e.activation(out=MASKb, in_=MASK, func=AF.Copy), reads=[bk], writes=[self.buf("moeMASKb")])
        S.op("act", lambda e: e.activation(out=GATEb, in_=GATE, func=AF.Copy), reads=[bga], writes=[self.buf("moeGATEb")])
        bkb, bgb, brk = self.buf("moeMASKb"), self.buf("moeGATEb"), self.buf("moeRANK")
        for i in range(NT):
            bi = self.nextbank()
            ps = self.PS[:, bi, 0:8]

            def mmr(e, i=i, ps=ps):
                for j in range(i):
                    e.matmul(ps, ONESb, MASKb[:, j, :], start=(j == 0), stop=False)
                return e.matmul(ps, TRIb, MASKb[:, i, :], start=(i == 0), stop=True)
            S.op("pe", mmr, reads=[bkb, bc], writes=[self.pbank[bi]])
            S.op("act", lambda e, i=i, ps=ps: e.activation(out=RANK[:, i, :], in_=ps, func=AF.Copy),
                 reads=[self.pbank[bi]], writes=[brk])
        if self.cfg.get("dbg_route"):
            dbg = self.dout("dbg_route", [128, 4 * 136])
            for n_, t_ in enumerate((LG, MASK, GATE, RANK)):
                S.op("sp", lambda e, n_=n_, t_=t_: e.dma_start(out=dbg[:, n_ * 136:(n_ + 1) * 136], in_=t_.rearrange("p a b -> p (a b)")),
                     reads=[bl, bk, bga, brk], dma=True)
        base_after_route = self.sb_off
        HTOK = self.sb(NT * 1024, BF16, [NT, D])
        btok = self.buf("HTOK")
        ld = [self.sb(D) for _ in range(2)]
        ldb = [self.buf("moeLD%d" % j) for j in range(2)]
        for i in range(NT):
            j = i % 2
            S.op("sp", lambda e, i=i, j=j: e.dma_start(out=ld[j], in_=self.hres[i * 128:(i + 1) * 128, :]),
                 reads=[hb[i]], writes=[ldb[j]], dma=True)
            if i % 2 == 0:
                S.op("act", lambda e, i=i, j=j: e.activation(out=HTOK[:, i, :], in_=ld[j], func=AF.Copy), reads=[ldb[j]], writes=[btok])
            else:
                S.op("pool", lambda e, i=i, j=j: e.tensor_copy(HTOK[:, i, :], ld[j]), reads=[ldb[j]], writes=[btok])
        SEL = [self.sb(NT * CAP // 2, BF16, [NT, CAP]) for _ in range(2)]
        selb = [self.buf("SEL%d" % j) for j in range(2)]
        XE = [self.sb(CAPT * KC * 64, BF16, [CAPT, KC, 128]) for _ in range(2)]
        xeb = [self.buf("XE%d" % j) for j in range(2)]
        STG = [self.sb(CAPT * 64, BF16, [CAPT, 128]) for _ in range(3)]
        stgb = [self.buf("STG%d" % j) for j in range(3)]
        gsb = self.buf("gs")
        groups = [(0, 3), (3, CAPT)]
        sctr = 0
        for ex in range(NE):
            j = ex % 2
            sel = SEL[j]
            for i in range(NT):
                S.op("dve", lambda e, i=i, sel=sel, ex=ex: e.tensor_scalar(
                    out=sel[:, i, :], in0=IOTA, scalar1=RANK[:, i, ex:ex + 1], scalar2=MASK[:, i, ex:ex + 1],
                    op0=ALU.is_equal, op1=ALU.mult), reads=[brk, bk, bc], writes=[selb[j]])
            xe = XE[j]
            for kc in range(KC):
                for (t0, t1) in groups:
                    n = (t1 - t0) * 128
                    bi = self.nextbank()
                    ps = self.PS[:, bi, 0:n]

                    def mmd(e, kc=kc, t0=t0, t1=t1, ps=ps, sel=sel):
                        for i in range(NT):
                            ins = e.matmul(ps, HTOK[:, i, kc * 128:(kc + 1) * 128], sel[:, i, t0 * 128:t1 * 128],
                                           start=(i == 0), stop=(i == NT - 1))
                        return ins
                    S.op("pe", mmd, reads=[btok, selb[j]], writes=[self.pbank[bi]])
                    dst = xe[:, t0:t1, kc, :]
                    psv = ps.rearrange("p (a b) -> p a b", b=128)
                    if kc % 2 == 0:
                        S.op("act", lambda e, dst=dst, psv=psv: e.activation(out=dst, in_=psv, func=AF.Copy),
                             reads=[self.pbank[bi]], writes=[xeb[j]])
                    else:
                        S.op("dve", lambda e, dst=dst, psv=psv: e.tensor_copy(dst, psv), reads=[self.pbank[bi]], writes=[xeb[j]])
            for t in range(CAPT):
                S.op("sp", lambda e, t=t, ex=ex, xe=xe: e.dma_start(out=self.XS[ex, t], in_=xe[:, t, :, :].rearrange("p a b -> p (a b)")),
                     reads=[xeb[j]], writes=[self.buf("XS%d" % ex)], dma=True)
            for sc in range(CAPT):
                bi = self.nextbank()
                ps = self.PS[:, bi, 0:1]

                def mmg(e, sc=sc, ps=ps, sel=sel, ex=ex):
                    for i in range(NT):
                        ins = e.matmul(ps, sel[:, i, sc * 128:(sc + 1) * 128], GATEb[:, i, ex:ex + 1],
                                       start=(i == 0), stop=(i == NT - 1))
                    return ins
                S.op("pe", mmg, reads=[selb[j], bgb], writes=[self.pbank[bi]])
                S.op("act", lambda e, ps=ps, ex=ex, sc=sc: e.activation(out=self.gs[:, ex * CAPT + sc:ex * CAPT + sc + 1], in_=ps, func=AF.Copy),
                     reads=[self.pbank[bi]], writes=[gsb])
            for i in range(NT):
                bi = self.nextbank()
                pv = self.PS[:, bi, 0:CAPT * 64].bitcast(BF16).rearrange("p (a b) -> p a b", a=CAPT)

                def trs(e, i=i, pv=pv, sel=sel):
                    for sc in range(CAPT):
                        ins = e.transpose(out=pv[:, sc, :], in_=sel[:, i, sc * 128:(sc + 1) * 128], identity=self.identb)
                    return ins
                S.op("pe", trs, reads=[selb[j], self.buf("identb")], writes=[self.pbank[bi]])
                k = sctr % 3
                sctr += 1
                stg = STG[k]
                if i % 2 == 0:
                    S.op("dve", lambda e, stg=stg, pv=pv: e.tensor_copy(stg, pv), reads=[self.pbank[bi]], writes=[stgb[k]])
                else:
                    S.op("act", lambda e, stg=stg, pv=pv: e.activation(out=stg, in_=pv, func=AF.Copy), reads=[self.pbank[bi]], writes=[stgb[k]])
                S.op("sp", lambda e, stg=stg, i=i, ex=ex: e.dma_start(
                    out=self.SELT[i, :, ex * CAP:(ex + 1) * CAP], in_=stg.rearrange("p a b -> p (a b)")),
                    reads=[stgb[k]], writes=[self.buf("SELT%d" % i)], dma=True)
        S.barrier()
        self.sb_reset()
        R = self.ffn_resources(CAPT)
        XT = self.sb(CAPT * KC * 64, BF16, [CAPT, KC, 128])
        xbs = [self.buf("XT%d" % t) for t in range(CAPT)]
        OTb = [self.sb(128, BF16) for _ in range(4)]
        fab = self.buf("FA")
        for ex in range(NE):
            for t in range(CAPT):
                S.op("sp", lambda e, t=t, ex=ex: e.dma_start(out=XT[:, t, :, :].rearrange("p a b -> p (a b)"), in_=self.XS[ex, t]),
                     reads=[self.buf("XS%d" % ex)], writes=[xbs[t]], dma=True)

            def sink(t, fp, pv, bankbuf, ex=ex):
                k = R["ctr"][5] % 4
                R["ctr"][5] += 1
                ot, otb = OTb[k], R["otb"][k]
                g = self.gs[:, ex * CAPT + t:ex * CAPT + t + 1]
                S.op("dve", lambda e: e.tensor_scalar(out=ot, in0=pv, scalar1=g, scalar2=None, op0=ALU.mult),
                     reads=[bankbuf, gsb], writes=[otb])
                r0 = ex * CAP + t * 128
                S.op("sp", lambda e: e.dma_start(out=self.FA[r0:r0 + 128, fp * 256:(fp + 1) * 256], in_=ot),
                     reads=[otb], writes=[fab], dma=True)
            self.ffn_pass(XT, xbs, CAPT, self.m_gu[wi, ex], self.m_dn[wi, ex], sink, R)
        S.barrier()
        self.sb_reset()
        NC_ = NE * CAPT
        FAh = self.sb(NC_ * 512, BF16, [NC_, 1024])
        fahb = self.buf("FAh")
        STi = [self.sb(NC_ * 64, BF16, [NC_, 128]) for _ in range(2)]
        stib = [self.buf("STi%d" % j) for j in range(2)]
        OC = [self.sb(1024) for _ in range(2)]
        ocb = [self.buf("OC%d" % j) for j in range(2)]
        FAv = self.FA.rearrange("(c p) f -> p c f", p=128)
        cc = 0
        for h in range(2):
            for ex in range(NE):
                S.op("sp", lambda e, ex=ex, h=h: e.dma_start(out=FAh[:, ex * CAPT:(ex + 1) * CAPT, :],
                                                              in_=FAv[:, ex * CAPT:(ex + 1) * CAPT, h * 1024:(h + 1) * 1024]),
                     reads=[fab], writes=[fahb], dma=True)
            for i in range(NT):
                j = cc % 2
                cc += 1
                S.op("sp", lambda e, i=i, j=j: e.dma_start(out=STi[j].rearrange("p a b -> p (a b)"), in_=self.SELT[i]),
                     reads=[self.buf("SELT%d" % i)], writes=[stib[j]], dma=True)
                for nch in range(2):
                    bi = self.nextbank()
                    ps = self.PS[:, bi, :]

                    def mmc(e, j=j, nch=nch, ps=ps):
                        for c in range(NC_):
                            ins = e.matmul(ps, STi[j][:, c, :], FAh[:, c, nch * 512:(nch + 1) * 512],
                                           start=(c == 0), stop=(c == NC_ - 1))
                        return ins
                    S.op("pe", mmc, reads=[stib[j], fahb], writes=[self.pbank[bi]])
                    if nch == 0:
                        S.op("act", lambda e, j=j, ps=ps: e.activation(out=OC[j][:, 0:512], in_=ps, func=AF.Copy),
                             reads=[self.pbank[bi]], writes=[ocb[j]])
                    else:
                        S.op("dve", lambda e, j=j, ps=ps: e.tensor_copy(OC[j][:, 512:1024], ps),
                             reads=[self.pbank[bi]], writes=[ocb[j]])
                S.op("sp", lambda e, i=i, j=j, h=h: e.dma_start(out=self.fd[i * 128:(i + 1) * 128, h * 1024:(h + 1) * 1024], in_=OC[j]),
                     reads=[ocb[j]], writes=[fb[i]], dma=True)


    def ph_kv(self):
        S = self.S
        htb = [self.buf("hT%d" % i) for i in range(NT)]
        wkv = self.din("kv_w_shared", [D, 512])
        self.kT2 = self.dscr("kT2", [128, 4 * TP], BF16)
        self.Vaug = self.dscr("Vaug", [NT, 128, 4 * 66], BF16)
        WKf = self.sb(KC * 512, F32, [KC, 512])
        WK2 = self.sb(KC * 4 * 64, BF16, [KC, 4, 128])
        WVb = self.sb(KC * 128, BF16, [KC, 256])
        XTall = self.sb(NT * 1024, BF16, [NT, KC, 128])
        KT = self.sb(4 * TP // 2, BF16, [4, TP])
        VA = [self.sb(4 * 33, BF16, [4, 66]) for _ in range(2)]
        bw, bw2, bx, bkt = self.buf("kvW"), self.buf("kvW2"), self.buf("kvX"), self.buf("kvKT")
        vab = [self.buf("kvVA%d" % j) for j in range(2)]
        S.op("sp", lambda e: e.dma_start(out=WKf, in_=wkv.rearrange("(kc p) f -> p kc f", p=128)), writes=[bw], dma=True)
        for g in range(4):
            for hf in range(2):
                eng = "act" if hf == 0 else "pool"
                if eng == "act":
                    S.op("act", lambda e, g=g, hf=hf: e.activation(out=WK2[:, :, g, hf * 64:(hf + 1) * 64], in_=WKf[:, :, g * 64:(g + 1) * 64], func=AF.Copy),
                         reads=[bw], writes=[bw2])
                else:
                    S.op("pool", lambda e, g=g, hf=hf: e.tensor_copy(WK2[:, :, g, hf * 64:(hf + 1) * 64], WKf[:, :, g * 64:(g + 1) * 64]),
                         reads=[bw], writes=[bw2])
        S.op("act", lambda e: e.activation(out=WVb, in_=WKf[:, :, 256:512], func=AF.Copy), reads=[bw], writes=[bw2])
        for i in range(NT):
            S.op("sp", lambda e, i=i: e.dma_start(out=XTall[:, i, :, :].rearrange("p a b -> p (a b)"), in_=self.hT[i]),
                 reads=[htb[i]], writes=[bx], dma=True)
        tg = [(0, 4), (4, 8), (8, 12), (12, 16), (16, 17)]
        for (t0, t1) in tg:
            n = (t1 - t0) * 128
            for g in range(4):
                bi = self.nextbank()
                ps = self.PS[:, bi, 0:n]

                def mmk(e, g=g, t0=t0, t1=t1, ps=ps):
                    for kc in range(KC):
                        ins = e.matmul(ps, WK2[:, kc, g, :], XTall[:, t0:t1, kc, :], start=(kc == 0), stop=(kc == KC - 1))
                    return ins
                S.op("pe", mmk, reads=[bw2, bx], writes=[self.pbank[bi]])
                if g % 2 == 0:
                    S.op("act", lambda e, g=g, t0=t0, t1=t1, ps=ps: e.activation(out=KT[:, g, t0 * 128:t1 * 128], in_=ps, func=AF.Copy),
                         reads=[self.pbank[bi]], writes=[bkt])
                else:
                    S.op("dve", lambda e, g=g, t0=t0, t1=t1, ps=ps: e.tensor_copy(KT[:, g, t0 * 128:t1 * 128], ps),
                         reads=[self.pbank[bi]], writes=[bkt])
        S.op("sp", lambda e: e.dma_start(out=self.kT2, in_=KT.rearrange("p a b -> p (a b)")), reads=[bkt], writes=[self.buf("kT2d")], dma=True)
        for i in range(NT):
            j = i % 2
            bi = self.nextbank()
            ps = self.PS[:, bi, 0:256]

            def mmv(e, i=i, ps=ps):
                for kc in range(KC):
                    ins = e.matmul(ps, XTall[:, i, kc, :], WVb[:, kc, :], start=(kc == 0), stop=(kc == KC - 1))
                return ins
            S.op("pe", mmv, reads=[bw2, bx], writes=[self.pbank[bi]])
            S.op("pool", lambda e, j=j: e.memset(VA[j][:, :, 64:66], 1.0), writes=[vab[j]])
            S.op("act", lambda e, j=j, ps=ps: e.activation(out=VA[j][:, :, 0:64], in_=ps.rearrange("p (a b) -> p a b", a=4), func=AF.Copy),
                 reads=[self.pbank[bi], vab[j]], writes=[vab[j]])
            S.op("sp", lambda e, i=i, j=j: e.dma_start(out=self.Vaug[i], in_=VA[j].rearrange("p a b -> p (a b)")),
                 reads=[vab[j]], writes=[self.buf("Vaugd")], dma=True)

    def ph_bias(self):
        S = self.S
        tab = self.din("rel_bias_table", [32, 32])
        ohs = {"cur": (128, 128), "prev": (128, 128), "meta0": (128, 16), "metac": (128, 16), "mm": (16, 16)}
        self.bias_d = {}
        T33 = self.sb(32)
        bt = self.buf("biasT")
        S.op("pool", lambda e: e.memset(T33, -30000.0), writes=[bt])
        S.op("sp", lambda e: e.dma_start(out=T33[0:32, :], in_=tab), reads=[bt], writes=[bt], dma=True)
        OH = [self.sb(128 * 128, F32, [128, 128]) for _ in range(2)]
        ohb = [self.buf("OH%d" % j) for j in range(2)]
        BT = [self.sb(32 * 128, F32, [32, 128]) for _ in range(2)]
        btb = [self.buf("BT%d" % j) for j in range(2)]
        for n_, (name, (nq, ns)) in enumerate(ohs.items()):
            j = n_ % 2
            src = self.din("oh_" + name, [33, nq * ns])
            dst = self.dscr("bias_" + name, [128, 32 * 128])
            self.bias_d[name] = dst
            oh = OH[j][:, 0:nq, 0:ns] if False else OH[j].rearrange("p a b -> p (a b)")[:, 0:nq * ns].rearrange("p (a b) -> p a b", a=nq)
            S.op("sp", lambda e, oh=oh, src=src: e.dma_start(out=oh[0:33].rearrange("p a b -> p (a b)"), in_=src), writes=[ohb[j]], dma=True)
            btile = BT[j]
            S.op("pool", lambda e, btile=btile: e.memset(btile, 0.0), writes=[btb[j]])
            for q0 in range(0, nq, 16):
                bi = self.nextbank()
                ps = self.PS[:, bi, :].rearrange("p (q h) -> p q h", q=16)

                def mmb(e, q0=q0, ps=ps, oh=oh, ns=ns):
                    for q in range(16):
                        ins = e.matmul(ps[0:ns, q, :], oh[0:33, q0 + q, :], T33[0:33, :], start=True, stop=True)
                    return ins
                S.op("pe", mmb, reads=[ohb[j], bt], writes=[self.pbank[bi]])
                S.op("dve", lambda e, q0=q0, ps=ps, btile=btile, ns=ns: e.tensor_copy(
                    btile[0:ns, :, q0:q0 + 16], ps[0:ns, :, :].rearrange("p q h -> p h q")),
                    reads=[self.pbank[bi]], writes=[btb[j]])
            S.op("sp", lambda e, dst=dst, btile=btile: e.dma_start(out=dst, in_=btile.rearrange("p a b -> p (a b)")),
                 reads=[btb[j]], writes=[self.buf("biasd")], dma=True)

    def ph_swa(self, jb):
        S = self.S
        htb = [self.buf("hT%d" % i) for i in range(NT)]
        fb = [self.buf("fd%d" % i) for i in range(NT)]
        wq = self.w_q[jb].rearrange("(kc p) f -> p kc f", p=128)
        wo = self.w_o[jb].rearrange("(kc p) f -> p kc f", p=128)
        QA = self.sb(KC * TP // 2, BF16, [KC, TP])
        qab = [self.buf("QA%d" % i) for i in range(NT)]
        base1 = self.sb_off
        XTall = self.sb(NT * 1024, BF16, [NT, KC, 128])
        bx = self.buf("swaX")
        stg = [self.sb(4096, F32, [KC, 256]) for _ in range(2)]
        stgb = [self.buf("swaStg%d" % j) for j in range(2)]
        wbf = [self.sb(2048, BF16, [KC, 256]) for _ in range(3)]
        wbfb = [self.buf("swaW%d" % j) for j in range(3)]
        for i in range(NT):
            S.op("sp", lambda e, i=i: e.dma_start(out=XTall[:, i, :, :].rearrange("p a b -> p (a b)"), in_=self.hT[i]),
                 reads=[htb[i]], writes=[bx], dma=True)
        tg = [(0, 4), (4, 8), (8, 12), (12, 16), (16, 17)]
        for cp in range(8):
            j, j2 = cp % 2, cp % 3
            S.op("sp", lambda e, cp=cp, j=j: e.dma_start(out=stg[j], in_=wq[:, :, cp * 256:(cp + 1) * 256]), writes=[stgb[j]], dma=True)
            S.op("pool", lambda e, j=j, j2=j2: e.tensor_copy(wbf[j2], stg[j]), reads=[stgb[j]], writes=[wbfb[j2]])
            for c2 in range(2):
                c = cp * 2 + c2
                for (t0, t1) in tg:
                    n = (t1 - t0) * 128
                    bi = self.nextbank()
                    ps = self.PS[:, bi, 0:n]

                    def mmq(e, j2=j2, c2=c2, t0=t0, t1=t1, ps=ps):
                        for kc in range(KC):
                            ins = e.matmul(ps, wbf[j2][:, kc, c2 * 128:(c2 + 1) * 128], XTall[:, t0:t1, kc, :],
                                           start=(kc == 0), stop=(kc == KC - 1))
                        return ins
                    S.op("pe", mmq, reads=[wbfb[j2], bx], writes=[self.pbank[bi]])
                    if (t0 // 4) % 2 == 0:
                        S.op("act", lambda e, c=c, t0=t0, t1=t1, ps=ps: e.activation(out=QA[:, c, t0 * 128:t1 * 128], in_=ps, func=AF.Copy),
                             reads=[self.pbank[bi]], writes=qab[t0:t1])
                    else:
                        S.op("dve", lambda e, c=c, t0=t0, t1=t1, ps=ps: e.tensor_copy(QA[:, c, t0 * 128:t1 * 128], ps),
                             reads=[self.pbank[bi]], writes=qab[t0:t1])
        S.barrier()
        self.sb_reset(base1)
        KTh = [self.sb(4 * TP // 2, BF16, [4, TP]) for _ in range(2)]
        VA = self.sb(NT * 4 * 33, BF16, [NT, 4, 66])
        Bc = self.sb(4096, F32, [32, 128])
        Bp = self.sb(4096, F32, [32, 128])
        Bm0 = self.sb(4096, F32, [32, 128])
        Bmc = self.sb(4096, F32, [32, 128])
        Bmm = self.sb(512, F32, [32, 16])
        SK = self.sb(32)
        SKe = self.sb(32)
        bkv, bbias, bsk = self.buf("swaKV"), self.buf("swaBias"), self.buf("swaSK")
        for hf_ in range(2):
            S.op("sp", lambda e, hf_=hf_: e.dma_start(out=KTh[hf_].rearrange("p a b -> p (a b)"), in_=self.kT2),
                 reads=[self.buf("kT2d")], writes=[bkv], dma=True)
        S.op("pool", lambda e: e.memset(KTh[0][64:128], 0.0), reads=[bkv], writes=[bkv])
        S.op("pool", lambda e: e.memset(KTh[1][0:64], 0.0), reads=[bkv], writes=[bkv])
        S.op("sp", lambda e: e.dma_start(out=VA.rearrange("p t a b -> p t (a b)"), in_=self.Vaug.rearrange("t p f -> p t f")),
             reads=[self.buf("Vaugd")], writes=[bkv], dma=True)
        for t_, nm in ((Bc, "cur"), (Bp, "prev"), (Bm0, "meta0"), (Bmc, "metac")):
            S.op("sp", lambda e, t_=t_, nm=nm: e.dma_start(out=t_.rearrange("p a b -> p (a b)"), in_=self.bias_d[nm]),
                 reads=[self.buf("biasd")], writes=[bbias], dma=True)
        S.op("sp", lambda e: e.dma_start(out=Bmm, in_=self.bias_d["mm"].rearrange("p (h q) -> p h q", h=32)[:, :, 0:16]),
             reads=[self.buf("biasd")], writes=[bbias], dma=True)
        S.op("sp", lambda e: e.dma_start(out=SK, in_=self.sinks[jb:jb + 1, :].partition_broadcast(128)), writes=[bsk], dma=True)
        S.op("act", lambda e: e.activation(out=SKe, in_=SK, func=AF.Exp), reads=[bsk], writes=[bsk])
        TMP = [self.sb(512) for _ in range(3)]
        tmpb = [self.buf("swaTMP%d" % j) for j in range(3)]
        EX = [self.sb(256, BF16) for _ in range(4)]
        exb = [self.buf("swaEX%d" % j) for j in range(4)]
        DEN = [self.sb(4) for _ in range(2)]
        denb = [self.buf("swaDEN%d" % j) for j in range(2)]
        AO = [self.sb(1024, BF16) for _ in range(2)]
        aob = [self.buf("swaAO%d" % j) for j in range(2)]
        cnt = [0, 0, 0]
        blocks = list(range(16)) + [16]
        for blk in blocks:
            meta_q = (blk == 16)
            nq = NMETA if meta_q else 128
            qs = slice(blk * 128, blk * 128 + nq)
            ao = AO[blk % 2]
            aobuf = aob[blk % 2]
            if meta_q:
                S.op("pool", lambda e, ao=ao: e.memset(ao, 0.0), writes=[aobuf])
            for hg in range(8):
                g = hg // 2
                pieces = []
                if meta_q:
                    pieces.append((slice(2048, 2064), 16, Bmm, 16))
                else:
                    pieces.append((slice(blk * 128, blk * 128 + 128), blk, Bc, 128))
                    if blk > 0:
                        pieces.append((slice((blk - 1) * 128, blk * 128), blk - 1, Bp, 128))
                    pieces.append((slice(2048, 2064), 16, Bm0 if blk == 0 else Bmc, 16))
                exs = []
                for (ks, vt, btile, ns) in pieces:
                    bi = self.nextbank()
                    ps = self.PS[:, bi, :].rearrange("p (h q) -> p h q", h=4)

                    def mms(e, ks=ks, ns=ns, ps=ps, hg=hg, g=g, qs=qs, nq=nq):
                        for hh in range(4):
                            h = hg * 4 + hh
                            kc, hf = h // 2, h % 2
                            ins = e.matmul(ps[0:ns, hh, 0:nq], KTh[hf][:, g, ks], QA[:, kc, qs], start=True, stop=True)
                        return ins
                    S.op("pe", mms, reads=[bkv, qab[blk]], writes=[self.pbank[bi]])
                    k = cnt[0] % 3
                    cnt[0] += 1
                    tmp = TMP[k].rearrange("p (h q) -> p h q", h=4)
                    S.op("dve", lambda e, tmp=tmp, ps=ps, ns=ns, nq=nq, btile=btile, hg=hg: e.scalar_tensor_tensor(
                        out=tmp[0:ns, :, 0:nq], in0=ps[0:ns, :, 0:nq], scalar=0.125, in1=btile[0:ns, hg * 4:(hg + 1) * 4, 0:nq],
                        op0=ALU.mult, op1=ALU.add), reads=[self.pbank[bi], bbias], writes=[tmpb[k]])
                    k2 = cnt[1] % 4
                    cnt[1] += 1
                    ex = EX[k2].rearrange("p (h q) -> p h q", h=4)
                    S.op("act", lambda e, ex=ex, tmp=tmp, ns=ns, nq=nq: e.activation(out=ex[0:ns, :, 0:nq], in_=tmp[0:ns, :, 0:nq], func=AF.Exp),
                         reads=[tmpb[k]], writes=[exb[k2]])
                    exs.append((ex, exb[k2], vt, ns))
                bi = self.nextbank()
                po = self.PS[:, bi, :].rearrange("p (h q) -> p h q", h=4)

                def mmo(e, exs=exs, po=po, g=g, nq=nq):
                    for hh in range(4):
                        for n_, (ex, _, vt, ns) in enumerate(exs):
                            ins = e.matmul(po[0:nq, hh, 0:65], ex[0:ns, hh, 0:nq], VA[0:ns, vt, g, 0:65],
                                           start=(n_ == 0), stop=(n_ == len(exs) - 1))
                    return ins
                S.op("pe", mmo, reads=[bkv] + [x[1] for x in exs], writes=[self.pbank[bi]])
                k3 = cnt[2] % 2
                cnt[2] += 1
                den = DEN[k3]
                S.op("dve", lambda e, den=den, po=po, hg=hg, nq=nq: e.tensor_tensor(
                    out=den[0:nq, :], in0=po[0:nq, :, 64], in1=SKe[0:nq, hg * 4:(hg + 1) * 4], op=ALU.add),
                    reads=[self.pbank[bi], bsk], writes=[denb[k3]])
                S.op("dve", lambda e, den=den, nq=nq: e.reciprocal(out=den[0:nq, :], in_=den[0:nq, :]), reads=[denb[k3]], writes=[denb[k3]])
                for hh in range(4):
                    h = hg * 4 + hh
                    if hh % 2 == 0:
                        S.op("act", lambda e, ao=ao, po=po, den=den, h=h, hh=hh, nq=nq: e.activation(
                            out=ao[0:nq, h * 64:(h + 1) * 64], in_=po[0:nq, hh, 0:64], func=AF.Copy, scale=den[0:nq, hh:hh + 1]),
                            reads=[self.pbank[bi], denb[k3]], writes=[aobuf])
                    else:
                        S.op("dve", lambda e, ao=ao, po=po, den=den, h=h, hh=hh, nq=nq: e.tensor_scalar(
                            out=ao[0:nq, h * 64:(h + 1) * 64], in0=po[0:nq, hh, 0:64], scalar1=den[0:nq, hh:hh + 1], scalar2=None, op0=ALU.mult),
                            reads=[self.pbank[bi], denb[k3]], writes=[aobuf])
            for half in range(2):
                bi = self.nextbank()
                pv = self.PS[:, bi, :].bitcast(BF16).rearrange("p (a b) -> p a b", a=8)

                def tr(e, ao=ao, pv=pv, half=half):
                    for q in range(8):
                        kc = half * 8 + q
                        ins = e.transpose(out=pv[:, q, :], in_=ao[:, kc * 128:(kc + 1) * 128], identity=self.identb)
                    return ins
                S.op("pe", tr, reads=[aobuf, self.buf("identb")], writes=[self.pbank[bi]])
                dst = QA[:, half * 8:(half + 1) * 8, blk * 128:(blk + 1) * 128]
                if half == 0:
                    S.op("dve", lambda e, dst=dst, pv=pv: e.tensor_copy(dst, pv), reads=[self.pbank[bi]], writes=[qab[blk]])
                else:
                    S.op("act", lambda e, dst=dst, pv=pv: e.activation(out=dst, in_=pv, func=AF.Copy), reads=[self.pbank[bi]], writes=[qab[blk]])
        self.outproj(QA, qab, wo, base1)

    def outproj(self, QA, qab, wo, base1):
        S = self.S
        fb = [self.buf("fd%d" % i) for i in range(NT)]
        S.barrier()
        self.sb_reset(base1)
        WO = self.sb(KC * 1024, BF16, [KC, D])
        wob = self.buf("swaWO")
        sgO = [self.sb(4096, F32, [KC, 256]) for _ in range(2)]
        sgO_b = [self.buf("swaStgO%d" % j) for j in range(2)]
        OT = [self.sb(D) for _ in range(2)]
        otb = [self.buf("swaOT%d" % j) for j in range(2)]
        for cp in range(8):
            j = cp % 2
            S.op("sp", lambda e, cp=cp, j=j: e.dma_start(out=sgO[j], in_=wo[:, :, cp * 256:(cp + 1) * 256]), writes=[sgO_b[j]], dma=True)
            if cp % 2 == 0:
                S.op("pool", lambda e, j=j, cp=cp: e.tensor_copy(WO[:, :, cp * 256:(cp + 1) * 256], sgO[j]), reads=[sgO_b[j]], writes=[wob])
            else:
                S.op("act", lambda e, j=j, cp=cp: e.activation(out=WO[:, :, cp * 256:(cp + 1) * 256], in_=sgO[j], func=AF.Copy), reads=[sgO_b[j]], writes=[wob])
        for i in range(NT):
            j = i % 2
            for n in range(4):
                bi = self.nextbank()
                ps = self.PS[:, bi, :]

                def mmw(e, i=i, n=n, ps=ps):
                    for kc in range(KC):
                        ins = e.matmul(ps, QA[:, kc, i * 128:(i + 1) * 128], WO[:, kc, n * 512:(n + 1) * 512],
                                       start=(kc == 0), stop=(kc == KC - 1))
                    return ins
                S.op("pe", mmw, reads=[wob, qab[i]], writes=[self.pbank[bi]])
                if n % 2 == 0:
                    S.op("act", lambda e, j=j, n=n, ps=ps: e.activation(out=OT[j][:, n * 512:(n + 1) * 512], in_=ps, func=AF.Copy),
                         reads=[self.pbank[bi]], writes=[otb[j]])
                else:
                    S.op("dve", lambda e, j=j, n=n, ps=ps: e.tensor_copy(OT[j][:, n * 512:(n + 1) * 512], ps),
                         reads=[self.pbank[bi]], writes=[otb[j]])
            S.op("sp", lambda e, i=i, j=j: e.dma_start(out=self.fd[i * 128:(i + 1) * 128, :], in_=OT[j]),
                 reads=[otb[j]], writes=[fb[i]], dma=True)


    def ph_gla(self, li):
        S = self.S
        htb = [self.buf("hT%d" % i) for i in range(NT)]
        w_in = self.g_win[li].rearrange("(kc p) f -> p kc f", p=128)
        wo = self.g_wout[li].rearrange("(kc p) f -> p kc f", p=128)
        if not hasattr(self, "Vd"):
            self.Vd = self.dscr("Vd", [TP, D], BF16)
            self.Rd = self.dscr("Rd", [TP, D], BF16)
        QK = self.sb(KC * TP // 2, BF16, [KC, TP])
        qkb = [self.buf("QK%d" % i) for i in range(NT)]
        base1 = self.sb_off
        GL = self.sb(TP)
        glb = self.buf("GL")
        base2 = self.sb_off
        XTall = self.sb(NT * 1024, BF16, [NT, KC, 128])
        bx = self.buf("glaX")
        g1stg = [self.sb(4096, F32, [KC, 256]) for _ in range(2)]
        g1stgb = [self.buf("glaStg%d" % j) for j in range(2)]
        g1w = [self.sb(2048, BF16, [KC, 256]) for _ in range(3)]
        g1wb = [self.buf("glaW%d" % j) for j in range(3)]
        VO = [self.sb(128, BF16) for _ in range(4)]
        vob = [self.buf("glaVO%d" % j) for j in range(4)]
        for i in range(NT):
            S.op("sp", lambda e, i=i: e.dma_start(out=XTall[:, i, :, :].rearrange("p a b -> p (a b)"), in_=self.hT[i]),
                 reads=[htb[i]], writes=[bx], dma=True)
        tg = [(0, 4), (4, 8), (8, 12), (12, 16), (16, 17)]
        vctr = [0]
        vdb, rdb = self.buf("Vd"), self.buf("Rd")
        for ct in range(25):
            j, j2 = ct % 2, ct % 3
            ncol = 256 if ct < 24 else 16
            stg_v = g1stg[j][:, :, 0:ncol]
            w_v = g1w[j2][:, :, 0:ncol]
            S.op("sp", lambda e, ct=ct, stg_v=stg_v, ncol=ncol: e.dma_start(out=stg_v, in_=w_in[:, :, ct * 256:ct * 256 + ncol]),
                 writes=[g1stgb[j]], dma=True)
            S.op("pool", lambda e, w_v=w_v, stg_v=stg_v: e.tensor_copy(w_v, stg_v), reads=[g1stgb[j]], writes=[g1wb[j2]])
            if ct < 8 or ct == 24:
                for c2 in range(2 if ct < 24 else 1):
                    mcols = 128 if ct < 24 else 16
                    for (t0, t1) in tg:
                        n = (t1 - t0) * 128
                        bi = self.nextbank()
                        ps = self.PS[0:mcols, bi, 0:n]

                        def mmq(e, w_v=w_v, c2=c2, t0=t0, t1=t1, ps=ps, mcols=mcols):
                            for kc in range(KC):
                                ins = e.matmul(ps, w_v[:, kc, c2 * 128:c2 * 128 + mcols], XTall[:, t0:t1, kc, :],
                                               start=(kc == 0), stop=(kc == KC - 1))
                            return ins
                        S.op("pe", mmq, reads=[g1wb[j2], bx], writes=[self.pbank[bi]])
                        if ct == 24:
                            S.op("act", lambda e, t0=t0, t1=t1, ps=ps: e.activation(out=GL[0:16, t0 * 128:t1 * 128], in_=ps, func=AF.Copy),
                                 reads=[self.pbank[bi]], writes=[glb])
                        else:
                            c = ct * 2 + c2
                            if (t0 // 4) % 2 == 0:
                                S.op("act", lambda e, c=c, t0=t0, t1=t1, ps=ps: e.activation(out=QK[:, c, t0 * 128:t1 * 128], in_=ps, func=AF.Copy),
                                     reads=[self.pbank[bi]], writes=qkb[t0:t1])
                            else:
                                S.op("dve", lambda e, c=c, t0=t0, t1=t1, ps=ps: e.tensor_copy(QK[:, c, t0 * 128:t1 * 128], ps),
                                     reads=[self.pbank[bi]], writes=qkb[t0:t1])
            else:
                is_r = ct >= 16
                col0 = (ct - 8) * 256 if not is_r else (ct - 16) * 256
                dstd = self.Rd if is_r else self.Vd
                dbuf = rdb if is_r else vdb
                for i in range(NT):
                    bi = self.nextbank()
                    ps = self.PS[:, bi, 0:256]

                    def mmv(e, w_v=w_v, i=i, ps=ps):
                        for kc in range(KC):
                            ins = e.matmul(ps, XTall[:, i, kc, :], w_v[:, kc, :], start=(kc == 0), stop=(kc == KC - 1))
                        return ins
                    S.op("pe", mmv, reads=[g1wb[j2], bx], writes=[self.pbank[bi]])
                    k = vctr[0] % 4
                    vctr[0] += 1
                    vo = VO[k]
                    if is_r:
                        S.op("act", lambda e, vo=vo, ps=ps: e.activation(out=vo, in_=ps, func=AF.Silu), reads=[self.pbank[bi]], writes=[vob[k]])
                    else:
                        S.op("dve", lambda e, vo=vo, ps=ps: e.tensor_copy(vo, ps), reads=[self.pbank[bi]], writes=[vob[k]])
                    S.op("sp", lambda e, vo=vo, i=i, col0=col0, dstd=dstd: e.dma_start(out=dstd[i * 128:(i + 1) * 128, col0:col0 + 256], in_=vo),
                         reads=[vob[k]], writes=[dbuf], dma=True)
        S.barrier()
        self.sb_reset(base2)
        KS = self.sb(8 * TP // 2, BF16, [8, TP])
        ksb = [self.buf("KS%d" % i) for i in range(NT)]
        DEC = self.sb(8 * 34, F32, [8, 34])
        decb = self.buf("DEC")
        WG2 = self.sb(1024)
        NB_ = self.sb(8)
        ONE1 = self.sb(1)
        M01 = self.sb(512)
        bcst = self.buf("glaC")
        S.op("sp", lambda e: e.dma_start(out=WG2[0:16, :], in_=self.g_wg2[li]), writes=[bcst], dma=True)
        S.op("sp", lambda e: e.dma_start(out=NB_, in_=self.g_bg[li].rearrange("(j p) -> p j", p=128), allow_slow_non_contiguous=True),
             writes=[bcst], dma=True)
        S.op("dve", lambda e: e.tensor_scalar(out=NB_, in0=NB_, scalar1=-1.0, scalar2=None, op0=ALU.mult), reads=[bcst], writes=[bcst])
        S.op("pool", lambda e: e.memset(ONE1, 1.0), writes=[bcst])
        S.op("pool", lambda e: e.memset(M01, 1.0), writes=[bcst])
        S.op("pool", lambda e: e.memset(M01.rearrange("p (c t) -> p c t", t=64)[:, :, 0:1], 0.0), reads=[bcst], writes=[bcst])
        NTMP = 2
        T1 = [self.sb(512) for _ in range(NTMP)]
        CS = [self.sb(512) for _ in range(NTMP)]
        EB = [self.sb(512) for _ in range(NTMP)]
        EBi = [self.sb(512) for _ in range(NTMP)]
        DF = [self.sb(512) for _ in range(NTMP)]
        tb = [[self.buf("gla%s%d" % (nm, j)) for j in range(NTMP)] for nm in ("T1", "CS", "EB", "EBi", "DF")]
        it = 0
        for (t0, t1) in tg:
            meta_g = (t0 == 16)
            n = 16 if meta_g else (t1 - t0) * 128
            csz = 16 if meta_g else 64
            nch = n // csz
            c0 = 32 if meta_g else t0 * 2
            tsl = slice(t0 * 128, t0 * 128 + n)
            for j in range(8):
                k = it % NTMP
                it += 1
                bi = self.nextbank()
                ps = self.PS[:, bi, 0:n]
                S.op("pe", lambda e, ps=ps, j=j, tsl=tsl: e.matmul(ps, WG2[0:16, j * 128:(j + 1) * 128], GL[0:16, tsl], start=True, stop=True),
                     reads=[bcst, glb], writes=[self.pbank[bi]])
                t1_, cs_, eb_, ebi_, df_ = T1[k][:, 0:n], CS[k][:, 0:n], EB[k][:, 0:n], EBi[k][:, 0:n], DF[k][:, 0:n]
                S.op("act", lambda e, t1_=t1_, ps=ps, j=j: e.activation(out=t1_, in_=ps, func=AF.Exp, bias=NB_[:, j:j + 1], scale=-1.0),
                     reads=[self.pbank[bi], bcst], writes=[tb[0][k]])
                S.op("act", lambda e, t1_=t1_: e.activation(out=t1_, in_=t1_, func=AF.Ln, bias=ONE1, scale=1.0),
                     reads=[tb[0][k], bcst], writes=[tb[0][k]])
                S.op("dve", lambda e, cs_=cs_, t1_=t1_, n=n: e.tensor_tensor_scan(out=cs_, data0=M01[:, 0:n], data1=t1_, initial=0.0,
                                                                                 op0=ALU.mult, op1=ALU.add),
                     reads=[tb[0][k], bcst], writes=[tb[1][k]])
                S.op("act", lambda e, eb_=eb_, cs_=cs_: e.activation(out=eb_, in_=cs_, func=AF.Exp, scale=-1.0 / 16), reads=[tb[1][k]], writes=[tb[2][k]])
                S.op("act", lambda e, ebi_=ebi_, cs_=cs_: e.activation(out=ebi_, in_=cs_, func=AF.Exp, scale=1.0 / 16), reads=[tb[1][k]], writes=[tb[3][k]])
                for c in range(nch):
                    S.op("dve", lambda e, df_=df_, cs_=cs_, c=c, csz=csz: e.tensor_scalar(
                        out=df_[:, c * csz:(c + 1) * csz], in0=cs_[:, c * csz:(c + 1) * csz], scalar1=cs_[:, (c + 1) * csz - 1:(c + 1) * csz],
                        scalar2=None, op0=ALU.subtract), reads=[tb[1][k]], writes=[tb[4][k]])
                S.op("act", lambda e, df_=df_: e.activation(out=df_, in_=df_, func=AF.Exp, scale=1.0 / 16), reads=[tb[4][k]], writes=[tb[4][k]])
                S.op("act", lambda e, cs_=cs_, j=j, c0=c0, nch=nch, csz=csz: e.activation(
                    out=DEC[:, j, c0:c0 + nch], in_=cs_.rearrange("p (c t) -> p c t", t=csz)[:, :, csz - 1], func=AF.Exp, scale=-1.0 / 16),
                    reads=[tb[1][k]], writes=[decb])
                tiles_b = qkb[t0:t1]
                S.op("dve", lambda e, j=j, tsl=tsl, eb_=eb_: e.scalar_tensor_tensor(out=QK[:, j, tsl], in0=QK[:, j, tsl], scalar=1.0 / 16, in1=eb_,
                                                                                    op0=ALU.mult, op1=ALU.mult),
                     reads=[tb[2][k]] + tiles_b, writes=tiles_b)
                S.op("pool", lambda e, j=j, tsl=tsl, df_=df_: e.tensor_tensor(out=KS[:, j, tsl], in0=QK[:, 8 + j, tsl], in1=df_, op=ALU.mult),
                     reads=[tb[4][k]] + tiles_b, writes=ksb[t0:t1])
                S.op("pool", lambda e, j=j, tsl=tsl, ebi_=ebi_: e.tensor_tensor(out=QK[:, 8 + j, tsl], in0=QK[:, 8 + j, tsl], in1=ebi_, op=ALU.mult),
                     reads=[tb[3][k]] + tiles_b + ksb[t0:t1], writes=tiles_b)
        S.barrier()
        base3 = self.sb_off
        S32 = self.sb(8 * 512, F32, [8, 512])
        Sbf = self.sb(8 * 256, BF16, [8, 512])
        s32b = [self.buf("S32_%d" % j) for j in range(8)]
        sbfb = [self.buf("Sbf_%d" % j) for j in range(8)]
        MK = self.sb(256, F32, [4, 64])
        NG = self.sb(D)
        bmk = self.buf("glaMK")
        S.op("pool", lambda e: e.memset(S32, 0.0), writes=s32b)
        S.op("pool", lambda e: e.memset(Sbf, 0.0), writes=sbfb)
        S.op("pool", lambda e: e.memset(MK, 1.0), writes=[bmk])
        S.op("pool", lambda e: e.affine_select(out=MK, in_=MK, pattern=[[0, 4], [1, 64]], base=0, channel_multiplier=-1,
                                               compare_op=ALU.is_ge, fill=0.0), reads=[bmk], writes=[bmk])
        S.op("sp", lambda e: e.dma_start(out=NG, in_=self.g_ng[li:li + 1, :].partition_broadcast(128)), writes=[bmk], dma=True)
        Vc = [self.sb(1024, BF16) for _ in range(2)]
        Rc = [self.sb(1024, BF16) for _ in range(2)]
        vcb = [self.buf("glaVc%d" % j) for j in range(2)]
        rcb = [self.buf("glaRc%d" % j) for j in range(2)]
        KSc = [self.sb(512, BF16, [8, 128]) for _ in range(2)]
        kscb = [self.buf("glaKSc%d" % j) for j in range(2)]
        ATT = [self.sb(128, BF16, [4, 64]) for _ in range(2)]
        attb = [self.buf("glaATT%d" % j) for j in range(2)]
        YF = [self.sb(512) for _ in range(2)]
        yfb = [self.buf("glaYF%d" % j) for j in range(2)]
        YB = [self.sb(1024, BF16) for _ in range(2)]
        ybb = [self.buf("glaYB%d" % j) for j in range(2)]
        ST = [self.sb(6) for _ in range(4)]
        MV = [self.sb(2) for _ in range(4)]
        RS = [self.sb(1) for _ in range(4)]
        stb = [self.buf("glaST%d" % j) for j in range(4)]
        order = [(16, 0, 16)] + [(t, hf, 64) for t in range(16) for hf in range(2)]
        hctr = 0
        for n_, (tile, hf, C) in enumerate(order):
            r0 = tile * 128 + hf * 64
            ts = slice(r0, r0 + C)
            cidx = 32 if tile == 16 else tile * 2 + hf
            b2 = n_ % 2
            vc, rc, ksc, att, yb = Vc[b2], Rc[b2], KSc[b2], ATT[b2], YB[b2]
            S.op("sp", lambda e, vc=vc, ts=ts, C=C: e.dma_start(out=vc[0:C, :], in_=self.Vd[ts, :]), reads=[vdb], writes=[vcb[b2]], dma=True)
            S.op("sp", lambda e, rc=rc, ts=ts, C=C: e.dma_start(out=rc[0:C, :], in_=self.Rd[ts, :]), reads=[rdb], writes=[rcb[b2]], dma=True)
            bi = self.nextbank()
            pk = self.PS[:, bi, :].bitcast(BF16).rearrange("p (a b) -> p a b", a=8)

            def trk(e, pk=pk, ts=ts, C=C):
                for j in range(8):
                    ins = e.transpose(out=pk[0:C, j, :], in_=KS[:, j, ts], identity=self.identb)
                return ins
            S.op("pe", trk, reads=[ksb[tile], self.buf("identb")], writes=[self.pbank[bi]])
            S.op("act", lambda e, ksc=ksc, pk=pk, C=C: e.activation(out=ksc[0:C], in_=pk[0:C], func=AF.Copy), reads=[self.pbank[bi]], writes=[kscb[b2]])
            bi = self.nextbank()
            pa = self.PS[:, bi, 0:256].rearrange("p (h c) -> p h c", h=4)

            def mma(e, pa=pa, ts=ts, C=C):
                for h in range(4):
                    for dc in range(2):
                        ins = e.matmul(pa[0:C, h, 0:C], QK[:, 8 + h * 2 + dc, ts], QK[:, h * 2 + dc, ts], start=(dc == 0), stop=(dc == 1))
                return ins
            S.op("pe", mma, reads=[qkb[tile]], writes=[self.pbank[bi]])
            S.op("dve", lambda e, att=att, pa=pa, C=C: e.tensor_tensor(out=att[0:C, :, 0:C], in0=pa[0:C, :, 0:C], in1=MK[0:C, :, 0:C], op=ALU.mult),
                 reads=[self.pbank[bi], bmk], writes=[attb[b2]])
            for h in range(4):
                bo = self.nextbank()
                po = self.PS[:, bo, :]

                def mmo(e, po=po, h=h, ts=ts, C=C, att=att, vc=vc):
                    e.matmul(po[0:C, :], att[0:C, h, 0:C], vc[0:C, h * 512:(h + 1) * 512], start=True, stop=False)
                    for dc in range(2):
                        ins = e.matmul(po[0:C, :], QK[:, h * 2 + dc, ts], Sbf[:, h * 2 + dc, :], start=False, stop=(dc == 1))
                    return ins
                S.op("pe", mmo, reads=[attb[b2], vcb[b2], qkb[tile], sbfb[h * 2], sbfb[h * 2 + 1]], writes=[self.pbank[bo]])
                for dc in range(2):
                    j = h * 2 + dc
                    bs_ = self.nextbank()
                    pss = self.PS[:, bs_, :]
                    S.op("pe", lambda e, pss=pss, ksc=ksc, j=j, h=h, C=C, vc=vc: e.matmul(pss, ksc[0:C, j, :], vc[0:C, h * 512:(h + 1) * 512], start=True, stop=True),
                         reads=[kscb[b2], vcb[b2]], writes=[self.pbank[bs_]])
                    S.op("dve", lambda e, pss=pss, j=j, cidx=cidx: e.scalar_tensor_tensor(out=S32[:, j, :], in0=S32[:, j, :], scalar=DEC[:, j, cidx:cidx + 1],
                                                                                          in1=pss, op0=ALU.mult, op1=ALU.add),
                         reads=[self.pbank[bs_], decb, s32b[j]], writes=[s32b[j]])
                    if dc == 0:
                        S.op("act", lambda e, j=j: e.activation(out=Sbf[:, j, :], in_=S32[:, j, :], func=AF.Copy), reads=[s32b[j]], writes=[sbfb[j]])
                    else:
                        S.op("pool", lambda e, j=j: e.tensor_copy(Sbf[:, j, :], S32[:, j, :]), reads=[s32b[j]], writes=[sbfb[j]])
                k4 = hctr % 4
                k2 = hctr % 2
                hctr += 1
                st, mv, rs, yf = ST[k4], MV[k4], RS[k4], YF[k2]
                S.op("dve", lambda e, st=st, po=po, C=C: e.bn_stats(out=st[0:C, :], in_=po[0:C, :]), reads=[self.pbank[bo]], writes=[stb[k4]])
                S.op("dve", lambda e, st=st, mv=mv, C=C: e.bn_aggr(out=mv[0:C, :], in_=st[0:C, :]), reads=[stb[k4]], writes=[stb[k4]])
                S.op("act", lambda e, mv=mv, rs=rs, C=C: e.activation(out=rs[0:C, :], in_=mv[0:C, 1:2], func=AF.Sqrt, bias=self.eps_ap[0:C, :], scale=1.0),
                     reads=[stb[k4], self.buf("eps")], writes=[stb[k4]])
                S.op("dve", lambda e, rs=rs, C=C: e.reciprocal(out=rs[0:C, :], in_=rs[0:C, :]), reads=[stb[k4]], writes=[stb[k4]])
                S.op("dve", lambda e, yf=yf, po=po, mv=mv, rs=rs, C=C: e.tensor_scalar(out=yf[0:C, :], in0=po[0:C, :], scalar1=mv[0:C, 0:1], scalar2=rs[0:C, :],
                                                                                       op0=ALU.subtract, op1=ALU.mult),
                     reads=[self.pbank[bo], stb[k4]], writes=[yfb[k2]])
                S.op("pool", lambda e, yf=yf, h=h, C=C: e.tensor_tensor(out=yf[0:C, :], in0=yf[0:C, :], in1=NG[0:C, h * 512:(h + 1) * 512], op=ALU.mult),
                     reads=[yfb[k2], bmk], writes=[yfb[k2]])
                S.op("pool", lambda e, yf=yf, yb=yb, rc=rc, h=h, C=C: e.tensor_tensor(out=yb[0:C, h * 512:(h + 1) * 512], in0=yf[0:C, :],
                                                                                     in1=rc[0:C, h * 512:(h + 1) * 512], op=ALU.mult),
                     reads=[yfb[k2], rcb[b2]], writes=[ybb[b2]])
            bi = self.nextbank()
            py = self.PS[:, bi, :].bitcast(BF16).rearrange("p (a b) -> p a b", a=16)

            def try_(e, py=py, yb=yb, C=C):
                for kc in range(KC):
                    ins = e.transpose(out=py[:, kc, 0:C], in_=yb[0:C, kc * 128:(kc + 1) * 128], identity=self.identb[0:C, 0:C])
                return ins
            S.op("pe", try_, reads=[ybb[b2], self.buf("identb")], writes=[self.pbank[bi]])
            S.op("act", lambda e, py=py, ts=ts, C=C: e.activation(out=QK[:, :, ts], in_=py[:, :, 0:C], func=AF.Copy),
                 reads=[self.pbank[bi]], writes=[qkb[tile]])
        S.op("pool", lambda e: e.memset(QK[:, :, 2048 + NMETA:TP], 0.0), writes=[qkb[16]])
        self.outproj(QK, qkb, wo, base1)


def t5_bucket_np(dist):
    d = np.maximum(dist, 0)
    df = np.maximum(d, 1).astype(np.float32)
    large = 16 + (np.log(df / np.float32(16)) / np.float32(np.log(128 / 16)) * np.float32(16)).astype(np.int32)
    large = np.minimum(large, 31)
    return np.where(d < 16, d, large)


def bias_onehots():
    out = {}
    j = np.arange(128)

    def mk(dist, valid):
        nq, ns = dist.shape
        b = t5_bucket_np(dist)
        oh = np.zeros((33, nq, ns), np.float32)
        qq, ss = np.meshgrid(np.arange(nq), np.arange(ns), indexing="ij")
        oh[np.where(valid, b, 32), qq, ss] = 1.0
        return oh.reshape(33, nq * ns)
    d = j[:, None] - j[None, :]
    out["oh_cur"] = mk(d, d >= 0)
    d = 128 + j[:, None] - j[None, :]
    out["oh_prev"] = mk(d, d < 128)
    m = np.arange(16)
    d = NMETA + j[:, None] - m[None, :]
    out["oh_meta0"] = mk(d, np.ones_like(d, bool))
    d = NMETA + 128 + j[:, None] - m[None, :]
    out["oh_metac"] = mk(d, np.ones_like(d, bool))
    d = m[:, None] - m[None, :]
    out["oh_mm"] = mk(d, d >= 0)
    return out


def build_program(cfg):
    b = Builder(cfg)
    nc = b.build()
    return nc, b


FULL_PHASES = [
    ("init",), ("ln", 0, 0, "plain"),
    ("gla", 0), ("ln", 0, 0, "ln"), ("ffn", 0), ("ln", 0, 1, "ln"),
    ("gla", 1), ("ln", 1, 0, "ln"), ("moe", 0), ("ln", 1, 1, "ln"),
    ("kv",), ("bias",),
    ("swa", 0), ("ln", 2, 0, "ln"), ("ffn", 1), ("ln", 2, 1, "ln"),
    ("swa", 1), ("ln", 3, 0, "ln"), ("moe", 1), ("ln", 3, 1, "final"),
]

_WEIGHT_KEYS = ["meta_tokens", "rel_bias_table", "ln_gain", "ln_bias", "gla_w_in", "gla_w_gate2", "gla_b_gate",
                "gla_norm_gain", "gla_w_out", "kv_w_shared", "swa_w_q", "swa_sinks", "swa_w_out",
                "ffn_w_gate_up", "ffn_w_down", "moe_w_router", "moe_w_gate_up", "moe_w_down"]


def kernel(**inputs):
    x = np.asarray(inputs["x"], dtype=np.float32)
    nb = x.shape[0]
    nc, _ = build_program({"phases": FULL_PHASES})
    shared = {k: np.ascontiguousarray(np.asarray(inputs[k], dtype=np.float32)) for k in _WEIGHT_KEYS}
    shared.update(bias_onehots())
    in_maps = []
    for b in range(nb):
        m = dict(shared)
        m["x"] = np.ascontiguousarray(x[b])
        in_maps.append(m)
    res = run_bass_kernel_spmd(nc, in_maps, core_ids=list(range(nb)))
    return np.stack([np.asarray(r["out"], dtype=np.float32) for r in res.results], axis=0)
```

```python
import contextlib
import numpy as np
import concourse.bass as bass
import concourse.mybir as mybir
from concourse.bass_utils import run_bass_kernel_spmd

F32 = mybir.dt.float32
BF16 = mybir.dt.bfloat16
AF = mybir.ActivationFunctionType
ALU = mybir.AluOpType

D = 2048
NT = 17
TP = NT * 128
NMETA = 16
ALPHA = float(8 ** 0.25)
EPS = 1e-5
FF = 7168
NE = 8
CAPT = 5
CAP = CAPT * 128
KC = 16


class Buf:
    __slots__ = ("name", "w", "r")

    def __init__(self, name):
        self.name = name
        self.w = None
        self.r = []


class Op:
    __slots__ = ("eng", "fn", "deps", "needed", "sem", "val", "inc", "dma")


class Sched:
    ENG = ("pe", "act", "dve", "pool", "sp")

    def __init__(self, nc, es, n_dma_sems=20):
        self.nc = nc
        self.streams = {e: [] for e in self.ENG}
        self.csem = {e: es.enter_context(nc.semaphore("c_" + e)) for e in ("pe", "act", "dve", "pool")}
        self.dsem = {"sp": [es.enter_context(nc.semaphore("d_sp%d" % i)) for i in range(n_dma_sems)],
                     "act": [es.enter_context(nc.semaphore("d_act%d" % i)) for i in range(8)]}
        self.dctr = {"sp": 0, "act": 0}
        self.dlast = {}
        self.last = {e: None for e in self.ENG}

    def op(self, eng, fn, reads=(), writes=(), dma=False):
        o = Op()
        o.eng, o.fn, o.dma, o.needed = eng, fn, dma, False
        o.sem = None
        o.val = 0
        o.inc = 0
        deps = []
        for b in reads:
            if b.w is not None:
                deps.append(b.w)
        for b in writes:
            if b.w is not None:
                deps.append(b.w)
            deps.extend(b.r)
        if dma:
            pool = self.dsem[eng]
            i = self.dctr[eng] % len(pool)
            self.dctr[eng] += 1
            o.sem = pool[i]
            prev = self.dlast.get((eng, i))
            if prev is not None:
                deps.append(prev)
            self.dlast[(eng, i)] = o
            o.needed = True
        seen = set()
        od = []
        for d in deps:
            if id(d) in seen or d is o:
                continue
            seen.add(id(d))
            if d.eng == "pe" and eng == "pe" and not d.dma and not dma:
                continue
            od.append(d)
        o.deps = od
        for d in od:
            d.needed = True
        for b in reads:
            if not dma:
                b.r = [x for x in b.r if x.dma or x.eng != eng]
            b.r.append(o)
        for b in writes:
            b.w = o
            b.r = []
        self.streams[eng].append(o)
        self.last[eng] = o
        return o

    def barrier(self):
        tails = [o for o in self.last.values() if o is not None]
        tails += [o for o in self.dlast.values()]
        bb = Buf("barrier")
        for e in self.ENG:
            o = Op()
            o.eng, o.fn, o.dma, o.needed = e, None, False, False
            o.sem, o.val, o.inc = None, 0, 0
            o.deps = [t for t in tails if not (t.eng == e and not t.dma and e == "pe")]
            for d in o.deps:
                d.needed = True
            self.streams[e].append(o)

    def assign(self):
        for e, ops in self.streams.items():
            c = 0
            dc = {}
            for o in ops:
                if o.fn is None:
                    continue
                if o.dma:
                    k = id(o.sem)
                    dc[k] = dc.get(k, 0) + 16
                    o.val, o.inc = dc[k], 16
                elif o.needed:
                    c += 1
                    o.val, o.inc, o.sem = c, 1, self.csem[e]

    def emit(self, name, e):
        waited = {}
        for o in self.streams[name]:
            for d in o.deps:
                k = id(d.sem)
                if waited.get(k, 0) < d.val:
                    e.wait_ge(d.sem, d.val)
                    waited[k] = d.val
            if o.fn is None:
                continue
            ins = o.fn(e)
            if o.needed:
                ins.then_inc(o.sem, o.inc)


class Builder:
    def __init__(self, cfg):
        self.cfg = cfg
        self.nc = bass.Bass("TRN2", target_bir_lowering=False)
        self.es = contextlib.ExitStack()
        self.bufs = {}

    def buf(self, name):
        b = self.bufs.get(name)
        if b is None:
            b = self.bufs[name] = Buf(name)
        return b

    def din(self, name, shape, dt=F32):
        return self.nc.dram_tensor(name, list(shape), dt, kind="ExternalInput").ap()

    def dout(self, name, shape, dt=F32):
        return self.nc.dram_tensor(name, list(shape), dt, kind="ExternalOutput").ap()

    def dscr(self, name, shape, dt=F32):
        kind = "ExternalOutput" if name in self.cfg.get("expose", ()) else "Internal"
        return self.nc.dram_tensor(name, list(shape), dt, kind=kind).ap()

    def sb_reset(self, base=None):
        self.sb_off = self.sb_persist if base is None else base

    def sb(self, words, dt=F32, shape=None):
        words = int(words)
        a = self.sb_off
        self.sb_off += words
        assert self.sb_off <= self.SBW, ("SBUF overflow", self.sb_off, self.SBW)
        v = self.SB[:, a:a + words]
        if dt != F32:
            v = v.bitcast(dt)
        if shape is not None:
            names = " ".join("d%d" % i for i in range(len(shape)))
            kw = {"d%d" % i: int(s) for i, s in enumerate(shape)}
            v = v.rearrange("p (%s) -> p %s" % (names, names), **kw)
        return v

    def build(self):
        nc, es, cfg = self.nc, self.es, self.cfg
        self.SBW = cfg.get("sbw", 53000)
        self.SB = es.enter_context(nc.sbuf_tensor("SB", [128, self.SBW], F32))
        self.PS = es.enter_context(nc.psum_tensor("PS", [128, 8, 512], F32))
        self.S = Sched(nc, es)
        self.pbank = [self.buf("psum%d" % i) for i in range(8)]
        S = self.S

        self.x = self.din("x", [2048, D])
        self.meta = self.din("meta_tokens", [NMETA, D])
        self.ln_gain = self.din("ln_gain", [4, 2, D])
        self.ln_bias = self.din("ln_bias", [4, 2, D])
        self.out = self.dout("out", [2048, D])
        self.hres = self.dscr("hres", [TP, D])
        self.hT = self.dscr("hT", [NT, 128, KC * 128], BF16)
        self.fd = self.dscr("fd", [TP, D])
        need = cfg["phases"]
        if any(p[0] == "ffn" for p in need):
            self.w_gu = self.din("ffn_w_gate_up", [2, D, 2 * FF])
            self.w_dn = self.din("ffn_w_down", [2, FF, D])
        if any(p[0] == "gla" for p in need):
            self.g_win = self.din("gla_w_in", [2, D, 6160])
            self.g_wg2 = self.din("gla_w_gate2", [2, 16, 1024])
            self.g_bg = self.din("gla_b_gate", [2, 1024])
            self.g_ng = self.din("gla_norm_gain", [2, D])
            self.g_wout = self.din("gla_w_out", [2, D, D])
        if any(p[0] == "swa" for p in need):
            self.w_q = self.din("swa_w_q", [2, D, D])
            self.w_o = self.din("swa_w_out", [2, D, D])
            self.sinks = self.din("swa_sinks", [2, 32])
        if any(p[0] == "moe" for p in need):
            self.m_rt = self.din("moe_w_router", [2, D, NE])
            self.m_gu = self.din("moe_w_gate_up", [2, NE, D, 2 * FF])
            self.m_dn = self.din("moe_w_down", [2, NE, FF, D])
            self.XS = self.dscr("XS", [NE, CAPT, 128, KC * 128], BF16)
            self.FA = self.dscr("FA", [NE * CAP, D], BF16)
            self.SELT = self.dscr("SELT", [NT, 128, NE * CAPT * 128], BF16)

        self.sb_off = 0
        self.identb = self.sb(64, BF16, [128])
        self.identf = self.sb(128, F32, [128])
        self.gs = self.sb(NE * CAPT, F32, [NE * CAPT])
        self.eps_ap = self.sb(1)
        self.sb_persist = self.sb_off
        cb = self.buf("consts")

        S.op("pool", lambda e: e.memset(self.identf, 0.0), writes=[cb])
        S.op("pool", lambda e: e.affine_select(out=self.identf, in_=self.identf, pattern=[[-1, 128]], base=0,
                                               channel_multiplier=1, compare_op=ALU.not_equal, fill=1.0),
             reads=[cb], writes=[cb])
        S.op("pool", lambda e: e.memset(self.eps_ap, EPS), writes=[self.buf("eps")])
        S.op("pool", lambda e: e.tensor_copy(self.identb, self.identf), reads=[cb], writes=[self.buf("identb")])

        for ph in need:
            S.barrier()
            self.sb_reset()
            getattr(self, "ph_" + ph[0])(*ph[1:])
        S.barrier()

        S.assign()
        with nc.Block() as block:
            @block.tensor
            def _(e):
                S.emit("pe", e)

            @block.scalar
            def _(e):
                S.emit("act", e)

            @block.vector
            def _(e):
                S.emit("dve", e)

            @block.gpsimd
            def _(e):
                S.emit("pool", e)

            @block.sync
            def _(e):
                S.emit("sp", e)
        return nc

    def ph_init(self):
        S = self.S
        hb = [self.buf("hres%d" % i) for i in range(NT)]
        for i in range(16):
            S.op("sp", lambda e, i=i: e.dma_start(out=self.hres[i * 128:(i + 1) * 128, :],
                                                   in_=self.x[i * 128:(i + 1) * 128, :]),
                 writes=[hb[i]], dma=True)
        z = self.sb(D, F32)
        zb = self.buf("ztile")
        S.op("pool", lambda e: e.memset(z, 0.0), writes=[zb])
        S.op("sp", lambda e: e.dma_start(out=z[0:NMETA, :], in_=self.meta[:, :]), reads=[zb], writes=[zb], dma=True)
        S.op("sp", lambda e: e.dma_start(out=self.hres[2048:2176, :], in_=z), reads=[zb], writes=[hb[16]], dma=True)

    def ph_ln(self, li, which, mode):
        S = self.S
        NB = 2
        A = [self.sb(D) for _ in range(NB)]
        Fq = [self.sb(D) for _ in range(NB)]
        Yb = [self.sb(D // 2, BF16) for _ in range(NB)]
        HT = [self.sb(KC * 64, BF16, [KC, 128]) for _ in range(NB)]
        Gb = self.sb(D)
        Bb = self.sb(D)
        st = [self.sb(24, F32, [4, 6]) for _ in range(NB)]
        mv = [self.sb(2) for _ in range(NB)]
        sd = [self.sb(1) for _ in range(NB)]
        rs = [self.sb(1) for _ in range(NB)]
        bA = [self.buf("lnA%d" % j) for j in range(NB)]
        bF = [self.buf("lnF%d" % j) for j in range(NB)]
        bY = [self.buf("lnY%d" % j) for j in range(NB)]
        bH = [self.buf("lnH%d" % j) for j in range(NB)]
        bs = [self.buf("lnS%d" % j) for j in range(NB)]
        bg = self.buf("lnG")
        if mode != "plain":
            S.op("sp", lambda e: e.dma_start(out=Gb, in_=self.ln_gain[li, which:which + 1, :].partition_broadcast(128)),
                 writes=[bg], dma=True)
            S.op("sp", lambda e: e.dma_start(out=Bb, in_=self.ln_bias[li, which:which + 1, :].partition_broadcast(128)),
                 writes=[bg], dma=True)
        hb = [self.buf("hres%d" % i) for i in range(NT)]
        htb = [self.buf("hT%d" % i) for i in range(NT)]
        fb = [self.buf("fd%d" % i) for i in range(NT)]
        for i in range(NT):
            j = i % NB
            a, f, yb, ht = A[j], Fq[j], Yb[j], HT[j]
            rows = slice(i * 128, (i + 1) * 128)
            S.op("sp", lambda e, a=a, rows=rows: e.dma_start(out=a, in_=self.hres[rows, :]),
                 reads=[hb[i]], writes=[bA[j]], dma=True)
            if mode != "plain":
                S.op("sp", lambda e, f=f, rows=rows: e.dma_start(out=f, in_=self.fd[rows, :]),
                     reads=[fb[i]], writes=[bF[j]], dma=True)
                S.op("dve", lambda e, a=a, f=f: e.scalar_tensor_tensor(out=a, in0=a, scalar=ALPHA, in1=f,
                                                                       op0=ALU.mult, op1=ALU.add),
                     reads=[bF[j], bA[j]], writes=[bA[j]])
                for q in range(4):
                    S.op("dve", lambda e, a=a, q=q, s=st[j]: e.bn_stats(out=s[:, q, :], in_=a[:, q * 512:(q + 1) * 512]),
                         reads=[bA[j]], writes=[bs[j]])
                S.op("dve", lambda e, s=st[j], m=mv[j]: e.bn_aggr(out=m, in_=s), reads=[bs[j]], writes=[bs[j]])
                S.op("act", lambda e, m=mv[j], d=sd[j]: e.activation(out=d, in_=m[:, 1:2], func=AF.Sqrt, bias=self.eps_ap, scale=1.0),
                     reads=[bs[j], self.buf("eps")], writes=[bs[j]])
                S.op("dve", lambda e, d=sd[j], r=rs[j]: e.reciprocal(out=r, in_=d), reads=[bs[j]], writes=[bs[j]])
                S.op("dve", lambda e, m=mv[j], r=rs[j], d=sd[j]: e.scalar_tensor_tensor(out=d, in0=m[:, 0:1], scalar=-1.0, in1=r,
                                                                                         op0=ALU.mult, op1=ALU.mult),
                     reads=[bs[j]], writes=[bs[j]])
                S.op("act", lambda e, a=a, r=rs[j], d=sd[j]: e.activation(out=a, in_=a, func=AF.Identity, bias=d, scale=r),
                     reads=[bs[j], bA[j]], writes=[bA[j]])
                S.op("dve", lambda e, a=a: e.tensor_tensor(out=a, in0=a, in1=Gb, op=ALU.mult),
                     reads=[bA[j], bg], writes=[bA[j]])
                S.op("dve", lambda e, a=a: e.tensor_tensor(out=a, in0=a, in1=Bb, op=ALU.add),
                     reads=[bA[j], bg], writes=[bA[j]])
                if mode == "final":
                    if i < 16:
                        S.op("sp", lambda e, a=a, rows=rows: e.dma_start(out=self.out[rows, :], in_=a),
                             reads=[bA[j]], writes=[self.buf("out%d" % i)], dma=True)
                    continue
                S.op("sp", lambda e, a=a, rows=rows: e.dma_start(out=self.hres[rows, :], in_=a),
                     reads=[bA[j]], writes=[hb[i]], dma=True)
            S.op("act", lambda e, a=a, yb=yb: e.activation(out=yb, in_=a, func=AF.Copy), reads=[bA[j]], writes=[bY[j]])
            for half in range(2):
                bank = self.pbank[(2 * i + half) % 8]
                pv = self.PS[:, (2 * i + half) % 8, :].bitcast(BF16).rearrange("p (a b) -> p a b", a=8)

                def tr(e, yb=yb, pv=pv, half=half):
                    for q in range(8):
                        kc = half * 8 + q
                        ins = e.transpose(out=pv[:, q, :], in_=yb[:, kc * 128:(kc + 1) * 128], identity=self.identb)
                    return ins
                S.op("pe", tr, reads=[bY[j], self.buf("identb")], writes=[bank])
                eng = "dve" if half == 0 else "act"
                if eng == "dve":
                    S.op("dve", lambda e, ht=ht, pv=pv, half=half: e.tensor_copy(ht[:, half * 8:(half + 1) * 8, :], pv),
                         reads=[bank], writes=[bH[j]])
                else:
                    S.op("act", lambda e, ht=ht, pv=pv, half=half: e.activation(out=ht[:, half * 8:(half + 1) * 8, :], in_=pv, func=AF.Copy),
                         reads=[bank], writes=[bH[j]])
            S.op("sp", lambda e, ht=ht, i=i: e.dma_start(out=self.hT[i], in_=ht.rearrange("p a b -> p (a b)")),
                 reads=[bH[j]], writes=[htb[i]], dma=True)

    def ffn_pass(self, XT, xbs, nt, wgu, wdn, sink, R):
        S = self.S
        GT, gtb = R["GT"], R["gtb"]
        groups = [(0, min(3, nt))] + ([(3, nt)] if nt > 3 else [])
        wguv = wgu.rearrange("(kc p) f -> p kc f", p=128)
        wdnv = wdn.rearrange("(hc p) f -> p hc f", p=128)
        ctr = R["ctr"]
        tiles = []
        for hp in range(FF // 256):
            tiles.append((wguv[:, :, hp * 256:(hp + 1) * 256], (KC, 256)))
            tiles.append((wguv[:, :, FF + hp * 256:FF + (hp + 1) * 256], (KC, 256)))
        for fp in range(D // 256):
            for q in range(4):
                tiles.append((wdnv[:, q * 14:(q + 1) * 14, fp * 256:(fp + 1) * 256], (14, 256)))
        issued = [0]
        handles = {}

        def load_cast(j):
            src_ap, shape3 = tiles[j]
            k = ctr[0] % 2
            ctr[0] += 1
            stg = R["stg"][k][:, 0:shape3[0] * shape3[1]].rearrange("p (a b) -> p a b", a=shape3[0])
            sbf = R["stgb"][k]
            k2 = ctr[1] % 3
            ctr[1] += 1
            wb = R["wbf"][k2][:, 0:shape3[0] * shape3[1]].rearrange("p (a b) -> p a b", a=shape3[0])
            wbb = R["wbfb"][k2]
            S.op("sp", lambda e: e.dma_start(out=stg, in_=src_ap), writes=[sbf], dma=True)
            if j % 2 == 0:
                S.op("act", lambda e: e.activation(out=wb, in_=stg, func=AF.Copy), reads=[sbf], writes=[wbb])
            else:
                S.op("dve", lambda e: e.tensor_copy(wb, stg), reads=[sbf], writes=[wbb])
            handles[j] = (wb, wbb)

        def get(j):
            while issued[0] <= min(j + 2, len(tiles) - 1):
                load_cast(issued[0])
                issued[0] += 1
            return handles.pop(j)

        def mm(e, w, p, t0, t1, h2):
            for kc in range(KC):
                ins = e.matmul(p, w[:, kc, h2 * 128:(h2 + 1) * 128], XT[:, t0:t1, kc, :],
                               start=(kc == 0), stop=(kc == KC - 1))
            return ins
        for hp in range(FF // 256):
            for part in range(2):
                w, wbb = get(hp * 2 + part)
                for h2 in range(2):
                    for gi, (t0, t1) in enumerate(groups):
                        n = (t1 - t0) * 128
                        bi = part * 4 + h2 * 2 + gi
                        p = self.PS[:, bi, 0:n]
                        S.op("pe", lambda e, w=w, p=p, t0=t0, t1=t1, h2=h2: mm(e, w, p, t0, t1, h2),
                             reads=[wbb] + xbs[t0:t1], writes=[self.pbank[bi]])
            for h2 in range(2):
                hc = hp * 2 + h2
                for gi, (t0, t1) in enumerate(groups):
                    n = (t1 - t0) * 128
                    bG = h2 * 2 + gi
                    bU = 4 + bG
                    pG = self.PS[:, bG, 0:n]
                    pU = self.PS[:, bU, 0:n]
                    k = ctr[2] % 2
                    ctr[2] += 1
                    sg = R["sil"][k][:, 0:n]
                    sgb = R["silb"][k]
                    S.op("act", lambda e, sg=sg, pG=pG: e.activation(out=sg, in_=pG, func=AF.Silu),
                         reads=[self.pbank[bG]], writes=[sgb])
                    S.op("dve", lambda e, sg=sg, pU=pU, hc=hc, t0=t0, t1=t1: e.tensor_tensor(
                        out=GT[:, hc, t0 * 128:t1 * 128], in0=sg, in1=pU, op=ALU.mult),
                        reads=[sgb, self.pbank[bU]], writes=[gtb])
        base = 2 * (FF // 256)
        for fp in range(D // 256):
            for q in range(4):
                w, wb_ = get(base + fp * 4 + q)
                for f2 in range(2):
                    for gi, (t0, t1) in enumerate(groups):
                        n = (t1 - t0) * 128
                        bi = f2 * 2 + gi
                        p = self.PS[:, bi, 0:n]

                        def mmb(e, w=w, p=p, t0=t0, t1=t1, f2=f2, q=q):
                            for h in range(14):
                                ins = e.matmul(p, w[:, h, f2 * 128:(f2 + 1) * 128], GT[:, q * 14 + h, t0 * 128:t1 * 128],
                                               start=(q == 0 and h == 0), stop=(q == 3 and h == 13))
                            return ins
                        S.op("pe", mmb, reads=[wb_, gtb], writes=[self.pbank[bi]])
            k = ctr[3] % 2
            ctr[3] += 1
            FT = R["FT"][k]
            ftb = R["ftb"][k]
            for f2 in range(2):
                for gi, (t0, t1) in enumerate(groups):
                    n = (t1 - t0) * 128
                    bi = f2 * 2 + gi
                    p = self.PS[:, bi, 0:n]
                    if (f2 + gi) % 2 == 0:
                        S.op("act", lambda e, p=p, f2=f2, t0=t0, t1=t1, FT=FT: e.activation(
                            out=FT[:, f2, t0 * 128:t1 * 128], in_=p, func=AF.Copy),
                            reads=[self.pbank[bi]], writes=[ftb])
                    else:
                        S.op("dve", lambda e, p=p, f2=f2, t0=t0, t1=t1, FT=FT: e.tensor_copy(FT[:, f2, t0 * 128:t1 * 128], p),
                             reads=[self.pbank[bi]], writes=[ftb])
            for t in range(nt):
                bi = 4 + (ctr[4] % 4)
                ctr[4] += 1
                pv = self.PS[:, bi, 0:256]

                def trb(e, pv=pv, FT=FT, t=t):
                    for f2 in range(2):
                        ins = e.transpose(out=pv[:, f2 * 128:(f2 + 1) * 128], in_=FT[:, f2, t * 128:(t + 1) * 128],
                                          identity=self.identf)
                    return ins
                S.op("pe", trb, reads=[ftb, self.buf("consts")], writes=[self.pbank[bi]])
                sink(t, fp, pv, self.pbank[bi])

    def ffn_resources(self, ntmax):
        R = {}
        R["GT"] = self.sb(56 * ntmax * 64, BF16, [56, ntmax * 128])
        R["gtb"] = self.buf("GT")
        R["stg"] = [self.sb(4096) for _ in range(2)]
        R["stgb"] = [self.buf("stg%d" % i) for i in range(2)]
        R["wbf"] = [self.sb(2048, BF16) for _ in range(3)]
        R["wbfb"] = [self.buf("wbf%d" % i) for i in range(3)]
        R["sil"] = [self.sb(384) for _ in range(2)]
        R["silb"] = [self.buf("sil%d" % i) for i in range(2)]
        R["FT"] = [self.sb(2 * ntmax * 128, F32, [2, ntmax * 128]) for _ in range(2)]
        R["ftb"] = [self.buf("FT%d" % i) for i in range(2)]
        R["OT"] = [self.sb(256) for _ in range(4)]
        R["otb"] = [self.buf("OT%d" % i) for i in range(4)]
        R["ctr"] = [0] * 8
        return R

    def ph_ffn(self, wi):
        S = self.S
        passes = [(0, 6), (6, 12), (12, 17)]
        R = self.ffn_resources(6)
        XT = self.sb(6 * KC * 64, BF16, [6, KC, 128])
        xbs = [self.buf("XT%d" % t) for t in range(6)]
        htb = [self.buf("hT%d" % i) for i in range(NT)]
        fb = [self.buf("fd%d" % i) for i in range(NT)]
        for (a, b) in passes:
            nt = b - a
            for t in range(nt):
                S.op("sp", lambda e, t=t, a=a: e.dma_start(out=XT[:, t, :, :].rearrange("p a b -> p (a b)"), in_=self.hT[a + t]),
                     reads=[htb[a + t]], writes=[xbs[t]], dma=True)

            def sink(t, fp, pv, bankbuf, a=a):
                k = R["ctr"][5] % 4
                R["ctr"][5] += 1
                ot, otb = R["OT"][k], R["otb"][k]
                S.op("dve", lambda e: e.tensor_copy(ot, pv), reads=[bankbuf], writes=[otb])
                S.op("sp", lambda e: e.dma_start(out=self.fd[(a + t) * 128:(a + t + 1) * 128, fp * 256:(fp + 1) * 256], in_=ot),
                     reads=[otb], writes=[fb[a + t]], dma=True)
            self.ffn_pass(XT, xbs, nt, self.w_gu[wi], self.w_dn[wi], sink, R)


    def nextbank(self):
        c = getattr(self, "_bankctr", 0)
        self._bankctr = c + 1
        return c % 8

    def ph_moe(self, wi):
        S = self.S
        htb = [self.buf("hT%d" % i) for i in range(NT)]
        hb = [self.buf("hres%d" % i) for i in range(NT)]
        fb = [self.buf("fd%d" % i) for i in range(NT)]
        WRf = self.sb(128, F32, [KC, 8])
        WRb = self.sb(64, BF16, [KC, 8])
        LG = self.sb(136, F32, [NT, 8])
        M8 = self.sb(136, F32, [NT, 8])
        MASK = self.sb(136, F32, [NT, 8])
        MASK1 = self.sb(136, F32, [NT, 8])
        GATE = self.sb(136, F32, [NT, 8])
        MASKb = self.sb(68, BF16, [NT, 8])
        GATEb = self.sb(68, BF16, [NT, 8])
        RANK = self.sb(136, F32, [NT, 8])
        D1, E1, DEN, G1, G2, DG = [self.sb(NT) for _ in range(6)]
        VAL = self.sb(1)
        ONESf = self.sb(128)
        TRIf = self.sb(128)
        ONESb = self.sb(64, BF16)
        TRIb = self.sb(64, BF16)
        IOTA = self.sb(CAP)
        bw, bl, bm = self.buf("moeW"), self.buf("moeLG"), self.buf("moeM")
        bc = self.buf("moeC")
        S.op("sp", lambda e: e.dma_start(out=WRf, in_=self.m_rt[wi].rearrange("(kc p) e -> p kc e", p=128)), writes=[bw], dma=True)
        S.op("act", lambda e: e.activation(out=WRb, in_=WRf, func=AF.Copy), reads=[bw], writes=[bw])
        S.op("pool", lambda e: e.memset(ONESf, 1.0), writes=[bc])
        S.op("pool", lambda e: e.affine_select(out=TRIf, in_=ONESf, pattern=[[1, 128]], base=-1, channel_multiplier=-1,
                                               compare_op=ALU.is_ge, fill=0.0), reads=[bc], writes=[bc])
        S.op("pool", lambda e: e.tensor_copy(ONESb, ONESf), reads=[bc], writes=[bc])
        S.op("pool", lambda e: e.tensor_copy(TRIb, TRIf), reads=[bc], writes=[bc])
        S.op("pool", lambda e: e.iota(IOTA, pattern=[[1, CAP]], base=0, channel_multiplier=0,
                                      allow_small_or_imprecise_dtypes=True), writes=[bc])
        S.op("pool", lambda e: e.memset(VAL, 0.0), writes=[bc])
        S.op("pool", lambda e: e.memset(VAL[0:NMETA, :], 1.0), reads=[bc], writes=[bc])
        HTi = [self.sb(1024, BF16, [KC, 128]) for _ in range(2)]
        hbuf = [self.buf("moeHT%d" % j) for j in range(2)]
        for i in range(NT):
            j = i % 2
            S.op("sp", lambda e, i=i, j=j: e.dma_start(out=HTi[j].rearrange("p a b -> p (a b)"), in_=self.hT[i]),
                 reads=[htb[i]], writes=[hbuf[j]], dma=True)
            bi = self.nextbank()
            ps = self.PS[:, bi, 0:8]

            def mml(e, j=j, ps=ps):
                for kc in range(KC):
                    ins = e.matmul(ps, HTi[j][:, kc, :], WRb[:, kc, :], start=(kc == 0), stop=(kc == KC - 1))
                return ins
            S.op("pe", mml, reads=[hbuf[j], bw], writes=[self.pbank[bi]])
            S.op("act", lambda e, i=i, ps=ps: e.activation(out=LG[:, i, :], in_=ps, func=AF.Copy),
                 reads=[self.pbank[bi]], writes=[bl])
        for i in range(NT):
            S.op("dve", lambda e, i=i: e.max(out=M8[:, i, :], in_=LG[:, i, :]), reads=[bl], writes=[bm])
        for i in range(NT):
            S.op("dve", lambda e, i=i: e.tensor_scalar(out=MASK[:, i, :], in0=LG[:, i, :], scalar1=M8[:, i, 1:2], scalar2=None,
                                                        op0=ALU.is_ge), reads=[bl, bm], writes=[self.buf("moeMASK")])
            S.op("dve", lambda e, i=i: e.tensor_scalar(out=MASK1[:, i, :], in0=LG[:, i, :], scalar1=M8[:, i, 0:1], scalar2=None,
                                                        op0=ALU.is_equal), reads=[bl, bm], writes=[self.buf("moeMASK1")])
        bk, bk1, bgt = self.buf("moeMASK"), self.buf("moeMASK1"), self.buf("moeG")
        S.op("dve", lambda e: e.tensor_scalar(out=MASK[:, NT - 1, :], in0=MASK[:, NT - 1, :], scalar1=VAL, scalar2=None, op0=ALU.mult),
             reads=[bk, bc], writes=[bk])
        S.op("dve", lambda e: e.tensor_tensor(out=D1, in0=M8[:, :, 1], in1=M8[:, :, 0], op=ALU.subtract), reads=[bm], writes=[bgt])
        S.op("act", lambda e: e.activation(out=E1, in_=D1, func=AF.Exp), reads=[bgt], writes=[bgt])
        S.op("dve", lambda e: e.tensor_scalar(out=DEN, in0=E1, scalar1=1.0, scalar2=None, op0=ALU.add), reads=[bgt], writes=[bgt])
        S.op("dve", lambda e: e.reciprocal(out=G1, in_=DEN), reads=[bgt], writes=[bgt])
        S.op("dve", lambda e: e.tensor_tensor(out=G2, in0=E1, in1=G1, op=ALU.mult), reads=[bgt], writes=[bgt])
        S.op("dve", lambda e: e.tensor_tensor(out=DG, in0=G1, in1=G2, op=ALU.subtract), reads=[bgt], writes=[bgt])
        bga = self.buf("moeGATE")
        for i in range(NT):
            S.op("dve", lambda e, i=i: e.tensor_scalar(out=GATE[:, i, :], in0=MASK[:, i, :], scalar1=G2[:, i:i + 1], scalar2=None,
                                                        op0=ALU.mult), reads=[bk, bgt], writes=[bga])
            S.op("dve", lambda e, i=i: e.scalar_tensor_tensor(out=GATE[:, i, :], in0=MASK1[:, i, :], scalar=DG[:, i:i + 1],
                                                               in1=GATE[:, i, :], op0=ALU.mult, op1=ALU.add),
                 reads=[bk1, bgt, bga], writes=[bga])
        S.op("act", lambda e: e.activation(out=MASKb, in_=MASK, func=AF.Copy), reads=[bk], writes=[self.buf("moeMASKb")])
        S.op("act", lambda e: e.activation(out=GATEb, in_=GATE, func=AF.Copy), reads=[bga], writes=[self.buf("moeGATEb")])
        bkb, bgb, brk = self.buf("moeMASKb"), self.buf("moeGATEb"), self.buf("moeRANK")
        for i in range(NT):
            bi = self.nextbank()
            ps = self.PS[:, bi, 0:8]

            def mmr(e, i=i, ps=ps):
                for j in range(i):
                    e.matmul(ps, ONESb, MASKb[:, j, :], start=(j == 0), stop=False)
                return e.matmul(ps, TRIb, MASKb[:, i, :], start=(i == 0), stop=True)
            S.op("pe", mmr, reads=[bkb, bc], writes=[self.pbank[bi]])
            S.op("act", lambda e, i=i, ps=ps: e.activation(out=RANK[:, i, :], in_=ps, func=AF.Copy),
                 reads=[self.pbank[bi]], writes=[brk])
        if self.cfg.get("dbg_route"):
            dbg = self.dout("dbg_route", [128, 4 * 136])
            for n_, t_ in enumerate((LG, MASK, GATE, RANK)):
                S.op("sp", lambda e, n_=n_, t_=t_: e.dma_start(out=dbg[:, n_ * 136:(n_ + 1) * 136], in_=t_.rearrange("p a b -> p (a b)")),
                     reads=[bl, bk, bga, brk], dma=True)
        base_after_route = self.sb_off
        HTOK = self.sb(NT * 1024, BF16, [NT, D])
        btok = self.buf("HTOK")
        ld = [self.sb(D) for _ in range(2)]
        ldb = [self.buf("moeLD%d" % j) for j in range(2)]
        for i in range(NT):
            j = i % 2
            S.op("sp", lambda e, i=i, j=j: e.dma_start(out=ld[j], in_=self.hres[i * 128:(i + 1) * 128, :]),
                 reads=[hb[i]], writes=[ldb[j]], dma=True)
            if i % 2 == 0:
                S.op("act", lambda e, i=i, j=j: e.activation(out=HTOK[:, i, :], in_=ld[j], func=AF.Copy), reads=[ldb[j]], writes=[btok])
            else:
                S.op("pool", lambda e, i=i, j=j: e.tensor_copy(HTOK[:, i, :], ld[j]), reads=[ldb[j]], writes=[btok])
        SEL = [self.sb(NT * CAP // 2, BF16, [NT, CAP]) for _ in range(2)]
        selb = [self.buf("SEL%d" % j) for j in range(2)]
        XE = [self.sb(CAPT * KC * 64, BF16, [CAPT, KC, 128]) for _ in range(2)]
        xeb = [self.buf("XE%d" % j) for j in range(2)]
        STG = [self.sb(CAPT * 64, BF16, [CAPT, 128]) for _ in range(3)]
        stgb = [self.buf("STG%d" % j) for j in range(3)]
        gsb = self.buf("gs")
        groups = [(0, 3), (3, CAPT)]
        sctr = 0
        for ex in range(NE):
            j = ex % 2
            sel = SEL[j]
            for i in range(NT):
                S.op("dve", lambda e, i=i, sel=sel, ex=ex: e.tensor_scalar(
                    out=sel[:, i, :], in0=IOTA, scalar1=RANK[:, i, ex:ex + 1], scalar2=MASK[:, i, ex:ex + 1],
                    op0=ALU.is_equal, op1=ALU.mult), reads=[brk, bk, bc], writes=[selb[j]])
            xe = XE[j]
            for kc in range(KC):
                for (t0, t1) in groups:
                    n = (t1 - t0) * 128
                    bi = self.nextbank()
                    ps = self.PS[:, bi, 0:n]

                    def mmd(e, kc=kc, t0=t0, t1=t1, ps=ps, sel=sel):
                        for i in range(NT):
                            ins = e.matmul(ps, HTOK[:, i, kc * 128:(kc + 1) * 128], sel[:, i, t0 * 128:t1 * 128],
                                           start=(i == 0), stop=(i == NT - 1))
                        return ins
                    S.op("pe", mmd, reads=[btok, selb[j]], writes=[self.pbank[bi]])
                    dst = xe[:, t0:t1, kc, :]
                    psv = ps.rearrange("p (a b) -> p a b", b=128)
                    if kc % 2 == 0:
                        S.op("act", lambda e, dst=dst, psv=psv: e.activation(out=dst, in_=psv, func=AF.Copy),
                             reads=[self.pbank[bi]], writes=[xeb[j]])
                    else:
                        S.op("dve", lambda e, dst=dst, psv=psv: e.tensor_copy(dst, psv), reads=[self.pbank[bi]], writes=[xeb[j]])
            for t in range(CAPT):
                S.op("sp", lambda e, t=t, ex=ex, xe=xe: e.dma_start(out=self.XS[ex, t], in_=xe[:, t, :, :].rearrange("p a b -> p (a b)")),
                     reads=[xeb[j]], writes=[self.buf("XS%d" % ex)], dma=True)
            for sc in range(CAPT):
                bi = self.nextbank()
                ps = self.PS[:, bi, 0:1]

                def mmg(e, sc=sc, ps=ps, sel=sel, ex=ex):
                    for i in range(NT):
                        ins = e.matmul(ps, sel[:, i, sc * 128:(sc + 1) * 128], GATEb[:, i, ex:ex + 1],
                                       start=(i == 0), stop=(i == NT - 1))
                    return ins
                S.op("pe", mmg, reads=[selb[j], bgb], writes=[self.pbank[bi]])
                S.op("act", lambda e, ps=ps, ex=ex, sc=sc: e.activation(out=self.gs[:, ex * CAPT + sc:ex * CAPT + sc + 1], in_=ps, func=AF.Copy),
                     reads=[self.pbank[bi]], writes=[gsb])
            for i in range(NT):
                bi = self.nextbank()
                pv = self.PS[:, bi, 0:CAPT * 64].bitcast(BF16).rearrange("p (a b) -> p a b", a=CAPT)

                def trs(e, i=i, pv=pv, sel=sel):
                    for sc in range(CAPT):
                        ins = e.transpose(out=pv[:, sc, :], in_=sel[:, i, sc * 128:(sc + 1) * 128], identity=self.identb)
                    return ins
                S.op("pe", trs, reads=[selb[j], self.buf("identb")], writes=[self.pbank[bi]])
                k = sctr % 3
                sctr += 1
                stg = STG[k]
                if i % 2 == 0:
                    S.op("dve", lambda e, stg=stg, pv=pv: e.tensor_copy(stg, pv), reads=[self.pbank[bi]], writes=[stgb[k]])
                else:
                    S.op("act", lambda e, stg=stg, pv=pv: e.activation(out=stg, in_=pv, func=AF.Copy), reads=[self.pbank[bi]], writes=[stgb[k]])
                S.op("sp", lambda e, stg=stg, i=i, ex=ex: e.dma_start(
                    out=self.SELT[i, :, ex * CAP:(ex + 1) * CAP], in_=stg.rearrange("p a b -> p (a b)")),
                    reads=[stgb[k]], writes=[self.buf("SELT%d" % i)], dma=True)
        S.barrier()
        self.sb_reset()
        R = self.ffn_resources(CAPT)
        XT = self.sb(CAPT * KC * 64, BF16, [CAPT, KC, 128])
        xbs = [self.buf("XT%d" % t) for t in range(CAPT)]
        OTb = [self.sb(128, BF16) for _ in range(4)]
        fab = self.buf("FA")
        for ex in range(NE):
            for t in range(CAPT):
                S.op("sp", lambda e, t=t, ex=ex: e.dma_start(out=XT[:, t, :, :].rearrange("p a b -> p (a b)"), in_=self.XS[ex, t]),
                     reads=[self.buf("XS%d" % ex)], writes=[xbs[t]], dma=True)

            def sink(t, fp, pv, bankbuf, ex=ex):
                k = R["ctr"][5] % 4
                R["ctr"][5] += 1
                ot, otb = OTb[k], R["otb"][k]
                g = self.gs[:, ex * CAPT + t:ex * CAPT + t + 1]
                S.op("dve", lambda e: e.tensor_scalar(out=ot, in0=pv, scalar1=g, scalar2=None, op0=ALU.mult),
                     reads=[bankbuf, gsb], writes=[otb])
                r0 = ex * CAP + t * 128
                S.op("sp", lambda e: e.dma_start(out=self.FA[r0:r0 + 128, fp * 256:(fp + 1) * 256], in_=ot),
                     reads=[otb], writes=[fab], dma=True)
            self.ffn_pass(XT, xbs, CAPT, self.m_gu[wi, ex], self.m_dn[wi, ex], sink, R)
        S.barrier()
        self.sb_reset()
        NC_ = NE * CAPT
        FAh = self.sb(NC_ * 512, BF16, [NC_, 1024])
        fahb = self.buf("FAh")
        STi = [self.sb(NC_ * 64, BF16, [NC_, 128]) for _ in range(2)]
        stib = [self.buf("STi%d" % j) for j in range(2)]
        OC = [self.sb(1024) for _ in range(2)]
        ocb = [self.buf("OC%d" % j) for j in range(2)]
        FAv = self.FA.rearrange("(c p) f -> p c f", p=128)
        cc = 0
        for h in range(2):
            for ex in range(NE):
                S.op("sp", lambda e, ex=ex, h=h: e.dma_start(out=FAh[:, ex * CAPT:(ex + 1) * CAPT, :],
                                                              in_=FAv[:, ex * CAPT:(ex + 1) * CAPT, h * 1024:(h + 1) * 1024]),
                     reads=[fab], writes=[fahb], dma=True)
            for i in range(NT):
                j = cc % 2
                cc += 1
                S.op("sp", lambda e, i=i, j=j: e.dma_start(out=STi[j].rearrange("p a b -> p (a b)"), in_=self.SELT[i]),
                     reads=[self.buf("SELT%d" % i)], writes=[stib[j]], dma=True)
                for nch in range(2):
                    bi = self.nextbank()
                    ps = self.PS[:, bi, :]

                    def mmc(e, j=j, nch=nch, ps=ps):
                        for c in range(NC_):
                            ins = e.matmul(ps, STi[j][:, c, :], FAh[:, c, nch * 512:(nch + 1) * 512],
                                           start=(c == 0), stop=(c == NC_ - 1))
                        return ins
                    S.op("pe", mmc, reads=[stib[j], fahb], writes=[self.pbank[bi]])
                    if nch == 0:
                        S.op("act", lambda e, j=j, ps=ps: e.activation(out=OC[j][:, 0:512], in_=ps, func=AF.Copy),
                             reads=[self.pbank[bi]], writes=[ocb[j]])
                    else:
                        S.op("dve", lambda e, j=j, ps=ps: e.tensor_copy(OC[j][:, 512:1024], ps),
                             reads=[self.pbank[bi]], writes=[ocb[j]])
                S.op("sp", lambda e, i=i, j=j, h=h: e.dma_start(out=self.fd[i * 128:(i + 1) * 128, h * 1024:(h + 1) * 1024], in_=OC[j]),
                     reads=[ocb[j]], writes=[fb[i]], dma=True)


    def ph_kv(self):
        S = self.S
        htb = [self.buf("hT%d" % i) for i in range(NT)]
        wkv = self.din("kv_w_shared", [D, 512])
        self.kT2 = self.dscr("kT2", [128, 4 * TP], BF16)
        self.Vaug = self.dscr("Vaug", [NT, 128, 4 * 66], BF16)
        WKf = self.sb(KC * 512, F32, [KC, 512])
        WK2 = self.sb(KC * 4 * 64, BF16, [KC, 4, 128])
        WVb = self.sb(KC * 128, BF16, [KC, 256])
        XTall = self.sb(NT * 1024, BF16, [NT, KC, 128])
        KT = self.sb(4 * TP // 2, BF16, [4, TP])
        VA = [self.sb(4 * 33, BF16, [4, 66]) for _ in range(2)]
        bw, bw2, bx, bkt = self.buf("kvW"), self.buf("kvW2"), self.buf("kvX"), self.buf("kvKT")
        vab = [self.buf("kvVA%d" % j) for j in range(2)]
        S.op("sp", lambda e: e.dma_start(out=WKf, in_=wkv.rearrange("(kc p) f -> p kc f", p=128)), writes=[bw], dma=True)
        for g in range(4):
            for hf in range(2):
                eng = "act" if hf == 0 else "pool"
                if eng == "act":
                    S.op("act", lambda e, g=g, hf=hf: e.activation(out=WK2[:, :, g, hf * 64:(hf + 1) * 64], in_=WKf[:, :, g * 64:(g + 1) * 64], func=AF.Copy),
                         reads=[bw], writes=[bw2])
                else:
                    S.op("pool", lambda e, g=g, hf=hf: e.tensor_copy(WK2[:, :, g, hf * 64:(hf + 1) * 64], WKf[:, :, g * 64:(g + 1) * 64]),
                         reads=[bw], writes=[bw2])
        S.op("act", lambda e: e.activation(out=WVb, in_=WKf[:, :, 256:512], func=AF.Copy), reads=[bw], writes=[bw2])
        for i in range(NT):
            S.op("sp", lambda e, i=i: e.dma_start(out=XTall[:, i, :, :].rearrange("p a b -> p (a b)"), in_=self.hT[i]),
                 reads=[htb[i]], writes=[bx], dma=True)
        tg = [(0, 4), (4, 8), (8, 12), (12, 16), (16, 17)]
        for (t0, t1) in tg:
            n = (t1 - t0) * 128
            for g in range(4):
                bi = self.nextbank()
                ps = self.PS[:, bi, 0:n]

                def mmk(e, g=g, t0=t0, t1=t1, ps=ps):
                    for kc in range(KC):
                        ins = e.matmul(ps, WK2[:, kc, g, :], XTall[:, t0:t1, kc, :], start=(kc == 0), stop=(kc == KC - 1))
                    return ins
                S.op("pe", mmk, reads=[bw2, bx], writes=[self.pbank[bi]])
                if g % 2 == 0:
                    S.op("act", lambda e, g=g, t0=t0, t1=t1, ps=ps: e.activation(out=KT[:, g, t0 * 128:t1 * 128], in_=ps, func=AF.Copy),
                         reads=[self.pbank[bi]], writes=[bkt])
                else:
                    S.op("dve", lambda e, g=g, t0=t0, t1=t1, ps=ps: e.tensor_copy(KT[:, g, t0 * 128:t1 * 128], ps),
                         reads=[self.pbank[bi]], writes=[bkt])
        S.op("sp", lambda e: e.dma_start(out=self.kT2, in_=KT.rearrange("p a b -> p (a b)")), reads=[bkt], writes=[self.buf("kT2d")], dma=True)
        for i in range(NT):
            j = i % 2
            bi = self.nextbank()
            ps = self.PS[:, bi, 0:256]

            def mmv(e, i=i, ps=ps):
                for kc in range(KC):
                    ins = e.matmul(ps, XTall[:, i, kc, :], WVb[:, kc, :], start=(kc == 0), stop=(kc == KC - 1))
                return ins
            S.op("pe", mmv, reads=[bw2, bx], writes=[self.pbank[bi]])
            S.op("pool", lambda e, j=j: e.memset(VA[j][:, :, 64:66], 1.0), writes=[vab[j]])
            S.op("act", lambda e, j=j, ps=ps: e.activation(out=VA[j][:, :, 0:64], in_=ps.rearrange("p (a b) -> p a b", a=4), func=AF.Copy),
                 reads=[self.pbank[bi], vab[j]], writes=[vab[j]])
            S.op("sp", lambda e, i=i, j=j: e.dma_start(out=self.Vaug[i], in_=VA[j].rearrange("p a b -> p (a b)")),
                 reads=[vab[j]], writes=[self.buf("Vaugd")], dma=True)

    def ph_bias(self):
        S = self.S
        tab = self.din("rel_bias_table", [32, 32])
        ohs = {"cur": (128, 128), "prev": (128, 128), "meta0": (128, 16), "metac": (128, 16), "mm": (16, 16)}
        self.bias_d = {}
        T33 = self.sb(32)
        bt = self.buf("biasT")
        S.op("pool", lambda e: e.memset(T33, -30000.0), writes=[bt])
        S.op("sp", lambda e: e.dma_start(out=T33[0:32, :], in_=tab), reads=[bt], writes=[bt], dma=True)
        OH = [self.sb(128 * 128, F32, [128, 128]) for _ in range(2)]
        ohb = [self.buf("OH%d" % j) for j in range(2)]
        BT = [self.sb(32 * 128, F32, [32, 128]) for _ in range(2)]
        btb = [self.buf("BT%d" % j) for j in range(2)]
        for n_, (name, (nq, ns)) in enumerate(ohs.items()):
            j = n_ % 2
            src = self.din("oh_" + name, [33, nq * ns])
            dst = self.dscr("bias_" + name, [128, 32 * 128])
            self.bias_d[name] = dst
            oh = OH[j][:, 0:nq, 0:ns] if False else OH[j].rearrange("p a b -> p (a b)")[:, 0:nq * ns].rearrange("p (a b) -> p a b", a=nq)
            S.op("sp", lambda e, oh=oh, src=src: e.dma_start(out=oh[0:33].rearrange("p a b -> p (a b)"), in_=src), writes=[ohb[j]], dma=True)
            btile = BT[j]
            S.op("pool", lambda e, btile=btile: e.memset(btile, 0.0), writes=[btb[j]])
            for q0 in range(0, nq, 16):
                bi = self.nextbank()
                ps = self.PS[:, bi, :].rearrange("p (q h) -> p q h", q=16)

                def mmb(e, q0=q0, ps=ps, oh=oh, ns=ns):
                    for q in range(16):
                        ins = e.matmul(ps[0:ns, q, :], oh[0:33, q0 + q, :], T33[0:33, :], start=True, stop=True)
                    return ins
                S.op("pe", mmb, reads=[ohb[j], bt], writes=[self.pbank[bi]])
                S.op("dve", lambda e, q0=q0, ps=ps, btile=btile, ns=ns: e.tensor_copy(
                    btile[0:ns, :, q0:q0 + 16], ps[0:ns, :, :].rearrange("p q h -> p h q")),
                    reads=[self.pbank[bi]], writes=[btb[j]])
            S.op("sp", lambda e, dst=dst, btile=btile: e.dma_start(out=dst, in_=btile.rearrange("p a b -> p (a b)")),
                 reads=[btb[j]], writes=[self.buf("biasd")], dma=True)

    def ph_swa(self, jb):
        S = self.S
        htb = [self.buf("hT%d" % i) for i in range(NT)]
        fb = [self.buf("fd%d" % i) for i in range(NT)]
        wq = self.w_q[jb].rearrange("(kc p) f -> p kc f", p=128)
        wo = self.w_o[jb].rearrange("(kc p) f -> p kc f", p=128)
        QA = self.sb(KC * TP // 2, BF16, [KC, TP])
        qab = [self.buf("QA%d" % i) for i in range(NT)]
        base1 = self.sb_off
        XTall = self.sb(NT * 1024, BF16, [NT, KC, 128])
        bx = self.buf("swaX")
        stg = [self.sb(4096, F32, [KC, 256]) for _ in range(2)]
        stgb = [self.buf("swaStg%d" % j) for j in range(2)]
        wbf = [self.sb(2048, BF16, [KC, 256]) for _ in range(3)]
        wbfb = [self.buf("swaW%d" % j) for j in range(3)]
        for i in range(NT):
            S.op("sp", lambda e, i=i: e.dma_start(out=XTall[:, i, :, :].rearrange("p a b -> p (a b)"), in_=self.hT[i]),
                 reads=[htb[i]], writes=[bx], dma=True)
        tg = [(0, 4), (4, 8), (8, 12), (12, 16), (16, 17)]
        for cp in range(8):
            j, j2 = cp % 2, cp % 3
            S.op("sp", lambda e, cp=cp, j=j: e.dma_start(out=stg[j], in_=wq[:, :, cp * 256:(cp + 1) * 256]), writes=[stgb[j]], dma=True)
            if cp % 2 == 0:
                S.op("act", lambda e, j=j, j2=j2: e.activation(out=wbf[j2], in_=stg[j], func=AF.Copy), reads=[stgb[j]], writes=[wbfb[j2]])
            else:
                S.op("dve", lambda e, j=j, j2=j2: e.tensor_copy(wbf[j2], stg[j]), reads=[stgb[j]], writes=[wbfb[j2]])
            for c2 in range(2):
                c = cp * 2 + c2
                for (t0, t1) in tg:
                    n = (t1 - t0) * 128
                    bi = self.nextbank()
                    ps = self.PS[:, bi, 0:n]

                    def mmq(e, j2=j2, c2=c2, t0=t0, t1=t1, ps=ps):
                        for kc in range(KC):
                            ins = e.matmul(ps, wbf[j2][:, kc, c2 * 128:(c2 + 1) * 128], XTall[:, t0:t1, kc, :],
                                           start=(kc == 0), stop=(kc == KC - 1))
                        return ins
                    S.op("pe", mmq, reads=[wbfb[j2], bx], writes=[self.pbank[bi]])
                    if (t0 // 4) % 2 == 0:
                        S.op("act", lambda e, c=c, t0=t0, t1=t1, ps=ps: e.activation(out=QA[:, c, t0 * 128:t1 * 128], in_=ps, func=AF.Copy),
                             reads=[self.pbank[bi]], writes=qab[t0:t1])
                    else:
                        S.op("dve", lambda e, c=c, t0=t0, t1=t1, ps=ps: e.tensor_copy(QA[:, c, t0 * 128:t1 * 128], ps),
                             reads=[self.pbank[bi]], writes=qab[t0:t1])
        S.barrier()
        self.sb_reset(base1)
        KTh = [self.sb(4 * TP // 2, BF16, [4, TP]) for _ in range(2)]
        VA = self.sb(NT * 4 * 33, BF16, [NT, 4, 66])
        Bc = self.sb(4096, F32, [32, 128])
        Bp = self.sb(4096, F32, [32, 128])
        Bm0 = self.sb(4096, F32, [32, 128])
        Bmc = self.sb(4096, F32, [32, 128])
        Bmm = self.sb(512, F32, [32, 16])
        SK = self.sb(32)
        SKe = self.sb(32)
        bkv, bbias, bsk = self.buf("swaKV"), self.buf("swaBias"), self.buf("swaSK")
        for hf_ in range(2):
            S.op("sp", lambda e, hf_=hf_: e.dma_start(out=KTh[hf_].rearrange("p a b -> p (a b)"), in_=self.kT2),
                 reads=[self.buf("kT2d")], writes=[bkv], dma=True)
        S.op("pool", lambda e: e.memset(KTh[0][64:128], 0.0), reads=[bkv], writes=[bkv])
        S.op("pool", lambda e: e.memset(KTh[1][0:64], 0.0), reads=[bkv], writes=[bkv])
        S.op("sp", lambda e: e.dma_start(out=VA.rearrange("p t a b -> p t (a b)"), in_=self.Vaug.rearrange("t p f -> p t f")),
             reads=[self.buf("Vaugd")], writes=[bkv], dma=True)
        for t_, nm in ((Bc, "cur"), (Bp, "prev"), (Bm0, "meta0"), (Bmc, "metac")):
            S.op("sp", lambda e, t_=t_, nm=nm: e.dma_start(out=t_.rearrange("p a b -> p (a b)"), in_=self.bias_d[nm]),
                 reads=[self.buf("biasd")], writes=[bbias], dma=True)
        S.op("sp", lambda e: e.dma_start(out=Bmm, in_=self.bias_d["mm"].rearrange("p (h q) -> p h q", h=32)[:, :, 0:16]),
             reads=[self.buf("biasd")], writes=[bbias], dma=True)
        S.op("sp", lambda e: e.dma_start(out=SK, in_=self.sinks[jb:jb + 1, :].partition_broadcast(128)), writes=[bsk], dma=True)
        S.op("act", lambda e: e.activation(out=SKe, in_=SK, func=AF.Exp), reads=[bsk], writes=[bsk])
        TMP = [self.sb(512) for _ in range(3)]
        tmpb = [self.buf("swaTMP%d" % j) for j in range(3)]
        EX = [self.sb(256, BF16) for _ in range(4)]
        exb = [self.buf("swaEX%d" % j) for j in range(4)]
        DEN = [self.sb(4) for _ in range(2)]
        denb = [self.buf("swaDEN%d" % j) for j in range(2)]
        AO = [self.sb(1024, BF16) for _ in range(2)]
        aob = [self.buf("swaAO%d" % j) for j in range(2)]
        cnt = [0, 0, 0]
        blocks = list(range(16)) + [16]
        for blk in blocks:
            meta_q = (blk == 16)
            nq = NMETA if meta_q else 128
            qs = slice(blk * 128, blk * 128 + nq)
            ao = AO[blk % 2]
            aobuf = aob[blk % 2]
            if meta_q:
                S.op("pool", lambda e, ao=ao: e.memset(ao, 0.0), writes=[aobuf])
            for hg in range(8):
                g = hg // 2
                pieces = []
                if meta_q:
                    pieces.append((slice(2048, 2064), 16, Bmm, 16))
                else:
                    pieces.append((slice(blk * 128, blk * 128 + 128), blk, Bc, 128))
                    if blk > 0:
                        pieces.append((slice((blk - 1) * 128, blk * 128), blk - 1, Bp, 128))
                    pieces.append((slice(2048, 2064), 16, Bm0 if blk == 0 else Bmc, 16))
                exs = []
                for (ks, vt, btile, ns) in pieces:
                    bi = self.nextbank()
                    ps = self.PS[:, bi, :].rearrange("p (h q) -> p h q", h=4)

                    def mms(e, ks=ks, ns=ns, ps=ps, hg=hg, g=g, qs=qs, nq=nq):
                        for hh in range(4):
                            h = hg * 4 + hh
                            kc, hf = h // 2, h % 2
                            ins = e.matmul(ps[0:ns, hh, 0:nq], KTh[hf][:, g, ks], QA[:, kc, qs], start=True, stop=True)
                        return ins
                    S.op("pe", mms, reads=[bkv, qab[blk]], writes=[self.pbank[bi]])
                    k = cnt[0] % 3
                    cnt[0] += 1
                    tmp = TMP[k].rearrange("p (h q) -> p h q", h=4)
                    S.op("dve", lambda e, tmp=tmp, ps=ps, ns=ns, nq=nq, btile=btile, hg=hg: e.scalar_tensor_tensor(
                        out=tmp[0:ns, :, 0:nq], in0=ps[0:ns, :, 0:nq], scalar=0.125, in1=btile[0:ns, hg * 4:(hg + 1) * 4, 0:nq],
                        op0=ALU.mult, op1=ALU.add), reads=[self.pbank[bi], bbias], writes=[tmpb[k]])
                    k2 = cnt[1] % 4
                    cnt[1] += 1
                    ex = EX[k2].rearrange("p (h q) -> p h q", h=4)
                    S.op("act", lambda e, ex=ex, tmp=tmp, ns=ns, nq=nq: e.activation(out=ex[0:ns, :, 0:nq], in_=tmp[0:ns, :, 0:nq], func=AF.Exp),
                         reads=[tmpb[k]], writes=[exb[k2]])
                    exs.append((ex, exb[k2], vt, ns))
                bi = self.nextbank()
                po = self.PS[:, bi, :].rearrange("p (h q) -> p h q", h=4)

                def mmo(e, exs=exs, po=po, g=g, nq=nq):
                    for hh in range(4):
                        for n_, (ex, _, vt, ns) in enumerate(exs):
                            ins = e.matmul(po[0:nq, hh, 0:65], ex[0:ns, hh, 0:nq], VA[0:ns, vt, g, 0:65],
                                           start=(n_ == 0), stop=(n_ == len(exs) - 1))
                    return ins
                S.op("pe", mmo, reads=[bkv] + [x[1] for x in exs], writes=[self.pbank[bi]])
                k3 = cnt[2] % 2
                cnt[2] += 1
                den = DEN[k3]
                S.op("dve", lambda e, den=den, po=po, hg=hg, nq=nq: e.tensor_tensor(
                    out=den[0:nq, :], in0=po[0:nq, :, 64], in1=SKe[0:nq, hg * 4:(hg + 1) * 4], op=ALU.add),
                    reads=[self.pbank[bi], bsk], writes=[denb[k3]])
                S.op("dve", lambda e, den=den, nq=nq: e.reciprocal(out=den[0:nq, :], in_=den[0:nq, :]), reads=[denb[k3]], writes=[denb[k3]])
                for hh in range(4):
                    h = hg * 4 + hh
                    if hh % 2 == 0:
                        S.op("act", lambda e, ao=ao, po=po, den=den, h=h, hh=hh, nq=nq: e.activation(
                            out=ao[0:nq, h * 64:(h + 1) * 64], in_=po[0:nq, hh, 0:64], func=AF.Copy, scale=den[0:nq, hh:hh + 1]),
                            reads=[self.pbank[bi], denb[k3]], writes=[aobuf])
                    else:
                        S.op("dve", lambda e, ao=ao, po=po, den=den, h=h, hh=hh, nq=nq: e.tensor_scalar(
                            out=ao[0:nq, h * 64:(h + 1) * 64], in0=po[0:nq, hh, 0:64], scalar1=den[0:nq, hh:hh + 1], scalar2=None, op0=ALU.mult),
                            reads=[self.pbank[bi], denb[k3]], writes=[aobuf])
            for half in range(2):
                bi = self.nextbank()
                pv = self.PS[:, bi, :].bitcast(BF16).rearrange("p (a b) -> p a b", a=8)

                def tr(e, ao=ao, pv=pv, half=half):
                    for q in range(8):
                        kc = half * 8 + q
                        ins = e.transpose(out=pv[:, q, :], in_=ao[:, kc * 128:(kc + 1) * 128], identity=self.identb)
                    return ins
                S.op("pe", tr, reads=[aobuf, self.buf("identb")], writes=[self.pbank[bi]])
                dst = QA[:, half * 8:(half + 1) * 8, blk * 128:(blk + 1) * 128]
                if half == 0:
                    S.op("dve", lambda e, dst=dst, pv=pv: e.tensor_copy(dst, pv), reads=[self.pbank[bi]], writes=[qab[blk]])
                else:
                    S.op("act", lambda e, dst=dst, pv=pv: e.activation(out=dst, in_=pv, func=AF.Copy), reads=[self.pbank[bi]], writes=[qab[blk]])
        self.outproj(QA, qab, wo, base1)

    def outproj(self, QA, qab, wo, base1):
        S = self.S
        fb = [self.buf("fd%d" % i) for i in range(NT)]
        S.barrier()
        self.sb_reset(base1)
        WO = self.sb(KC * 1024, BF16, [KC, D])
        wob = self.buf("swaWO")
        sgO = [self.sb(4096, F32, [KC, 256]) for _ in range(2)]
        sgO_b = [self.buf("swaStgO%d" % j) for j in range(2)]
        OT = [self.sb(D) for _ in range(2)]
        otb = [self.buf("swaOT%d" % j) for j in range(2)]
        for cp in range(8):
            j = cp % 2
            S.op("sp", lambda e, cp=cp, j=j: e.dma_start(out=sgO[j], in_=wo[:, :, cp * 256:(cp + 1) * 256]), writes=[sgO_b[j]], dma=True)
            if cp % 2 == 0:
                S.op("dve", lambda e, j=j, cp=cp: e.tensor_copy(WO[:, :, cp * 256:(cp + 1) * 256], sgO[j]), reads=[sgO_b[j]], writes=[wob])
            else:
                S.op("act", lambda e, j=j, cp=cp: e.activation(out=WO[:, :, cp * 256:(cp + 1) * 256], in_=sgO[j], func=AF.Copy), reads=[sgO_b[j]], writes=[wob])
        for i in range(NT):
            j = i % 2
            for n in range(4):
                bi = self.nextbank()
                ps = self.PS[:, bi, :]

                def mmw(e, i=i, n=n, ps=ps):
                    for kc in range(KC):
                        ins = e.matmul(ps, QA[:, kc, i * 128:(i + 1) * 128], WO[:, kc, n * 512:(n + 1) * 512],
                                       start=(kc == 0), stop=(kc == KC - 1))
                    return ins
                S.op("pe", mmw, reads=[wob, qab[i]], writes=[self.pbank[bi]])
                if n % 2 == 0:
                    S.op("act", lambda e, j=j, n=n, ps=ps: e.activation(out=OT[j][:, n * 512:(n + 1) * 512], in_=ps, func=AF.Copy),
                         reads=[self.pbank[bi]], writes=[otb[j]])
                else:
                    S.op("dve", lambda e, j=j, n=n, ps=ps: e.tensor_copy(OT[j][:, n * 512:(n + 1) * 512], ps),
                         reads=[self.pbank[bi]], writes=[otb[j]])
            S.op("sp", lambda e, i=i, j=j: e.dma_start(out=self.fd[i * 128:(i + 1) * 128, :], in_=OT[j]),
                 reads=[otb[j]], writes=[fb[i]], dma=True)


    def ph_gla(self, li):
        S = self.S
        htb = [self.buf("hT%d" % i) for i in range(NT)]
        w_in = self.g_win[li].rearrange("(kc p) f -> p kc f", p=128)
        wo = self.g_wout[li].rearrange("(kc p) f -> p kc f", p=128)
        if not hasattr(self, "Vd"):
            self.Vd = self.dscr("Vd", [TP, D], BF16)
            self.Rd = self.dscr("Rd", [TP, D], BF16)
        QK = self.sb(KC * TP // 2, BF16, [KC, TP])
        qkb = [self.buf("QK%d" % i) for i in range(NT)]
        base1 = self.sb_off
        GL = self.sb(TP)
        glb = self.buf("GL")
        base2 = self.sb_off
        XTall = self.sb(NT * 1024, BF16, [NT, KC, 128])
        bx = self.buf("glaX")
        g1stg = [self.sb(4096, F32, [KC, 256]) for _ in range(2)]
        g1stgb = [self.buf("glaStg%d" % j) for j in range(2)]
        g1w = [self.sb(2048, BF16, [KC, 256]) for _ in range(2)]
        g1wb = [self.buf("glaW%d" % j) for j in range(2)]
        VO = [self.sb(128, BF16) for _ in range(4)]
        vob = [self.buf("glaVO%d" % j) for j in range(4)]
        RT = [self.sb(256) for _ in range(2)]
        rtb = [self.buf("glaRT%d" % j) for j in range(2)]
        NG1 = self.sb(D)
        ngb1 = self.buf("glaNG1")
        S.op("sp", lambda e: e.dma_start(out=NG1, in_=self.g_ng[li:li + 1, :].partition_broadcast(128)), writes=[ngb1], dma=True)
        for i in range(NT):
            S.op("sp", lambda e, i=i: e.dma_start(out=XTall[:, i, :, :].rearrange("p a b -> p (a b)"), in_=self.hT[i]),
                 reads=[htb[i]], writes=[bx], dma=True)
        tg = [(0, 4), (4, 8), (8, 12), (12, 16), (16, 17)]
        vctr = [0]
        vdb, rdb = self.buf("Vd"), self.buf("Rd")
        for ct in range(25):
            j, j2 = ct % 2, ct % 2
            ncol = 256 if ct < 24 else 16
            stg_v = g1stg[j][:, :, 0:ncol]
            w_v = g1w[j2][:, :, 0:ncol]
            S.op("sp", lambda e, ct=ct, stg_v=stg_v, ncol=ncol: e.dma_start(out=stg_v, in_=w_in[:, :, ct * 256:ct * 256 + ncol]),
                 writes=[g1stgb[j]], dma=True)
            if ct % 2 == 0:
                S.op("act", lambda e, w_v=w_v, stg_v=stg_v: e.activation(out=w_v, in_=stg_v, func=AF.Copy), reads=[g1stgb[j]], writes=[g1wb[j2]])
            else:
                S.op("dve", lambda e, w_v=w_v, stg_v=stg_v: e.tensor_copy(w_v, stg_v), reads=[g1stgb[j]], writes=[g1wb[j2]])
            if ct < 8 or ct == 24:
                for c2 in range(2 if ct < 24 else 1):
                    mcols = 128 if ct < 24 else 16
                    for (t0, t1) in tg:
                        n = (t1 - t0) * 128
                        bi = self.nextbank()
                        ps = self.PS[0:mcols, bi, 0:n]

                        def mmq(e, w_v=w_v, c2=c2, t0=t0, t1=t1, ps=ps, mcols=mcols):
                            for kc in range(KC):
                                ins = e.matmul(ps, w_v[:, kc, c2 * 128:c2 * 128 + mcols], XTall[:, t0:t1, kc, :],
                                               start=(kc == 0), stop=(kc == KC - 1))
                            return ins
                        S.op("pe", mmq, reads=[g1wb[j2], bx], writes=[self.pbank[bi]])
                        if ct == 24:
                            S.op("act", lambda e, t0=t0, t1=t1, ps=ps: e.activation(out=GL[0:16, t0 * 128:t1 * 128], in_=ps, func=AF.Copy),
                                 reads=[self.pbank[bi]], writes=[glb])
                        else:
                            c = ct * 2 + c2
                            if (t0 // 4) % 2 == 0:
                                S.op("act", lambda e, c=c, t0=t0, t1=t1, ps=ps: e.activation(out=QK[:, c, t0 * 128:t1 * 128], in_=ps, func=AF.Copy),
                                     reads=[self.pbank[bi]], writes=qkb[t0:t1])
                            else:
                                S.op("dve", lambda e, c=c, t0=t0, t1=t1, ps=ps: e.tensor_copy(QK[:, c, t0 * 128:t1 * 128], ps),
                                     reads=[self.pbank[bi]], writes=qkb[t0:t1])
            else:
                is_r = ct >= 16
                col0 = (ct - 8) * 256 if not is_r else (ct - 16) * 256
                dstd = self.Rd if is_r else self.Vd
                dbuf = rdb if is_r else vdb
                for i in range(NT):
                    bi = self.nextbank()
                    ps = self.PS[:, bi, 0:256]

                    def mmv(e, w_v=w_v, i=i, ps=ps):
                        for kc in range(KC):
                            ins = e.matmul(ps, XTall[:, i, kc, :], w_v[:, kc, :], start=(kc == 0), stop=(kc == KC - 1))
                        return ins
                    S.op("pe", mmv, reads=[g1wb[j2], bx], writes=[self.pbank[bi]])
                    k = vctr[0] % 4
                    vctr[0] += 1
                    vo = VO[k]
                    if is_r:
                        k5 = vctr[0] % 2
                        S.op("act", lambda e, k5=k5, ps=ps: e.activation(out=RT[k5], in_=ps, func=AF.Silu), reads=[self.pbank[bi]], writes=[rtb[k5]])
                        S.op("dve", lambda e, vo=vo, k5=k5, col0=col0: e.tensor_tensor(out=vo, in0=RT[k5], in1=NG1[:, col0:col0 + 256], op=ALU.mult),
                             reads=[rtb[k5], ngb1], writes=[vob[k]])
                    else:
                        S.op("dve", lambda e, vo=vo, ps=ps: e.tensor_copy(vo, ps), reads=[self.pbank[bi]], writes=[vob[k]])
                    S.op("sp", lambda e, vo=vo, i=i, col0=col0, dstd=dstd: e.dma_start(out=dstd[i * 128:(i + 1) * 128, col0:col0 + 256], in_=vo),
                         reads=[vob[k]], writes=[dbuf], dma=True)
        S.barrier()
        self.sb_reset(base2)
        KS = self.sb(8 * TP // 2, BF16, [8, TP])
        ksb = [self.buf("KS%d" % i) for i in range(NT)]
        DEC = self.sb(8 * 34, F32, [8, 34])
        decb = self.buf("DEC")
        WG2 = self.sb(1024)
        NB_ = self.sb(8)
        ONE1 = self.sb(1)
        M01 = self.sb(512)
        bcst = self.buf("glaC")
        S.op("sp", lambda e: e.dma_start(out=WG2[0:16, :], in_=self.g_wg2[li]), writes=[bcst], dma=True)
        S.op("sp", lambda e: e.dma_start(out=NB_, in_=self.g_bg[li].rearrange("(j p) -> p j", p=128), allow_slow_non_contiguous=True),
             writes=[bcst], dma=True)
        S.op("dve", lambda e: e.tensor_scalar(out=NB_, in0=NB_, scalar1=-1.0, scalar2=None, op0=ALU.mult), reads=[bcst], writes=[bcst])
        S.op("pool", lambda e: e.memset(ONE1, 1.0), writes=[bcst])
        S.op("pool", lambda e: e.memset(M01, 1.0), writes=[bcst])
        S.op("pool", lambda e: e.memset(M01.rearrange("p (c t) -> p c t", t=64)[:, :, 0:1], 0.0), reads=[bcst], writes=[bcst])
        NTMP = 2
        T1 = [self.sb(512) for _ in range(NTMP)]
        CS = [self.sb(512) for _ in range(NTMP)]
        EB = [self.sb(512) for _ in range(NTMP)]
        EBi = [self.sb(512) for _ in range(NTMP)]
        DF = [self.sb(512) for _ in range(NTMP)]
        tb = [[self.buf("gla%s%d" % (nm, j)) for j in range(NTMP)] for nm in ("T1", "CS", "EB", "EBi", "DF")]
        it = 0
        for (t0, t1) in tg:
            meta_g = (t0 == 16)
            n = 16 if meta_g else (t1 - t0) * 128
            csz = 16 if meta_g else 64
            nch = n // csz
            c0 = 32 if meta_g else t0 * 2
            tsl = slice(t0 * 128, t0 * 128 + n)
            for j in range(8):
                k = it % NTMP
                it += 1
                bi = self.nextbank()
                ps = self.PS[:, bi, 0:n]
                S.op("pe", lambda e, ps=ps, j=j, tsl=tsl: e.matmul(ps, WG2[0:16, j * 128:(j + 1) * 128], GL[0:16, tsl], start=True, stop=True),
                     reads=[bcst, glb], writes=[self.pbank[bi]])
                t1_, cs_, eb_, ebi_, df_ = T1[k][:, 0:n], CS[k][:, 0:n], EB[k][:, 0:n], EBi[k][:, 0:n], DF[k][:, 0:n]
                S.op("act", lambda e, t1_=t1_, ps=ps, j=j: e.activation(out=t1_, in_=ps, func=AF.Exp, bias=NB_[:, j:j + 1], scale=-1.0),
                     reads=[self.pbank[bi], bcst], writes=[tb[0][k]])
                S.op("act", lambda e, t1_=t1_: e.activation(out=t1_, in_=t1_, func=AF.Ln, bias=ONE1, scale=1.0),
                     reads=[tb[0][k], bcst], writes=[tb[0][k]])
                S.op("dve", lambda e, cs_=cs_, t1_=t1_, n=n: e.tensor_tensor_scan(out=cs_, data0=M01[:, 0:n], data1=t1_, initial=0.0,
                                                                                 op0=ALU.mult, op1=ALU.add),
                     reads=[tb[0][k], bcst], writes=[tb[1][k]])
                S.op("act", lambda e, eb_=eb_, cs_=cs_: e.activation(out=eb_, in_=cs_, func=AF.Exp, scale=-1.0 / 16), reads=[tb[1][k]], writes=[tb[2][k]])
                S.op("act", lambda e, ebi_=ebi_, cs_=cs_: e.activation(out=ebi_, in_=cs_, func=AF.Exp, scale=1.0 / 16), reads=[tb[1][k]], writes=[tb[3][k]])
                for c in range(nch):
                    S.op("dve", lambda e, df_=df_, cs_=cs_, c=c, csz=csz: e.tensor_scalar(
                        out=df_[:, c * csz:(c + 1) * csz], in0=cs_[:, c * csz:(c + 1) * csz], scalar1=cs_[:, (c + 1) * csz - 1:(c + 1) * csz],
                        scalar2=None, op0=ALU.subtract), reads=[tb[1][k]], writes=[tb[4][k]])
                S.op("act", lambda e, df_=df_: e.activation(out=df_, in_=df_, func=AF.Exp, scale=1.0 / 16), reads=[tb[4][k]], writes=[tb[4][k]])
                S.op("act", lambda e, cs_=cs_, j=j, c0=c0, nch=nch, csz=csz: e.activation(
                    out=DEC[:, j, c0:c0 + nch], in_=cs_.rearrange("p (c t) -> p c t", t=csz)[:, :, csz - 1], func=AF.Exp, scale=-1.0 / 16),
                    reads=[tb[1][k]], writes=[decb])
                tiles_b = qkb[t0:t1]
                S.op("dve", lambda e, j=j, tsl=tsl, eb_=eb_: e.scalar_tensor_tensor(out=QK[:, j, tsl], in0=QK[:, j, tsl], scalar=1.0 / 16, in1=eb_,
                                                                                    op0=ALU.mult, op1=ALU.mult),
                     reads=[tb[2][k]] + tiles_b, writes=tiles_b)
                S.op("pool", lambda e, j=j, tsl=tsl, df_=df_: e.tensor_tensor(out=KS[:, j, tsl], in0=QK[:, 8 + j, tsl], in1=df_, op=ALU.mult),
                     reads=[tb[4][k]] + tiles_b, writes=ksb[t0:t1])
                S.op("pool", lambda e, j=j, tsl=tsl, ebi_=ebi_: e.tensor_tensor(out=QK[:, 8 + j, tsl], in0=QK[:, 8 + j, tsl], in1=ebi_, op=ALU.mult),
                     reads=[tb[3][k]] + tiles_b + ksb[t0:t1], writes=tiles_b)
        S.barrier()
        base3 = self.sb_off
        S32 = self.sb(8 * 512, F32, [8, 512])
        Sbf = self.sb(8 * 256, BF16, [8, 512])
        s32b = [self.buf("S32_%d" % j) for j in range(8)]
        sbfb = [self.buf("Sbf_%d" % j) for j in range(8)]
        MK = self.sb(256, F32, [4, 64])
        NG = self.sb(D)
        bmk = self.buf("glaMK")
        S.op("pool", lambda e: e.memset(S32, 0.0), writes=s32b)
        S.op("pool", lambda e: e.memset(Sbf, 0.0), writes=sbfb)
        S.op("pool", lambda e: e.memset(MK, 1.0), writes=[bmk])
        S.op("pool", lambda e: e.affine_select(out=MK, in_=MK, pattern=[[0, 4], [1, 64]], base=0, channel_multiplier=-1,
                                               compare_op=ALU.is_ge, fill=0.0), reads=[bmk], writes=[bmk])
        S.op("sp", lambda e: e.dma_start(out=NG, in_=self.g_ng[li:li + 1, :].partition_broadcast(128)), writes=[bmk], dma=True)
        Vc = [self.sb(1024, BF16) for _ in range(2)]
        Rc = [self.sb(1024, BF16) for _ in range(2)]
        vcb = [self.buf("glaVc%d" % j) for j in range(2)]
        rcb = [self.buf("glaRc%d" % j) for j in range(2)]
        KSc = [self.sb(512, BF16, [8, 128]) for _ in range(2)]
        kscb = [self.buf("glaKSc%d" % j) for j in range(2)]
        ATT = [self.sb(128, BF16, [4, 64]) for _ in range(2)]
        attb = [self.buf("glaATT%d" % j) for j in range(2)]
        YF = [self.sb(512) for _ in range(2)]
        yfb = [self.buf("glaYF%d" % j) for j in range(2)]
        YB = [self.sb(1024, BF16) for _ in range(2)]
        ybb = [self.buf("glaYB%d" % j) for j in range(2)]
        ST = [self.sb(6) for _ in range(4)]
        MV = [self.sb(2) for _ in range(4)]
        RS = [self.sb(1) for _ in range(4)]
        stb = [self.buf("glaST%d" % j) for j in range(4)]
        order = [(16, 0, 16)] + [(t, hf, 64) for t in range(16) for hf in range(2)]
        hctr = 0
        for n_, (tile, hf, C) in enumerate(order):
            r0 = tile * 128 + hf * 64
            ts = slice(r0, r0 + C)
            cidx = 32 if tile == 16 else tile * 2 + hf
            b2 = n_ % 2
            vc, rc, ksc, att, yb = Vc[b2], Rc[b2], KSc[b2], ATT[b2], YB[b2]
            S.op("sp", lambda e, vc=vc, ts=ts, C=C: e.dma_start(out=vc[0:C, :], in_=self.Vd[ts, :]), reads=[vdb], writes=[vcb[b2]], dma=True)
            S.op("sp", lambda e, rc=rc, ts=ts, C=C: e.dma_start(out=rc[0:C, :], in_=self.Rd[ts, :]), reads=[rdb], writes=[rcb[b2]], dma=True)
            bi = self.nextbank()
            pk = self.PS[:, bi, :].bitcast(BF16).rearrange("p (a b) -> p a b", a=8)

            def trk(e, pk=pk, ts=ts, C=C):
                for j in range(8):
                    ins = e.transpose(out=pk[0:C, j, :], in_=KS[:, j, ts], identity=self.identb)
                return ins
            S.op("pe", trk, reads=[ksb[tile], self.buf("identb")], writes=[self.pbank[bi]])
            S.op("act", lambda e, ksc=ksc, pk=pk, C=C: e.activation(out=ksc[0:C], in_=pk[0:C], func=AF.Copy), reads=[self.pbank[bi]], writes=[kscb[b2]])
            bi = self.nextbank()
            pa = self.PS[:, bi, 0:256].rearrange("p (h c) -> p h c", h=4)

            def mma(e, pa=pa, ts=ts, C=C):
                for h in range(4):
                    for dc in range(2):
                        ins = e.matmul(pa[0:C, h, 0:C], QK[:, 8 + h * 2 + dc, ts], QK[:, h * 2 + dc, ts], start=(dc == 0), stop=(dc == 1))
                return ins
            S.op("pe", mma, reads=[qkb[tile]], writes=[self.pbank[bi]])
            S.op("dve", lambda e, att=att, pa=pa, C=C: e.tensor_tensor(out=att[0:C, :, 0:C], in0=pa[0:C, :, 0:C], in1=MK[0:C, :, 0:C], op=ALU.mult),
                 reads=[self.pbank[bi], bmk], writes=[attb[b2]])
            for h in range(4):
                bo = self.nextbank()
                po = self.PS[:, bo, :]

                def mmo(e, po=po, h=h, ts=ts, C=C, att=att, vc=vc):
                    e.matmul(po[0:C, :], att[0:C, h, 0:C], vc[0:C, h * 512:(h + 1) * 512], start=True, stop=False)
                    for dc in range(2):
                        ins = e.matmul(po[0:C, :], QK[:, h * 2 + dc, ts], Sbf[:, h * 2 + dc, :], start=False, stop=(dc == 1))
                    return ins
                S.op("pe", mmo, reads=[attb[b2], vcb[b2], qkb[tile], sbfb[h * 2], sbfb[h * 2 + 1]], writes=[self.pbank[bo]])
                for dc in range(2):
                    j = h * 2 + dc
                    bs_ = self.nextbank()
                    pss = self.PS[:, bs_, :]
                    S.op("pe", lambda e, pss=pss, ksc=ksc, j=j, h=h, C=C, vc=vc: e.matmul(pss, ksc[0:C, j, :], vc[0:C, h * 512:(h + 1) * 512], start=True, stop=True),
                         reads=[kscb[b2], vcb[b2]], writes=[self.pbank[bs_]])
                    S.op("dve", lambda e, pss=pss, j=j, cidx=cidx: e.scalar_tensor_tensor(out=S32[:, j, :], in0=S32[:, j, :], scalar=DEC[:, j, cidx:cidx + 1],
                                                                                          in1=pss, op0=ALU.mult, op1=ALU.add),
                         reads=[self.pbank[bs_], decb, s32b[j]], writes=[s32b[j]])
                    if dc == 0:
                        S.op("act", lambda e, j=j: e.activation(out=Sbf[:, j, :], in_=S32[:, j, :], func=AF.Copy), reads=[s32b[j]], writes=[sbfb[j]])
                    else:
                        S.op("act", lambda e, j=j: e.activation(out=Sbf[:, j, :], in_=S32[:, j, :], func=AF.Copy), reads=[s32b[j]], writes=[sbfb[j]])
                k4 = hctr % 4
                k2 = hctr % 2
                hctr += 1
                st, mv, rs, yf = ST[k4], MV[k4], RS[k4], YF[k2]
                S.op("dve", lambda e, st=st, po=po, C=C: e.bn_stats(out=st[0:C, :], in_=po[0:C, :]), reads=[self.pbank[bo]], writes=[stb[k4]])
                S.op("dve", lambda e, st=st, mv=mv, C=C: e.bn_aggr(out=mv[0:C, :], in_=st[0:C, :]), reads=[stb[k4]], writes=[stb[k4]])
                S.op("act", lambda e, mv=mv, rs=rs, C=C: e.activation(out=rs[0:C, :], in_=mv[0:C, 1:2], func=AF.Sqrt, bias=self.eps_ap[0:C, :], scale=1.0),
                     reads=[stb[k4], self.buf("eps")], writes=[stb[k4]])
                S.op("dve", lambda e, rs=rs, C=C: e.reciprocal(out=rs[0:C, :], in_=rs[0:C, :]), reads=[stb[k4]], writes=[stb[k4]])
                S.op("dve", lambda e, mv=mv, rs=rs, st=st, C=C: e.scalar_tensor_tensor(out=st[0:C, 0:1], in0=mv[0:C, 0:1], scalar=-1.0, in1=rs[0:C, :],
                                                                                       op0=ALU.mult, op1=ALU.mult),
                     reads=[stb[k4]], writes=[stb[k4]])
                S.op("act", lambda e, yf=yf, po=po, st=st, rs=rs, C=C: e.activation(out=yf[0:C, :], in_=po[0:C, :], func=AF.Identity,
                                                                                    bias=st[0:C, 0:1], scale=rs[0:C, :]),
                     reads=[self.pbank[bo], stb[k4]], writes=[yfb[k2]])
                S.op("dve", lambda e, yf=yf, yb=yb, rc=rc, h=h, C=C: e.tensor_tensor(out=yb[0:C, h * 512:(h + 1) * 512], in0=yf[0:C, :],
                                                                                    in1=rc[0:C, h * 512:(h + 1) * 512], op=ALU.mult),
                     reads=[yfb[k2], rcb[b2]], writes=[ybb[b2]])
            bi = self.nextbank()
            py = self.PS[:, bi, :].bitcast(BF16).rearrange("p (a b) -> p a b", a=16)

            def try_(e, py=py, yb=yb, C=C):
                for kc in range(KC):
                    ins = e.transpose(out=py[:, kc, 0:C], in_=yb[0:C, kc * 128:(kc + 1) * 128], identity=self.identb[0:C, 0:C])
                return ins
            S.op("pe", try_, reads=[ybb[b2], self.buf("identb")], writes=[self.pbank[bi]])
            S.op("act", lambda e, py=py, ts=ts, C=C: e.activation(out=QK[:, :, ts], in_=py[:, :, 0:C], func=AF.Copy),
                 reads=[self.pbank[bi]], writes=[qkb[tile]])
        S.op("pool", lambda e: e.memset(QK[:, :, 2048 + NMETA:TP], 0.0), writes=[qkb[16]])
        self.outproj(QK, qkb, wo, base1)


def t5_bucket_np(dist):
    d = np.maximum(dist, 0)
    df = np.maximum(d, 1).astype(np.float32)
    large = 16 + (np.log(df / np.float32(16)) / np.float32(np.log(128 / 16)) * np.float32(16)).astype(np.int32)
    large = np.minimum(large, 31)
    return np.where(d < 16, d, large)


def bias_onehots():
    out = {}
    j = np.arange(128)

    def mk(dist, valid):
        nq, ns = dist.shape
        b = t5_bucket_np(dist)
        oh = np.zeros((33, nq, ns), np.float32)
        qq, ss = np.meshgrid(np.arange(nq), np.arange(ns), indexing="ij")
        oh[np.where(valid, b, 32), qq, ss] = 1.0
        return oh.reshape(33, nq * ns)
    d = j[:, None] - j[None, :]
    out["oh_cur"] = mk(d, d >= 0)
    d = 128 + j[:, None] - j[None, :]
    out["oh_prev"] = mk(d, d < 128)
    m = np.arange(16)
    d = NMETA + j[:, None] - m[None, :]
    out["oh_meta0"] = mk(d, np.ones_like(d, bool))
    d = NMETA + 128 + j[:, None] - m[None, :]
    out["oh_metac"] = mk(d, np.ones_like(d, bool))
    d = m[:, None] - m[None, :]
    out["oh_mm"] = mk(d, d >= 0)
    return out


def build_program(cfg):
    b = Builder(cfg)
    nc = b.build()
    return nc, b


FULL_PHASES = [
    ("init",), ("ln", 0, 0, "plain"),
    ("gla", 0), ("ln", 0, 0, "ln"), ("ffn", 0), ("ln", 0, 1, "ln"),
    ("gla", 1), ("ln", 1, 0, "ln"), ("moe", 0), ("ln", 1, 1, "ln"),
    ("kv",), ("bias",),
    ("swa", 0), ("ln", 2, 0, "ln"), ("ffn", 1), ("ln", 2, 1, "ln"),
    ("swa", 1), ("ln", 3, 0, "ln"), ("moe", 1), ("ln", 3, 1, "final"),
]

_WEIGHT_KEYS = ["meta_tokens", "rel_bias_table", "ln_gain", "ln_bias", "gla_w_in", "gla_w_gate2", "gla_b_gate",
                "gla_norm_gain", "gla_w_out", "kv_w_shared", "swa_w_q", "swa_sinks", "swa_w_out",
                "ffn_w_gate_up", "ffn_w_down", "moe_w_router", "moe_w_gate_up", "moe_w_down"]


def kernel(**inputs):
    x = np.asarray(inputs["x"], dtype=np.float32)
    nb = x.shape[0]
    nc, _ = build_program({"phases": FULL_PHASES})
    shared = {k: np.ascontiguousarray(np.asarray(inputs[k], dtype=np.float32)) for k in _WEIGHT_KEYS}
    shared.update(bias_onehots())
    in_maps = []
    for b in range(nb):
        m = dict(shared)
        m["x"] = np.ascontiguousarray(x[b])
        in_maps.append(m)
    res = run_bass_kernel_spmd(nc, in_maps, core_ids=list(range(nb)))
    return np.stack([np.asarray(r["out"], dtype=np.float32) for r in res.results], axis=0)
```

```python
import contextlib
import numpy as np
import concourse.bass as bass
import concourse.mybir as mybir
from concourse.bass_utils import run_bass_kernel_spmd

F32 = mybir.dt.float32
BF16 = mybir.dt.bfloat16
AF = mybir.ActivationFunctionType
ALU = mybir.AluOpType

D = 2048
NT = 17
TP = NT * 128
NMETA = 16
ALPHA = float(8 ** 0.25)
EPS = 1e-5
FF = 7168
NE = 8
CAPT = 5
CAP = CAPT * 128
KC = 16


class Buf:
    __slots__ = ("name", "w", "r")

    def __init__(self, name):
        self.name = name
        self.w = None
        self.r = []


class Op:
    __slots__ = ("eng", "fn", "deps", "needed", "sem", "val", "inc", "dma")


class Sched:
    ENG = ("pe", "act", "dve", "pool", "sp")

    def __init__(self, nc, es, n_dma_sems=20):
        self.nc = nc
        self.streams = {e: [] for e in self.ENG}
        self.csem = {e: es.enter_context(nc.semaphore("c_" + e)) for e in ("pe", "act", "dve", "pool")}
        self.dsem = {"sp": [es.enter_context(nc.semaphore("d_sp%d" % i)) for i in range(n_dma_sems)],
                     "act": [es.enter_context(nc.semaphore("d_act%d" % i)) for i in range(8)]}
        self.dctr = {"sp": 0, "act": 0}
        self.dlast = {}
        self.last = {e: None for e in self.ENG}

    def op(self, eng, fn, reads=(), writes=(), dma=False):
        o = Op()
        o.eng, o.fn, o.dma, o.needed = eng, fn, dma, False
        o.sem = None
        o.val = 0
        o.inc = 0
        deps = []
        for b in reads:
            if b.w is not None:
                deps.append(b.w)
        for b in writes:
            if b.w is not None:
                deps.append(b.w)
            deps.extend(b.r)
        if dma:
            pool = self.dsem[eng]
            i = self.dctr[eng] % len(pool)
            self.dctr[eng] += 1
            o.sem = pool[i]
            prev = self.dlast.get((eng, i))
            if prev is not None:
                deps.append(prev)
            self.dlast[(eng, i)] = o
            o.needed = True
        seen = set()
        od = []
        for d in deps:
            if id(d) in seen or d is o:
                continue
            seen.add(id(d))
            if d.eng == "pe" and eng == "pe" and not d.dma and not dma:
                continue
            od.append(d)
        o.deps = od
        for d in od:
            d.needed = True
        for b in reads:
            if not dma:
                b.r = [x for x in b.r if x.dma or x.eng != eng]
            b.r.append(o)
        for b in writes:
            b.w = o
            b.r = []
        self.streams[eng].append(o)
        self.last[eng] = o
        return o

    def barrier(self):
        tails = [o for o in self.last.values() if o is not None]
        tails += [o for o in self.dlast.values()]
        bb = Buf("barrier")
        for e in self.ENG:
            o = Op()
            o.eng, o.fn, o.dma, o.needed = e, None, False, False
            o.sem, o.val, o.inc = None, 0, 0
            o.deps = [t for t in tails if not (t.eng == e and not t.dma and e == "pe")]
            for d in o.deps:
                d.needed = True
            self.streams[e].append(o)

    def assign(self):
        for e, ops in self.streams.items():
            c = 0
            dc = {}
            for o in ops:
                if o.fn is None:
                    continue
                if o.dma:
                    k = id(o.sem)
                    dc[k] = dc.get(k, 0) + 16
                    o.val, o.inc = dc[k], 16
                elif o.needed:
                    c += 1
                    o.val, o.inc, o.sem = c, 1, self.csem[e]

    def emit(self, name, e):
        waited = {}
        for o in self.streams[name]:
            for d in o.deps:
                k = id(d.sem)
                if waited.get(k, 0) < d.val:
                    e.wait_ge(d.sem, d.val)
                    waited[k] = d.val
            if o.fn is None:
                continue
            ins = o.fn(e)
            if o.needed:
                ins.then_inc(o.sem, o.inc)


class Builder:
    def __init__(self, cfg):
        self.cfg = cfg
        self.nc = bass.Bass("TRN2", target_bir_lowering=False)
        self.es = contextlib.ExitStack()
        self.bufs = {}

    def buf(self, name):
        b = self.bufs.get(name)
        if b is None:
            b = self.bufs[name] = Buf(name)
        return b

    def din(self, name, shape, dt=F32):
        return self.nc.dram_tensor(name, list(shape), dt, kind="ExternalInput").ap()

    def dout(self, name, shape, dt=F32):
        return self.nc.dram_tensor(name, list(shape), dt, kind="ExternalOutput").ap()

    def dscr(self, name, shape, dt=F32):
        kind = "ExternalOutput" if name in self.cfg.get("expose", ()) else "Internal"
        return self.nc.dram_tensor(name, list(shape), dt, kind=kind).ap()

    def sb_reset(self, base=None):
        self.sb_off = self.sb_persist if base is None else base

    def sb(self, words, dt=F32, shape=None):
        words = int(words)
        a = self.sb_off
        self.sb_off += words
        assert self.sb_off <= self.SBW, ("SBUF overflow", self.sb_off, self.SBW)
        v = self.SB[:, a:a + words]
        if dt != F32:
            v = v.bitcast(dt)
        if shape is not None:
            names = " ".join("d%d" % i for i in range(len(shape)))
            kw = {"d%d" % i: int(s) for i, s in enumerate(shape)}
            v = v.rearrange("p (%s) -> p %s" % (names, names), **kw)
        return v

    def build(self):
        nc, es, cfg = self.nc, self.es, self.cfg
        self.SBW = cfg.get("sbw", 53000)
        self.SB = es.enter_context(nc.sbuf_tensor("SB", [128, self.SBW], F32))
        self.PS = es.enter_context(nc.psum_tensor("PS", [128, 8, 512], F32))
        self.S = Sched(nc, es)
        self.pbank = [self.buf("psum%d" % i) for i in range(8)]
        S = self.S

        self.x = self.din("x", [2048, D])
        self.meta = self.din("meta_tokens", [NMETA, D])
        self.ln_gain = self.din("ln_gain", [4, 2, D])
        self.ln_bias = self.din("ln_bias", [4, 2, D])
        self.out = self.dout("out", [2048, D])
        self.hres = self.dscr("hres", [TP, D])
        self.hT = self.dscr("hT", [NT, 128, KC * 128], BF16)
        self.fd = self.dscr("fd", [TP, D])
        need = cfg["phases"]
        if any(p[0] == "ffn" for p in need):
            self.w_gu = self.din("ffn_w_gate_up", [2, D, 2 * FF])
            self.w_dn = self.din("ffn_w_down", [2, FF, D])
        if any(p[0] == "gla" for p in need):
            self.g_win = self.din("gla_w_in", [2, D, 6160])
            self.g_wg2 = self.din("gla_w_gate2", [2, 16, 1024])
            self.g_bg = self.din("gla_b_gate", [2, 1024])
            self.g_ng = self.din("gla_norm_gain", [2, D])
            self.g_wout = self.din("gla_w_out", [2, D, D])
        if any(p[0] == "swa" for p in need):
            self.w_q = self.din("swa_w_q", [2, D, D])
            self.w_o = self.din("swa_w_out", [2, D, D])
            self.sinks = self.din("swa_sinks", [2, 32])
        if any(p[0] == "moe" for p in need):
            self.m_rt = self.din("moe_w_router", [2, D, NE])
            self.m_gu = self.din("moe_w_gate_up", [2, NE, D, 2 * FF])
            self.m_dn = self.din("moe_w_down", [2, NE, FF, D])
            self.XS = self.dscr("XS", [NE, CAPT, 128, KC * 128], BF16)
            self.FA = self.dscr("FA", [NE * CAP, D], BF16)
            self.SELT = self.dscr("SELT", [NT, 128, NE * CAPT * 128], BF16)

        self.sb_off = 0
        self.identb = self.sb(64, BF16, [128])
        self.identf = self.sb(128, F32, [128])
        self.gs = self.sb(NE * CAPT, F32, [NE * CAPT])
        self.eps_ap = self.sb(1)
        self.sb_persist = self.sb_off
        cb = self.buf("consts")

        S.op("pool", lambda e: e.memset(self.identf, 0.0), writes=[cb])
        S.op("pool", lambda e: e.affine_select(out=self.identf, in_=self.identf, pattern=[[-1, 128]], base=0,
                                               channel_multiplier=1, compare_op=ALU.not_equal, fill=1.0),
             reads=[cb], writes=[cb])
        S.op("pool", lambda e: e.memset(self.eps_ap, EPS), writes=[self.buf("eps")])
        S.op("pool", lambda e: e.tensor_copy(self.identb, self.identf), reads=[cb], writes=[self.buf("identb")])

        for ph in need:
            S.barrier()
            self.sb_reset()
            getattr(self, "ph_" + ph[0])(*ph[1:])
        S.barrier()

        S.assign()
        with nc.Block() as block:
            @block.tensor
            def _(e):
                S.emit("pe", e)

            @block.scalar
            def _(e):
                S.emit("act", e)

            @block.vector
            def _(e):
                S.emit("dve", e)

            @block.gpsimd
            def _(e):
                S.emit("pool", e)

            @block.sync
            def _(e):
                S.emit("sp", e)
        return nc

    def ph_init(self):
        S = self.S
        hb = [self.buf("hres%d" % i) for i in range(NT)]
        for i in range(16):
            S.op("sp", lambda e, i=i: e.dma_start(out=self.hres[i * 128:(i + 1) * 128, :],
                                                   in_=self.x[i * 128:(i + 1) * 128, :]),
                 writes=[hb[i]], dma=True)
        z = self.sb(D, F32)
        zb = self.buf("ztile")
        S.op("pool", lambda e: e.memset(z, 0.0), writes=[zb])
        S.op("sp", lambda e: e.dma_start(out=z[0:NMETA, :], in_=self.meta[:, :]), reads=[zb], writes=[zb], dma=True)
        S.op("sp", lambda e: e.dma_start(out=self.hres[2048:2176, :], in_=z), reads=[zb], writes=[hb[16]], dma=True)

    def ph_ln(self, li, which, mode):
        S = self.S
        NB = 4
        A = [self.sb(D) for _ in range(NB)]
        Fq = [self.sb(D) for _ in range(NB)]
        Yb = [self.sb(D // 2, BF16) for _ in range(NB)]
        HT = [self.sb(KC * 64, BF16, [KC, 128]) for _ in range(NB)]
        Gb = self.sb(D)
        Bb = self.sb(D)
        st = [self.sb(24, F32, [4, 6]) for _ in range(NB)]
        mv = [self.sb(2) for _ in range(NB)]
        sd = [self.sb(1) for _ in range(NB)]
        rs = [self.sb(1) for _ in range(NB)]
        bA = [self.buf("lnA%d" % j) for j in range(NB)]
        bF = [self.buf("lnF%d" % j) for j in range(NB)]
        bY = [self.buf("lnY%d" % j) for j in range(NB)]
        bH = [self.buf("lnH%d" % j) for j in range(NB)]
        bs = [self.buf("lnS%d" % j) for j in range(NB)]
        bg = self.buf("lnG")
        if mode != "plain":
            S.op("sp", lambda e: e.dma_start(out=Gb, in_=self.ln_gain[li, which:which + 1, :].partition_broadcast(128)),
                 writes=[bg], dma=True)
            S.op("sp", lambda e: e.dma_start(out=Bb, in_=self.ln_bias[li, which:which + 1, :].partition_broadcast(128)),
                 writes=[bg], dma=True)
        hb = [self.buf("hres%d" % i) for i in range(NT)]
        htb = [self.buf("hT%d" % i) for i in range(NT)]
        fb = [self.buf("fd%d" % i) for i in range(NT)]
        for i in range(NT):
            j = i % NB
            a, f, yb, ht = A[j], Fq[j], Yb[j], HT[j]
            rows = slice(i * 128, (i + 1) * 128)
            S.op("sp", lambda e, a=a, rows=rows: e.dma_start(out=a, in_=self.hres[rows, :]),
                 reads=[hb[i]], writes=[bA[j]], dma=True)
            if mode != "plain":
                S.op("sp", lambda e, f=f, rows=rows: e.dma_start(out=f, in_=self.fd[rows, :]),
                     reads=[fb[i]], writes=[bF[j]], dma=True)
                S.op("dve", lambda e, a=a, f=f: e.scalar_tensor_tensor(out=a, in0=a, scalar=ALPHA, in1=f,
                                                                       op0=ALU.mult, op1=ALU.add),
                     reads=[bF[j], bA[j]], writes=[bA[j]])
                for q in range(4):
                    S.op("dve", lambda e, a=a, q=q, s=st[j]: e.bn_stats(out=s[:, q, :], in_=a[:, q * 512:(q + 1) * 512]),
                         reads=[bA[j]], writes=[bs[j]])
                S.op("dve", lambda e, s=st[j], m=mv[j]: e.bn_aggr(out=m, in_=s), reads=[bs[j]], writes=[bs[j]])
                S.op("act", lambda e, m=mv[j], d=sd[j]: e.activation(out=d, in_=m[:, 1:2], func=AF.Sqrt, bias=self.eps_ap, scale=1.0),
                     reads=[bs[j], self.buf("eps")], writes=[bs[j]])
                S.op("dve", lambda e, d=sd[j], r=rs[j]: e.reciprocal(out=r, in_=d), reads=[bs[j]], writes=[bs[j]])
                S.op("dve", lambda e, a=a, m=mv[j]: e.scalar_tensor_tensor(out=a, in0=a, scalar=m[:, 0:1], in1=Gb,
                                                                            op0=ALU.subtract, op1=ALU.mult),
                     reads=[bs[j], bA[j], bg], writes=[bA[j]])
                S.op("dve", lambda e, a=a, r=rs[j]: e.scalar_tensor_tensor(out=a, in0=a, scalar=r, in1=Bb,
                                                                            op0=ALU.mult, op1=ALU.add),
                     reads=[bs[j], bA[j], bg], writes=[bA[j]])
                if mode == "final":
                    if i < 16:
                        S.op("act", lambda e, a=a, rows=rows: e.dma_start(out=self.out[rows, :], in_=a),
                             reads=[bA[j]], writes=[self.buf("out%d" % i)], dma=True)
                    continue
                S.op("act", lambda e, a=a, rows=rows: e.dma_start(out=self.hres[rows, :], in_=a),
                     reads=[bA[j]], writes=[hb[i]], dma=True)
            S.op("act", lambda e, a=a, yb=yb: e.activation(out=yb, in_=a, func=AF.Copy), reads=[bA[j]], writes=[bY[j]])
            for half in range(2):
                bank = self.pbank[(2 * i + half) % 8]
                pv = self.PS[:, (2 * i + half) % 8, :].bitcast(BF16).rearrange("p (a b) -> p a b", a=8)

                def tr(e, yb=yb, pv=pv, half=half):
                    for q in range(8):
                        kc = half * 8 + q
                        ins = e.transpose(out=pv[:, q, :], in_=yb[:, kc * 128:(kc + 1) * 128], identity=self.identb)
                    return ins
                S.op("pe", tr, reads=[bY[j], self.buf("identb")], writes=[bank])
                eng = "act"
                if eng == "dve":
                    S.op("dve", lambda e, ht=ht, pv=pv, half=half: e.tensor_copy(ht[:, half * 8:(half + 1) * 8, :], pv),
                         reads=[bank], writes=[bH[j]])
                else:
                    S.op("act", lambda e, ht=ht, pv=pv, half=half: e.activation(out=ht[:, half * 8:(half + 1) * 8, :], in_=pv, func=AF.Copy),
                         reads=[bank], writes=[bH[j]])
            S.op("act", lambda e, ht=ht, i=i: e.dma_start(out=self.hT[i], in_=ht.rearrange("p a b -> p (a b)")),
                 reads=[bH[j]], writes=[htb[i]], dma=True)

    def ffn_pass(self, XT, xbs, nt, wgu, wdn, sink, R):
        S = self.S
        GT, gtb = R["GT"], R["gtb"]
        groups = [(0, min(3, nt))] + ([(3, nt)] if nt > 3 else [])
        wguv = wgu.rearrange("(kc p) f -> p kc f", p=128)
        wdnv = wdn.rearrange("(hc p) f -> p hc f", p=128)
        ctr = R["ctr"]
        tiles = []
        for hp in range(FF // 256):
            tiles.append((wguv[:, :, hp * 256:(hp + 1) * 256], (KC, 256)))
            tiles.append((wguv[:, :, FF + hp * 256:FF + (hp + 1) * 256], (KC, 256)))
        for fp in range(D // 256):
            for q in range(4):
                tiles.append((wdnv[:, q * 14:(q + 1) * 14, fp * 256:(fp + 1) * 256], (14, 256)))
        issued = [0]
        handles = {}

        def load_cast(j):
            src_ap, shape3 = tiles[j]
            k = ctr[0] % 2
            ctr[0] += 1
            stg = R["stg"][k][:, 0:shape3[0] * shape3[1]].rearrange("p (a b) -> p a b", a=shape3[0])
            sbf = R["stgb"][k]
            k2 = ctr[1] % 3
            ctr[1] += 1
            wb = R["wbf"][k2][:, 0:shape3[0] * shape3[1]].rearrange("p (a b) -> p a b", a=shape3[0])
            wbb = R["wbfb"][k2]
            S.op("sp", lambda e: e.dma_start(out=stg, in_=src_ap), writes=[sbf], dma=True)
            if j % 2 == 0:
                S.op("act", lambda e: e.activation(out=wb, in_=stg, func=AF.Copy), reads=[sbf], writes=[wbb])
            else:
                S.op("dve", lambda e: e.tensor_copy(wb, stg), reads=[sbf], writes=[wbb])
            handles[j] = (wb, wbb)

        def get(j):
            while issued[0] <= min(j + 2, len(tiles) - 1):
                load_cast(issued[0])
                issued[0] += 1
            return handles.pop(j)

        def mm(e, w, p, t0, t1, h2):
            for kc in range(KC):
                ins = e.matmul(p, w[:, kc, h2 * 128:(h2 + 1) * 128], XT[:, t0:t1, kc, :],
                               start=(kc == 0), stop=(kc == KC - 1))
            return ins
        for hp in range(FF // 256):
            for part in range(2):
                w, wbb = get(hp * 2 + part)
                for h2 in range(2):
                    for gi, (t0, t1) in enumerate(groups):
                        n = (t1 - t0) * 128
                        bi = part * 4 + h2 * 2 + gi
                        p = self.PS[:, bi, 0:n]
                        S.op("pe", lambda e, w=w, p=p, t0=t0, t1=t1, h2=h2: mm(e, w, p, t0, t1, h2),
                             reads=[wbb] + xbs[t0:t1], writes=[self.pbank[bi]])
            for h2 in range(2):
                hc = hp * 2 + h2
                for gi, (t0, t1) in enumerate(groups):
                    n = (t1 - t0) * 128
                    bG = h2 * 2 + gi
                    bU = 4 + bG
                    pG = self.PS[:, bG, 0:n]
                    pU = self.PS[:, bU, 0:n]
                    k = ctr[2] % 2
                    ctr[2] += 1
                    sg = R["sil"][k][:, 0:n]
                    sgb = R["silb"][k]
                    S.op("act", lambda e, sg=sg, pG=pG: e.activation(out=sg, in_=pG, func=AF.Silu),
                         reads=[self.pbank[bG]], writes=[sgb])
                    S.op("dve", lambda e, sg=sg, pU=pU, hc=hc, t0=t0, t1=t1: e.tensor_tensor(
                        out=GT[:, hc, t0 * 128:t1 * 128], in0=sg, in1=pU, op=ALU.mult),
                        reads=[sgb, self.pbank[bU]], writes=[gtb])
        base = 2 * (FF // 256)
        for fp in range(D // 256):
            for q in range(4):
                w, wb_ = get(base + fp * 4 + q)
                for f2 in range(2):
                    for gi, (t0, t1) in enumerate(groups):
                        n = (t1 - t0) * 128
                        bi = f2 * 2 + gi
                        p = self.PS[:, bi, 0:n]

                        def mmb(e, w=w, p=p, t0=t0, t1=t1, f2=f2, q=q):
                            for h in range(14):
                                ins = e.matmul(p, w[:, h, f2 * 128:(f2 + 1) * 128], GT[:, q * 14 + h, t0 * 128:t1 * 128],
                                               start=(q == 0 and h == 0), stop=(q == 3 and h == 13))
                            return ins
                        S.op("pe", mmb, reads=[wb_, gtb], writes=[self.pbank[bi]])
            k = ctr[3] % 2
            ctr[3] += 1
            FT = R["FT"][k]
            ftb = R["ftb"][k]
            for f2 in range(2):
                for gi, (t0, t1) in enumerate(groups):
                    n = (t1 - t0) * 128
                    bi = f2 * 2 + gi
                    p = self.PS[:, bi, 0:n]
                    if (f2 + gi) % 2 == 0:
                        S.op("act", lambda e, p=p, f2=f2, t0=t0, t1=t1, FT=FT: e.activation(
                            out=FT[:, f2, t0 * 128:t1 * 128], in_=p, func=AF.Copy),
                            reads=[self.pbank[bi]], writes=[ftb])
                    else:
                        S.op("dve", lambda e, p=p, f2=f2, t0=t0, t1=t1, FT=FT: e.tensor_copy(FT[:, f2, t0 * 128:t1 * 128], p),
                             reads=[self.pbank[bi]], writes=[ftb])
            for t in range(nt):
                bi = 4 + (ctr[4] % 4)
                ctr[4] += 1
                pv = self.PS[:, bi, 0:256]

                def trb(e, pv=pv, FT=FT, t=t):
                    for f2 in range(2):
                        ins = e.transpose(out=pv[:, f2 * 128:(f2 + 1) * 128], in_=FT[:, f2, t * 128:(t + 1) * 128],
                                          identity=self.identf)
                    return ins
                S.op("pe", trb, reads=[ftb, self.buf("consts")], writes=[self.pbank[bi]])
                sink(t, fp, pv, self.pbank[bi])

    def ffn_resources(self, ntmax):
        R = {}
        R["GT"] = self.sb(56 * ntmax * 64, BF16, [56, ntmax * 128])
        R["gtb"] = self.buf("GT")
        R["stg"] = [self.sb(4096) for _ in range(2)]
        R["stgb"] = [self.buf("stg%d" % i) for i in range(2)]
        R["wbf"] = [self.sb(2048, BF16) for _ in range(3)]
        R["wbfb"] = [self.buf("wbf%d" % i) for i in range(3)]
        R["sil"] = [self.sb(384) for _ in range(2)]
        R["silb"] = [self.buf("sil%d" % i) for i in range(2)]
        R["FT"] = [self.sb(2 * ntmax * 128, F32, [2, ntmax * 128]) for _ in range(2)]
        R["ftb"] = [self.buf("FT%d" % i) for i in range(2)]
        R["OT"] = [self.sb(256) for _ in range(4)]
        R["otb"] = [self.buf("OT%d" % i) for i in range(4)]
        R["ctr"] = [0] * 8
        return R

    def ph_ffn(self, wi):
        S = self.S
        passes = [(0, 6), (6, 12), (12, 17)]
        R = self.ffn_resources(6)
        XT = self.sb(6 * KC * 64, BF16, [6, KC, 128])
        xbs = [self.buf("XT%d" % t) for t in range(6)]
        htb = [self.buf("hT%d" % i) for i in range(NT)]
        fb = [self.buf("fd%d" % i) for i in range(NT)]
        for (a, b) in passes:
            nt = b - a
            for t in range(nt):
                S.op("sp", lambda e, t=t, a=a: e.dma_start(out=XT[:, t, :, :].rearrange("p a b -> p (a b)"), in_=self.hT[a + t]),
                     reads=[htb[a + t]], writes=[xbs[t]], dma=True)

            def sink(t, fp, pv, bankbuf, a=a):
                k = R["ctr"][5] % 4
                R["ctr"][5] += 1
                ot, otb = R["OT"][k], R["otb"][k]
                S.op("dve", lambda e: e.tensor_copy(ot, pv), reads=[bankbuf], writes=[otb])
                S.op("sp", lambda e: e.dma_start(out=self.fd[(a + t) * 128:(a + t + 1) * 128, fp * 256:(fp + 1) * 256], in_=ot),
                     reads=[otb], writes=[fb[a + t]], dma=True)
            self.ffn_pass(XT, xbs, nt, self.w_gu[wi], self.w_dn[wi], sink, R)


    def nextbank(self):
        c = getattr(self, "_bankctr", 0)
        self._bankctr = c + 1
        return c % 8

    def ph_moe(self, wi):
        S = self.S
        htb = [self.buf("hT%d" % i) for i in range(NT)]
        hb = [self.buf("hres%d" % i) for i in range(NT)]
        fb = [self.buf("fd%d" % i) for i in range(NT)]
        WRf = self.sb(128, F32, [KC, 8])
        WRb = self.sb(64, BF16, [KC, 8])
        LG = self.sb(136, F32, [NT, 8])
        M8 = self.sb(136, F32, [NT, 8])
        MASK = self.sb(136, F32, [NT, 8])
        MASK1 = self.sb(136, F32, [NT, 8])
        GATE = self.sb(136, F32, [NT, 8])
        MASKb = self.sb(68, BF16, [NT, 8])
        GATEb = self.sb(68, BF16, [NT, 8])
        RANK = self.sb(136, F32, [NT, 8])
        D1, E1, DEN, G1, G2, DG = [self.sb(NT) for _ in range(6)]
        VAL = self.sb(1)
        ONESf = self.sb(128)
        TRIf = self.sb(128)
        ONESb = self.sb(64, BF16)
        TRIb = self.sb(64, BF16)
        IOTA = self.sb(CAP)
        bw, bl, bm = self.buf("moeW"), self.buf("moeLG"), self.buf("moeM")
        bc = self.buf("moeC")
        S.op("sp", lambda e: e.dma_start(out=WRf, in_=self.m_rt[wi].rearrange("(kc p) e -> p kc e", p=128)), writes=[bw], dma=True)
        S.op("act", lambda e: e.activation(out=WRb, in_=WRf, func=AF.Copy), reads=[bw], writes=[bw])
        S.op("pool", lambda e: e.memset(ONESf, 1.0), writes=[bc])
        S.op("pool", lambda e: e.affine_select(out=TRIf, in_=ONESf, pattern=[[1, 128]], base=-1, channel_multiplier=-1,
                                               compare_op=ALU.is_ge, fill=0.0), reads=[bc], writes=[bc])
        S.op("pool", lambda e: e.tensor_copy(ONESb, ONESf), reads=[bc], writes=[bc])
        S.op("pool", lambda e: e.tensor_copy(TRIb, TRIf), reads=[bc], writes=[bc])
        S.op("pool", lambda e: e.iota(IOTA, pattern=[[1, CAP]], base=0, channel_multiplier=0,
                                      allow_small_or_imprecise_dtypes=True), writes=[bc])
        S.op("pool", lambda e: e.memset(VAL, 0.0), writes=[bc])
        S.op("pool", lambda e: e.memset(VAL[0:NMETA, :], 1.0), reads=[bc], writes=[bc])
        HTi = [self.sb(1024, BF16, [KC, 128]) for _ in range(2)]
        hbuf = [self.buf("moeHT%d" % j) for j in range(2)]
        for i in range(NT):
            j = i % 2
            S.op("sp", lambda e, i=i, j=j: e.dma_start(out=HTi[j].rearrange("p a b -> p (a b)"), in_=self.hT[i]),
                 reads=[htb[i]], writes=[hbuf[j]], dma=True)
            bi = self.nextbank()
            ps = self.PS[:, bi, 0:8]

            def mml(e, j=j, ps=ps):
                for kc in range(KC):
                    ins = e.matmul(ps, HTi[j][:, kc, :], WRb[:, kc, :], start=(kc == 0), stop=(kc == KC - 1))
                return ins
            S.op("pe", mml, reads=[hbuf[j], bw], writes=[self.pbank[bi]])
            S.op("act", lambda e, i=i, ps=ps: e.activation(out=LG[:, i, :], in_=ps, func=AF.Copy),
                 reads=[self.pbank[bi]], writes=[bl])
        for i in range(NT):
            S.op("dve", lambda e, i=i: e.max(out=M8[:, i, :], in_=LG[:, i, :]), reads=[bl], writes=[bm])
        for i in range(NT):
            S.op("dve", lambda e, i=i: e.tensor_scalar(out=MASK[:, i, :], in0=LG[:, i, :], scalar1=M8[:, i, 1:2], scalar2=None,
                                                        op0=ALU.is_ge), reads=[bl, bm], writes=[self.buf("moeMASK")])
            S.op("dve", lambda e, i=i: e.tensor_scalar(out=MASK1[:, i, :], in0=LG[:, i, :], scalar1=M8[:, i, 0:1], scalar2=None,
                                                        op0=ALU.is_equal), reads=[bl, bm], writes=[self.buf("moeMASK1")])
        bk, bk1, bgt = self.buf("moeMASK"), self.buf("moeMASK1"), self.buf("moeG")
        S.op("dve", lambda e: e.tensor_scalar(out=MASK[:, NT - 1, :], in0=MASK[:, NT - 1, :], scalar1=VAL, scalar2=None, op0=ALU.mult),
             reads=[bk, bc], writes=[bk])
        S.op("dve", lambda e: e.tensor_tensor(out=D1, in0=M8[:, :, 1], in1=M8[:, :, 0], op=ALU.subtract), reads=[bm], writes=[bgt])
        S.op("act", lambda e: e.activation(out=E1, in_=D1, func=AF.Exp), reads=[bgt], writes=[bgt])
        S.op("dve", lambda e: e.tensor_scalar(out=DEN, in0=E1, scalar1=1.0, scalar2=None, op0=ALU.add), reads=[bgt], writes=[bgt])
        S.op("dve", lambda e: e.reciprocal(out=G1, in_=DEN), reads=[bgt], writes=[bgt])
        S.op("dve", lambda e: e.tensor_tensor(out=G2, in0=E1, in1=G1, op=ALU.mult), reads=[bgt], writes=[bgt])
        S.op("dve", lambda e: e.tensor_tensor(out=DG, in0=G1, in1=G2, op=ALU.subtract), reads=[bgt], writes=[bgt])
        bga = self.buf("moeGATE")
        for i in range(NT):
            S.op("dve", lambda e, i=i: e.tensor_scalar(out=GATE[:, i, :], in0=MASK[:, i, :], scalar1=G2[:, i:i + 1], scalar2=None,
                                                        op0=ALU.mult), reads=[bk, bgt], writes=[bga])
            S.op("dve", lambda e, i=i: e.scalar_tensor_tensor(out=GATE[:, i, :], in0=MASK1[:, i, :], scalar=DG[:, i:i + 1],
                                                               in1=GATE[:, i, :], op0=ALU.mult, op1=ALU.add),
                 reads=[bk1, bgt, bga], writes=[bga])
        S.op("act", lambda e: e.activation(out=MASKb, in_=MASK, func=AF.Copy), reads=[bk], writes=[self.buf("moeMASKb")])
        S.op("act", lambda e: e.activation(out=GATEb, in_=GATE, func=AF.Copy), reads=[bga], writes=[self.buf("moeGATEb")])
        bkb, bgb, brk = self.buf("moeMASKb"), self.buf("moeGATEb"), self.buf("moeRANK")
        for i in range(NT):
            bi = self.nextbank()
            ps = self.PS[:, bi, 0:8]

            def mmr(e, i=i, ps=ps):
                for j in range(i):
                    e.matmul(ps, ONESb, MASKb[:, j, :], start=(j == 0), stop=False)
                return e.matmul(ps, TRIb, MASKb[:, i, :], start=(i == 0), stop=True)
            S.op("pe", mmr, reads=[bkb, bc], writes=[self.pbank[bi]])
            S.op("act", lambda e, i=i, ps=ps: e.activation(out=RANK[:, i, :], in_=ps, func=AF.Copy),
                 reads=[self.pbank[bi]], writes=[brk])
        if self.cfg.get("dbg_route"):
            dbg = self.dout("dbg_route", [128, 4 * 136])
            for n_, t_ in enumerate((LG, MASK, GATE, RANK)):
                S.op("sp", lambda e, n_=n_, t_=t_: e.dma_start(out=dbg[:, n_ * 136:(n_ + 1) * 136], in_=t_.rearrange("p a b -> p (a b)")),
                     reads=[bl, bk, bga, brk], dma=True)
        base_after_route = self.sb_off
        HTOK = self.sb(NT * 1024, BF16, [NT, D])
        btok = self.buf("HTOK")
        ld = [self.sb(D) for _ in range(2)]
        ldb = [self.buf("moeLD%d" % j) for j in range(2)]
        for i in range(NT):
            j = i % 2
            S.op("sp", lambda e, i=i, j=j: e.dma_start(out=ld[j], in_=self.hres[i * 128:(i + 1) * 128, :]),
                 reads=[hb[i]], writes=[ldb[j]], dma=True)
            if i % 2 == 0:
                S.op("act", lambda e, i=i, j=j: e.activation(out=HTOK[:, i, :], in_=ld[j], func=AF.Copy), reads=[ldb[j]], writes=[btok])
            else:
                S.op("pool", lambda e, i=i, j=j: e.tensor_copy(HTOK[:, i, :], ld[j]), reads=[ldb[j]], writes=[btok])
        SEL = [self.sb(NT * CAP // 2, BF16, [NT, CAP]) for _ in range(2)]
        selb = [self.buf("SEL%d" % j) for j in range(2)]
        XE = [self.sb(CAPT * KC * 64, BF16, [CAPT, KC, 128]) for _ in range(2)]
        xeb = [self.buf("XE%d" % j) for j in range(2)]
        STG = [self.sb(CAPT * 64, BF16, [CAPT, 128]) for _ in range(3)]
        stgb = [self.buf("STG%d" % j) for j in range(3)]
        gsb = self.buf("gs")
        groups = [(0, 3), (3, CAPT)]
        sctr = 0
        for ex in range(NE):
            j = ex % 2
            sel = SEL[j]
            for i in range(NT):
                S.op("dve", lambda e, i=i, sel=sel, ex=ex: e.tensor_scalar(
                    out=sel[:, i, :], in0=IOTA, scalar1=RANK[:, i, ex:ex + 1], scalar2=MASK[:, i, ex:ex + 1],
                    op0=ALU.is_equal, op1=ALU.mult), reads=[brk, bk, bc], writes=[selb[j]])
            xe = XE[j]
            for kc in range(KC):
                for (t0, t1) in groups:
                    n = (t1 - t0) * 128
                    bi = self.nextbank()
                    ps = self.PS[:, bi, 0:n]

                    def mmd(e, kc=kc, t0=t0, t1=t1, ps=ps, sel=sel):
                        for i in range(NT):
                            ins = e.matmul(ps, HTOK[:, i, kc * 128:(kc + 1) * 128], sel[:, i, t0 * 128:t1 * 128],
                                           start=(i == 0), stop=(i == NT - 1))
                        return ins
                    S.op("pe", mmd, reads=[btok, selb[j]], writes=[self.pbank[bi]])
                    dst = xe[:, t0:t1, kc, :]
                    psv = ps.rearrange("p (a b) -> p a b", b=128)
                    if kc % 2 == 0:
                        S.op("act", lambda e, dst=dst, psv=psv: e.activation(out=dst, in_=psv, func=AF.Copy),
                             reads=[self.pbank[bi]], writes=[xeb[j]])
                    else:
                        S.op("dve", lambda e, dst=dst, psv=psv: e.tensor_copy(dst, psv), reads=[self.pbank[bi]], writes=[xeb[j]])
            for t in range(CAPT):
                S.op("sp", lambda e, t=t, ex=ex, xe=xe: e.dma_start(out=self.XS[ex, t], in_=xe[:, t, :, :].rearrange("p a b -> p (a b)")),
                     reads=[xeb[j]], writes=[self.buf("XS%d" % ex)], dma=True)
            for sc in range(CAPT):
                bi = self.nextbank()
                ps = self.PS[:, bi, 0:1]

                def mmg(e, sc=sc, ps=ps, sel=sel, ex=ex):
                    for i in range(NT):
                        ins = e.matmul(ps, sel[:, i, sc * 128:(sc + 1) * 128], GATEb[:, i, ex:ex + 1],
                                       start=(i == 0), stop=(i == NT - 1))
                    return ins
                S.op("pe", mmg, reads=[selb[j], bgb], writes=[self.pbank[bi]])
                S.op("act", lambda e, ps=ps, ex=ex, sc=sc: e.activation(out=self.gs[:, ex * CAPT + sc:ex * CAPT + sc + 1], in_=ps, func=AF.Copy),
                     reads=[self.pbank[bi]], writes=[gsb])
            for i in range(NT):
                bi = self.nextbank()
                pv = self.PS[:, bi, 0:CAPT * 64].bitcast(BF16).rearrange("p (a b) -> p a b", a=CAPT)

                def trs(e, i=i, pv=pv, sel=sel):
                    for sc in range(CAPT):
                        ins = e.transpose(out=pv[:, sc, :], in_=sel[:, i, sc * 128:(sc + 1) * 128], identity=self.identb)
                    return ins
                S.op("pe", trs, reads=[selb[j], self.buf("identb")], writes=[self.pbank[bi]])
                k = sctr % 3
                sctr += 1
                stg = STG[k]
                if i % 2 == 0:
                    S.op("dve", lambda e, stg=stg, pv=pv: e.tensor_copy(stg, pv), reads=[self.pbank[bi]], writes=[stgb[k]])
                else:
                    S.op("act", lambda e, stg=stg, pv=pv: e.activation(out=stg, in_=pv, func=AF.Copy), reads=[self.pbank[bi]], writes=[stgb[k]])
                S.op("sp", lambda e, stg=stg, i=i, ex=ex: e.dma_start(
                    out=self.SELT[i, :, ex * CAP:(ex + 1) * CAP], in_=stg.rearrange("p a b -> p (a b)")),
                    reads=[stgb[k]], writes=[self.buf("SELT%d" % i)], dma=True)
        S.barrier()
        self.sb_reset()
        R = self.ffn_resources(CAPT)
        XT = self.sb(CAPT * KC * 64, BF16, [CAPT, KC, 128])
        xbs = [self.buf("XT%d" % t) for t in range(CAPT)]
        OTb = [self.sb(128, BF16) for _ in range(4)]
        fab = self.buf("FA")
        for ex in range(NE):
            for t in range(CAPT):
                S.op("sp", lambda e, t=t, ex=ex: e.dma_start(out=XT[:, t, :, :].rearrange("p a b -> p (a b)"), in_=self.XS[ex, t]),
                     reads=[self.buf("XS%d" % ex)], writes=[xbs[t]], dma=True)

            def sink(t, fp, pv, bankbuf, ex=ex):
                k = R["ctr"][5] % 4
                R["ctr"][5] += 1
                ot, otb = OTb[k], R["otb"][k]
                g = self.gs[:, ex * CAPT + t:ex * CAPT + t + 1]
                S.op("dve", lambda e: e.tensor_scalar(out=ot, in0=pv, scalar1=g, scalar2=None, op0=ALU.mult),
                     reads=[bankbuf, gsb], writes=[otb])
                r0 = ex * CAP + t * 128
                S.op("sp", lambda e: e.dma_start(out=self.FA[r0:r0 + 128, fp * 256:(fp + 1) * 256], in_=ot),
                     reads=[otb], writes=[fab], dma=True)
            self.ffn_pass(XT, xbs, CAPT, self.m_gu[wi, ex], self.m_dn[wi, ex], sink, R)
        S.barrier()
        self.sb_reset()
        NC_ = NE * CAPT
        FAh = self.sb(NC_ * 512, BF16, [NC_, 1024])
        fahb = self.buf("FAh")
        STi = [self.sb(NC_ * 64, BF16, [NC_, 128]) for _ in range(2)]
        stib = [self.buf("STi%d" % j) for j in range(2)]
        OC = [self.sb(1024) for _ in range(2)]
        ocb = [self.buf("OC%d" % j) for j in range(2)]
        FAv = self.FA.rearrange("(c p) f -> p c f", p=128)
        cc = 0
        for h in range(2):
            for ex in range(NE):
                S.op("sp", lambda e, ex=ex, h=h: e.dma_start(out=FAh[:, ex * CAPT:(ex + 1) * CAPT, :],
                                                              in_=FAv[:, ex * CAPT:(ex + 1) * CAPT, h * 1024:(h + 1) * 1024]),
                     reads=[fab], writes=[fahb], dma=True)
            for i in range(NT):
                j = cc % 2
                cc += 1
                S.op("sp", lambda e, i=i, j=j: e.dma_start(out=STi[j].rearrange("p a b -> p (a b)"), in_=self.SELT[i]),
                     reads=[self.buf("SELT%d" % i)], writes=[stib[j]], dma=True)
                for nch in range(2):
                    bi = self.nextbank()
                    ps = self.PS[:, bi, :]

                    def mmc(e, j=j, nch=nch, ps=ps):
                        for c in range(NC_):
                            ins = e.matmul(ps, STi[j][:, c, :], FAh[:, c, nch * 512:(nch + 1) * 512],
                                           start=(c == 0), stop=(c == NC_ - 1))
                        return ins
                    S.op("pe", mmc, reads=[stib[j], fahb], writes=[self.pbank[bi]])
                    if nch == 0:
                        S.op("act", lambda e, j=j, ps=ps: e.activation(out=OC[j][:, 0:512], in_=ps, func=AF.Copy),
                             reads=[self.pbank[bi]], writes=[ocb[j]])
                    else:
                        S.op("dve", lambda e, j=j, ps=ps: e.tensor_copy(OC[j][:, 512:1024], ps),
                             reads=[self.pbank[bi]], writes=[ocb[j]])
                S.op("act", lambda e, i=i, j=j, h=h: e.dma_start(out=self.fd[i * 128:(i + 1) * 128, h * 1024:(h + 1) * 1024], in_=OC[j]),
                     reads=[ocb[j]], writes=[fb[i]], dma=True)


    def ph_kv(self):
        S = self.S
        htb = [self.buf("hT%d" % i) for i in range(NT)]
        wkv = self.din("kv_w_shared", [D, 512])
        self.kT2 = self.dscr("kT2", [128, 4 * TP], BF16)
        self.Vaug = self.dscr("Vaug", [NT, 128, 4 * 66], BF16)
        WKf = self.sb(KC * 512, F32, [KC, 512])
        WK2 = self.sb(KC * 4 * 64, BF16, [KC, 4, 128])
        WVb = self.sb(KC * 128, BF16, [KC, 256])
        XTall = self.sb(NT * 1024, BF16, [NT, KC, 128])
        KT = self.sb(4 * TP // 2, BF16, [4, TP])
        VA = [self.sb(4 * 33, BF16, [4, 66]) for _ in range(2)]
        bw, bw2, bx, bkt = self.buf("kvW"), self.buf("kvW2"), self.buf("kvX"), self.buf("kvKT")
        vab = [self.buf("kvVA%d" % j) for j in range(2)]
        S.op("sp", lambda e: e.dma_start(out=WKf, in_=wkv.rearrange("(kc p) f -> p kc f", p=128)), writes=[bw], dma=True)
        for g in range(4):
            for hf in range(2):
                eng = "act" if hf == 0 else "pool"
                if eng == "act":
                    S.op("act", lambda e, g=g, hf=hf: e.activation(out=WK2[:, :, g, hf * 64:(hf + 1) * 64], in_=WKf[:, :, g * 64:(g + 1) * 64], func=AF.Copy),
                         reads=[bw], writes=[bw2])
                else:
                    S.op("pool", lambda e, g=g, hf=hf: e.tensor_copy(WK2[:, :, g, hf * 64:(hf + 1) * 64], WKf[:, :, g * 64:(g + 1) * 64]),
                         reads=[bw], writes=[bw2])
        S.op("act", lambda e: e.activation(out=WVb, in_=WKf[:, :, 256:512], func=AF.Copy), reads=[bw], writes=[bw2])
        for i in range(NT):
            S.op("sp", lambda e, i=i: e.dma_start(out=XTall[:, i, :, :].rearrange("p a b -> p (a b)"), in_=self.hT[i]),
                 reads=[htb[i]], writes=[bx], dma=True)
        tg = [(0, 4), (4, 8), (8, 12), (12, 16), (16, 17)]
        for (t0, t1) in tg:
            n = (t1 - t0) * 128
            for g in range(4):
                bi = self.nextbank()
                ps = self.PS[:, bi, 0:n]

                def mmk(e, g=g, t0=t0, t1=t1, ps=ps):
                    for kc in range(KC):
                        ins = e.matmul(ps, WK2[:, kc, g, :], XTall[:, t0:t1, kc, :], start=(kc == 0), stop=(kc == KC - 1))
                    return ins
                S.op("pe", mmk, reads=[bw2, bx], writes=[self.pbank[bi]])
                if g % 2 == 0:
                    S.op("act", lambda e, g=g, t0=t0, t1=t1, ps=ps: e.activation(out=KT[:, g, t0 * 128:t1 * 128], in_=ps, func=AF.Copy),
                         reads=[self.pbank[bi]], writes=[bkt])
                else:
                    S.op("dve", lambda e, g=g, t0=t0, t1=t1, ps=ps: e.tensor_copy(KT[:, g, t0 * 128:t1 * 128], ps),
                         reads=[self.pbank[bi]], writes=[bkt])
        S.op("sp", lambda e: e.dma_start(out=self.kT2, in_=KT.rearrange("p a b -> p (a b)")), reads=[bkt], writes=[self.buf("kT2d")], dma=True)
        for i in range(NT):
            j = i % 2
            bi = self.nextbank()
            ps = self.PS[:, bi, 0:256]

            def mmv(e, i=i, ps=ps):
                for kc in range(KC):
                    ins = e.matmul(ps, XTall[:, i, kc, :], WVb[:, kc, :], start=(kc == 0), stop=(kc == KC - 1))
                return ins
            S.op("pe", mmv, reads=[bw2, bx], writes=[self.pbank[bi]])
            S.op("pool", lambda e, j=j: e.memset(VA[j][:, :, 64:66], 1.0), writes=[vab[j]])
            S.op("act", lambda e, j=j, ps=ps: e.activation(out=VA[j][:, :, 0:64], in_=ps.rearrange("p (a b) -> p a b", a=4), func=AF.Copy),
                 reads=[self.pbank[bi], vab[j]], writes=[vab[j]])
            S.op("sp", lambda e, i=i, j=j: e.dma_start(out=self.Vaug[i], in_=VA[j].rearrange("p a b -> p (a b)")),
                 reads=[vab[j]], writes=[self.buf("Vaugd")], dma=True)

    def ph_bias(self):
        S = self.S
        tab = self.din("rel_bias_table", [32, 32])
        ohs = {"cur": (128, 128), "prev": (128, 128), "meta0": (128, 16), "metac": (128, 16), "mm": (16, 16)}
        self.bias_d = {}
        T33 = self.sb(32)
        bt = self.buf("biasT")
        S.op("pool", lambda e: e.memset(T33, -30000.0), writes=[bt])
        S.op("sp", lambda e: e.dma_start(out=T33[0:32, :], in_=tab), reads=[bt], writes=[bt], dma=True)
        OH = [self.sb(128 * 128, F32, [128, 128]) for _ in range(2)]
        ohb = [self.buf("OH%d" % j) for j in range(2)]
        BT = [self.sb(32 * 128, F32, [32, 128]) for _ in range(2)]
        btb = [self.buf("BT%d" % j) for j in range(2)]
        for n_, (name, (nq, ns)) in enumerate(ohs.items()):
            j = n_ % 2
            src = self.din("oh_" + name, [33, nq * ns])
            dst = self.dscr("bias_" + name, [128, 32 * 128])
            self.bias_d[name] = dst
            oh = OH[j][:, 0:nq, 0:ns] if False else OH[j].rearrange("p a b -> p (a b)")[:, 0:nq * ns].rearrange("p (a b) -> p a b", a=nq)
            S.op("sp", lambda e, oh=oh, src=src: e.dma_start(out=oh[0:33].rearrange("p a b -> p (a b)"), in_=src), writes=[ohb[j]], dma=True)
            btile = BT[j]
            S.op("pool", lambda e, btile=btile: e.memset(btile, 0.0), writes=[btb[j]])
            for q0 in range(0, nq, 16):
                bi = self.nextbank()
                ps = self.PS[:, bi, :].rearrange("p (q h) -> p q h", q=16)

                def mmb(e, q0=q0, ps=ps, oh=oh, ns=ns):
                    for q in range(16):
                        ins = e.matmul(ps[0:ns, q, :], oh[0:33, q0 + q, :], T33[0:33, :], start=True, stop=True)
                    return ins
                S.op("pe", mmb, reads=[ohb[j], bt], writes=[self.pbank[bi]])
                S.op("dve", lambda e, q0=q0, ps=ps, btile=btile, ns=ns: e.tensor_copy(
                    btile[0:ns, :, q0:q0 + 16], ps[0:ns, :, :].rearrange("p q h -> p h q")),
                    reads=[self.pbank[bi]], writes=[btb[j]])
            S.op("sp", lambda e, dst=dst, btile=btile: e.dma_start(out=dst, in_=btile.rearrange("p a b -> p (a b)")),
                 reads=[btb[j]], writes=[self.buf("biasd")], dma=True)

    def ph_swa(self, jb):
        S = self.S
        htb = [self.buf("hT%d" % i) for i in range(NT)]
        fb = [self.buf("fd%d" % i) for i in range(NT)]
        wq = self.w_q[jb].rearrange("(kc p) f -> p kc f", p=128)
        wo = self.w_o[jb].rearrange("(kc p) f -> p kc f", p=128)
        QA = self.sb(KC * TP // 2, BF16, [KC, TP])
        qab = [self.buf("QA%d" % i) for i in range(NT)]
        base1 = self.sb_off
        XTall = self.sb(NT * 1024, BF16, [NT, KC, 128])
        bx = self.buf("swaX")
        stg = [self.sb(4096, F32, [KC, 256]) for _ in range(2)]
        stgb = [self.buf("swaStg%d" % j) for j in range(2)]
        wbf = [self.sb(2048, BF16, [KC, 256]) for _ in range(3)]
        wbfb = [self.buf("swaW%d" % j) for j in range(3)]
        for i in range(NT):
            S.op("sp", lambda e, i=i: e.dma_start(out=XTall[:, i, :, :].rearrange("p a b -> p (a b)"), in_=self.hT[i]),
                 reads=[htb[i]], writes=[bx], dma=True)
        tg = [(0, 4), (4, 8), (8, 12), (12, 16), (16, 17)]
        for cp in range(8):
            j, j2 = cp % 2, cp % 3
            S.op("sp", lambda e, cp=cp, j=j: e.dma_start(out=stg[j], in_=wq[:, :, cp * 256:(cp + 1) * 256]), writes=[stgb[j]], dma=True)
            if cp % 2 == 0:
                S.op("act", lambda e, j=j, j2=j2: e.activation(out=wbf[j2], in_=stg[j], func=AF.Copy), reads=[stgb[j]], writes=[wbfb[j2]])
            else:
                S.op("dve", lambda e, j=j, j2=j2: e.tensor_copy(wbf[j2], stg[j]), reads=[stgb[j]], writes=[wbfb[j2]])
            for c2 in range(2):
                c = cp * 2 + c2
                for (t0, t1) in tg:
                    n = (t1 - t0) * 128
                    bi = self.nextbank()
                    ps = self.PS[:, bi, 0:n]

                    def mmq(e, j2=j2, c2=c2, t0=t0, t1=t1, ps=ps):
                        for kc in range(KC):
                            ins = e.matmul(ps, wbf[j2][:, kc, c2 * 128:(c2 + 1) * 128], XTall[:, t0:t1, kc, :],
                                           start=(kc == 0), stop=(kc == KC - 1))
                        return ins
                    S.op("pe", mmq, reads=[wbfb[j2], bx], writes=[self.pbank[bi]])
                    if (t0 // 4) % 2 == 0:
                        S.op("act", lambda e, c=c, t0=t0, t1=t1, ps=ps: e.activation(out=QA[:, c, t0 * 128:t1 * 128], in_=ps, func=AF.Copy),
                             reads=[self.pbank[bi]], writes=qab[t0:t1])
                    else:
                        S.op("dve", lambda e, c=c, t0=t0, t1=t1, ps=ps: e.tensor_copy(QA[:, c, t0 * 128:t1 * 128], ps),
                             reads=[self.pbank[bi]], writes=qab[t0:t1])
        S.barrier()
        self.sb_reset(base1)
        KTh = [self.sb(4 * TP // 2, BF16, [4, TP]) for _ in range(2)]
        VA = self.sb(NT * 4 * 33, BF16, [NT, 4, 66])
        Bc = self.sb(4096, F32, [32, 128])
        Bp = self.sb(4096, F32, [32, 128])
        Bm0 = self.sb(4096, F32, [32, 128])
        Bmc = self.sb(4096, F32, [32, 128])
        Bmm = self.sb(512, F32, [32, 16])
        SK = self.sb(32)
        SKe = self.sb(32)
        bkv, bbias, bsk = self.buf("swaKV"), self.buf("swaBias"), self.buf("swaSK")
        for hf_ in range(2):
            S.op("sp", lambda e, hf_=hf_: e.dma_start(out=KTh[hf_].rearrange("p a b -> p (a b)"), in_=self.kT2),
                 reads=[self.buf("kT2d")], writes=[bkv], dma=True)
        S.op("pool", lambda e: e.memset(KTh[0][64:128], 0.0), reads=[bkv], writes=[bkv])
        S.op("pool", lambda e: e.memset(KTh[1][0:64], 0.0), reads=[bkv], writes=[bkv])
        S.op("sp", lambda e: e.dma_start(out=VA.rearrange("p t a b -> p t (a b)"), in_=self.Vaug.rearrange("t p f -> p t f")),
             reads=[self.buf("Vaugd")], writes=[bkv], dma=True)
        for t_, nm in ((Bc, "cur"), (Bp, "prev"), (Bm0, "meta0"), (Bmc, "metac")):
            S.op("sp", lambda e, t_=t_, nm=nm: e.dma_start(out=t_.rearrange("p a b -> p (a b)"), in_=self.bias_d[nm]),
                 reads=[self.buf("biasd")], writes=[bbias], dma=True)
        S.op("sp", lambda e: e.dma_start(out=Bmm, in_=self.bias_d["mm"].rearrange("p (h q) -> p h q", h=32)[:, :, 0:16]),
             reads=[self.buf("biasd")], writes=[bbias], dma=True)
        S.op("sp", lambda e: e.dma_start(out=SK, in_=self.sinks[jb:jb + 1, :].partition_broadcast(128)), writes=[bsk], dma=True)
        S.op("act", lambda e: e.activation(out=SKe, in_=SK, func=AF.Exp), reads=[bsk], writes=[bsk])
        TMP = [self.sb(512) for _ in range(4)]
        tmpb = [self.buf("swaTMP%d" % j) for j in range(4)]
        EX = [self.sb(256, BF16) for _ in range(7)]
        exb = [self.buf("swaEX%d" % j) for j in range(7)]
        DEN = [self.sb(4) for _ in range(4)]
        denb = [self.buf("swaDEN%d" % j) for j in range(4)]
        AO = [self.sb(1024, BF16) for _ in range(2)]
        aob = [self.buf("swaAO%d" % j) for j in range(2)]
        cnt = [0, 0, 0]
        blocks = list(range(16)) + [16]
        pending = [None]
        for blk in blocks:
            meta_q = (blk == 16)
            nq = NMETA if meta_q else 128
            qs = slice(blk * 128, blk * 128 + nq)
            ao = AO[blk % 2]
            aobuf = aob[blk % 2]
            if meta_q:
                S.op("pool", lambda e, ao=ao: e.memset(ao, 0.0), writes=[aobuf])
            for hg in range(8):
                g = hg // 2
                pieces = []
                if meta_q:
                    pieces.append((slice(2048, 2064), 16, Bmm, 16))
                else:
                    pieces.append((slice(blk * 128, blk * 128 + 128), blk, Bc, 128))
                    if blk > 0:
                        pieces.append((slice((blk - 1) * 128, blk * 128), blk - 1, Bp, 128))
                    pieces.append((slice(2048, 2064), 16, Bm0 if blk == 0 else Bmc, 16))
                exs = []
                for (ks, vt, btile, ns) in pieces:
                    bi = self.nextbank()
                    ps = self.PS[:, bi, :].rearrange("p (h q) -> p h q", h=4)

                    def mms(e, ks=ks, ns=ns, ps=ps, hg=hg, g=g, qs=qs, nq=nq):
                        for hh in range(4):
                            h = hg * 4 + hh
                            kc, hf = h // 2, h % 2
                            ins = e.matmul(ps[0:ns, hh, 0:nq], KTh[hf][:, g, ks], QA[:, kc, qs], start=True, stop=True)
                        return ins
                    S.op("pe", mms, reads=[bkv, qab[blk]], writes=[self.pbank[bi]])
                    k = cnt[0] % 4
                    cnt[0] += 1
                    tmp = TMP[k].rearrange("p (h q) -> p h q", h=4)
                    S.op("dve", lambda e, tmp=tmp, ps=ps, ns=ns, nq=nq, btile=btile, hg=hg: e.scalar_tensor_tensor(
                        out=tmp[0:ns, :, 0:nq], in0=ps[0:ns, :, 0:nq], scalar=0.125, in1=btile[0:ns, hg * 4:(hg + 1) * 4, 0:nq],
                        op0=ALU.mult, op1=ALU.add), reads=[self.pbank[bi], bbias], writes=[tmpb[k]])
                    k2 = cnt[1] % 7
                    cnt[1] += 1
                    ex = EX[k2].rearrange("p (h q) -> p h q", h=4)
                    S.op("act", lambda e, ex=ex, tmp=tmp, ns=ns, nq=nq: e.activation(out=ex[0:ns, :, 0:nq], in_=tmp[0:ns, :, 0:nq], func=AF.Exp),
                         reads=[tmpb[k]], writes=[exb[k2]])
                    exs.append((ex, exb[k2], vt, ns))
                bi = self.nextbank()
                po = self.PS[:, bi, :].rearrange("p (h q) -> p h q", h=4)

                def mmo(e, exs=exs, po=po, g=g, nq=nq):
                    for hh in range(4):
                        for n_, (ex, _, vt, ns) in enumerate(exs):
                            ins = e.matmul(po[0:nq, hh, 0:65], ex[0:ns, hh, 0:nq], VA[0:ns, vt, g, 0:65],
                                           start=(n_ == 0), stop=(n_ == len(exs) - 1))
                    return ins
                S.op("pe", mmo, reads=[bkv] + [x[1] for x in exs], writes=[self.pbank[bi]])
                def back(bi=bi, po=po, hg=hg, nq=nq, ao=ao, aobuf=aobuf):
                    k3 = cnt[2] % 4
                    cnt[2] += 1
                    den = DEN[k3]
                    S.op("dve", lambda e, den=den, po=po, hg=hg, nq=nq: e.tensor_tensor(
                        out=den[0:nq, :], in0=po[0:nq, :, 64], in1=SKe[0:nq, hg * 4:(hg + 1) * 4], op=ALU.add),
                        reads=[self.pbank[bi], bsk], writes=[denb[k3]])
                    S.op("dve", lambda e, den=den, nq=nq: e.reciprocal(out=den[0:nq, :], in_=den[0:nq, :]), reads=[denb[k3]], writes=[denb[k3]])
                    for hh in range(4):
                        h = hg * 4 + hh
                        if hh % 2 == 0:
                            S.op("act", lambda e, ao=ao, po=po, den=den, h=h, hh=hh, nq=nq: e.activation(
                                out=ao[0:nq, h * 64:(h + 1) * 64], in_=po[0:nq, hh, 0:64], func=AF.Copy, scale=den[0:nq, hh:hh + 1]),
                                reads=[self.pbank[bi], denb[k3]], writes=[aobuf])
                        else:
                            S.op("dve", lambda e, ao=ao, po=po, den=den, h=h, hh=hh, nq=nq: e.tensor_scalar(
                                out=ao[0:nq, h * 64:(h + 1) * 64], in0=po[0:nq, hh, 0:64], scalar1=den[0:nq, hh:hh + 1], scalar2=None, op0=ALU.mult),
                                reads=[self.pbank[bi], denb[k3]], writes=[aobuf])
                if pending[0] is not None:
                    pending[0]()
                pending[0] = back
            if pending[0] is not None:
                pending[0]()
                pending[0] = None
            for half in range(2):
                bi = self.nextbank()
                pv = self.PS[:, bi, :].bitcast(BF16).rearrange("p (a b) -> p a b", a=8)

                def tr(e, ao=ao, pv=pv, half=half):
                    for q in range(8):
                        kc = half * 8 + q
                        ins = e.transpose(out=pv[:, q, :], in_=ao[:, kc * 128:(kc + 1) * 128], identity=self.identb)
                    return ins
                S.op("pe", tr, reads=[aobuf, self.buf("identb")], writes=[self.pbank[bi]])
                dst = QA[:, half * 8:(half + 1) * 8, blk * 128:(blk + 1) * 128]
                if half == 0:
                    S.op("dve", lambda e, dst=dst, pv=pv: e.tensor_copy(dst, pv), reads=[self.pbank[bi]], writes=[qab[blk]])
                else:
                    S.op("act", lambda e, dst=dst, pv=pv: e.activation(out=dst, in_=pv, func=AF.Copy), reads=[self.pbank[bi]], writes=[qab[blk]])
        self.outproj(QA, qab, wo, base1)

    def outproj(self, QA, qab, wo, base1):
        S = self.S
        fb = [self.buf("fd%d" % i) for i in range(NT)]
        S.barrier()
        self.sb_reset(base1)
        WO = self.sb(KC * 1024, BF16, [KC, D])
        wob = self.buf("swaWO")
        sgO = [self.sb(4096, F32, [KC, 256]) for _ in range(2)]
        sgO_b = [self.buf("swaStgO%d" % j) for j in range(2)]
        OT = [self.sb(D) for _ in range(2)]
        otb = [self.buf("swaOT%d" % j) for j in range(2)]
        for cp in range(8):
            j = cp % 2
            S.op("sp", lambda e, cp=cp, j=j: e.dma_start(out=sgO[j], in_=wo[:, :, cp * 256:(cp + 1) * 256]), writes=[sgO_b[j]], dma=True)
            if cp % 2 == 0:
                S.op("dve", lambda e, j=j, cp=cp: e.tensor_copy(WO[:, :, cp * 256:(cp + 1) * 256], sgO[j]), reads=[sgO_b[j]], writes=[wob])
            else:
                S.op("act", lambda e, j=j, cp=cp: e.activation(out=WO[:, :, cp * 256:(cp + 1) * 256], in_=sgO[j], func=AF.Copy), reads=[sgO_b[j]], writes=[wob])
        for i in range(NT):
            j = i % 2
            for n in range(4):
                bi = self.nextbank()
                ps = self.PS[:, bi, :]

                def mmw(e, i=i, n=n, ps=ps):
                    for kc in range(KC):
                        ins = e.matmul(ps, QA[:, kc, i * 128:(i + 1) * 128], WO[:, kc, n * 512:(n + 1) * 512],
                                       start=(kc == 0), stop=(kc == KC - 1))
                    return ins
                S.op("pe", mmw, reads=[wob, qab[i]], writes=[self.pbank[bi]])
                if n % 2 == 0:
                    S.op("act", lambda e, j=j, n=n, ps=ps: e.activation(out=OT[j][:, n * 512:(n + 1) * 512], in_=ps, func=AF.Copy),
                         reads=[self.pbank[bi]], writes=[otb[j]])
                else:
                    S.op("dve", lambda e, j=j, n=n, ps=ps: e.tensor_copy(OT[j][:, n * 512:(n + 1) * 512], ps),
                         reads=[self.pbank[bi]], writes=[otb[j]])
            S.op("sp", lambda e, i=i, j=j: e.dma_start(out=self.fd[i * 128:(i + 1) * 128, :], in_=OT[j]),
                 reads=[otb[j]], writes=[fb[i]], dma=True)


    def ph_gla(self, li):
        S = self.S
        htb = [self.buf("hT%d" % i) for i in range(NT)]
        w_in = self.g_win[li].rearrange("(kc p) f -> p kc f", p=128)
        wo = self.g_wout[li].rearrange("(kc p) f -> p kc f", p=128)
        if not hasattr(self, "Vd"):
            self.Vd = self.dscr("Vd", [TP, D], BF16)
            self.Rd = self.dscr("Rd", [TP, D], BF16)
        QK = self.sb(KC * TP // 2, BF16, [KC, TP])
        qkb = [self.buf("QK%d" % i) for i in range(NT)]
        base1 = self.sb_off
        GL = self.sb(TP)
        glb = self.buf("GL")
        base2 = self.sb_off
        XTall = self.sb(NT * 1024, BF16, [NT, KC, 128])
        bx = self.buf("glaX")
        g1stg = [self.sb(4096, F32, [KC, 256]) for _ in range(2)]
        g1stgb = [self.buf("glaStg%d" % j) for j in range(2)]
        g1w = [self.sb(2048, BF16, [KC, 256]) for _ in range(2)]
        g1wb = [self.buf("glaW%d" % j) for j in range(2)]
        VO = [self.sb(128, BF16) for _ in range(4)]
        vob = [self.buf("glaVO%d" % j) for j in range(4)]
        RT = [self.sb(256) for _ in range(2)]
        rtb = [self.buf("glaRT%d" % j) for j in range(2)]
        NG1 = self.sb(D)
        ngb1 = self.buf("glaNG1")
        S.op("sp", lambda e: e.dma_start(out=NG1, in_=self.g_ng[li:li + 1, :].partition_broadcast(128)), writes=[ngb1], dma=True)
        for i in range(NT):
            S.op("sp", lambda e, i=i: e.dma_start(out=XTall[:, i, :, :].rearrange("p a b -> p (a b)"), in_=self.hT[i]),
                 reads=[htb[i]], writes=[bx], dma=True)
        tg = [(0, 4), (4, 8), (8, 12), (12, 16), (16, 17)]
        vctr = [0]
        vdb, rdb = self.buf("Vd"), self.buf("Rd")
        def g1_load(ct):
            j = ct % 2
            ncol = 256 if ct < 24 else 16
            stg_v = g1stg[j][:, :, 0:ncol]
            w_v = g1w[j][:, :, 0:ncol]
            S.op("sp", lambda e: e.dma_start(out=stg_v, in_=w_in[:, :, ct * 256:ct * 256 + ncol]),
                 writes=[g1stgb[j]], dma=True)
            if ct % 2 == 0:
                S.op("act", lambda e: e.activation(out=w_v, in_=stg_v, func=AF.Copy), reads=[g1stgb[j]], writes=[g1wb[j]])
            else:
                S.op("dve", lambda e: e.tensor_copy(w_v, stg_v), reads=[g1stgb[j]], writes=[g1wb[j]])
        g1_load(0)
        for ct in range(25):
            j, j2 = ct % 2, ct % 2
            ncol = 256 if ct < 24 else 16
            w_v = g1w[j2][:, :, 0:ncol]
            if ct + 1 < 25:
                g1_load(ct + 1)
            if ct < 8 or ct == 24:
                for c2 in range(2 if ct < 24 else 1):
                    mcols = 128 if ct < 24 else 16
                    for (t0, t1) in tg:
                        n = (t1 - t0) * 128
                        bi = self.nextbank()
                        ps = self.PS[0:mcols, bi, 0:n]

                        def mmq(e, w_v=w_v, c2=c2, t0=t0, t1=t1, ps=ps, mcols=mcols):
                            for kc in range(KC):
                                ins = e.matmul(ps, w_v[:, kc, c2 * 128:c2 * 128 + mcols], XTall[:, t0:t1, kc, :],
                                               start=(kc == 0), stop=(kc == KC - 1))
                            return ins
                        S.op("pe", mmq, reads=[g1wb[j2], bx], writes=[self.pbank[bi]])
                        if ct == 24:
                            S.op("act", lambda e, t0=t0, t1=t1, ps=ps: e.activation(out=GL[0:16, t0 * 128:t1 * 128], in_=ps, func=AF.Copy),
                                 reads=[self.pbank[bi]], writes=[glb])
                        else:
                            c = ct * 2 + c2
                            if (t0 // 4) % 2 == 0:
                                S.op("act", lambda e, c=c, t0=t0, t1=t1, ps=ps: e.activation(out=QK[:, c, t0 * 128:t1 * 128], in_=ps, func=AF.Copy),
                                     reads=[self.pbank[bi]], writes=qkb[t0:t1])
                            else:
                                S.op("dve", lambda e, c=c, t0=t0, t1=t1, ps=ps: e.tensor_copy(QK[:, c, t0 * 128:t1 * 128], ps),
                                     reads=[self.pbank[bi]], writes=qkb[t0:t1])
            else:
                is_r = ct >= 16
                col0 = (ct - 8) * 256 if not is_r else (ct - 16) * 256
                dstd = self.Rd if is_r else self.Vd
                dbuf = rdb if is_r else vdb
                for i in range(NT):
                    bi = self.nextbank()
                    ps = self.PS[:, bi, 0:256]

                    def mmv(e, w_v=w_v, i=i, ps=ps):
                        for kc in range(KC):
                            ins = e.matmul(ps, XTall[:, i, kc, :], w_v[:, kc, :], start=(kc == 0), stop=(kc == KC - 1))
                        return ins
                    S.op("pe", mmv, reads=[g1wb[j2], bx], writes=[self.pbank[bi]])
                    k = vctr[0] % 4
                    vctr[0] += 1
                    vo = VO[k]
                    if is_r:
                        k5 = vctr[0] % 2
                        S.op("act", lambda e, k5=k5, ps=ps: e.activation(out=RT[k5], in_=ps, func=AF.Silu), reads=[self.pbank[bi]], writes=[rtb[k5]])
                        S.op("dve", lambda e, vo=vo, k5=k5, col0=col0: e.tensor_tensor(out=vo, in0=RT[k5], in1=NG1[:, col0:col0 + 256], op=ALU.mult),
                             reads=[rtb[k5], ngb1], writes=[vob[k]])
                    else:
                        S.op("dve", lambda e, vo=vo, ps=ps: e.tensor_copy(vo, ps), reads=[self.pbank[bi]], writes=[vob[k]])
                    S.op("sp", lambda e, vo=vo, i=i, col0=col0, dstd=dstd: e.dma_start(out=dstd[i * 128:(i + 1) * 128, col0:col0 + 256], in_=vo),
                         reads=[vob[k]], writes=[dbuf], dma=True)
        S.barrier()
        self.sb_reset(base2)
        KS = self.sb(8 * TP // 2, BF16, [8, TP])
        ksb = [self.buf("KS%d" % i) for i in range(NT)]
        DEC = self.sb(8 * 34, F32, [8, 34])
        decb = self.buf("DEC")
        WG2 = self.sb(1024)
        NB_ = self.sb(8)
        ONE1 = self.sb(1)
        M01 = self.sb(512)
        bcst = self.buf("glaC")
        S.op("sp", lambda e: e.dma_start(out=WG2[0:16, :], in_=self.g_wg2[li]), writes=[bcst], dma=True)
        S.op("sp", lambda e: e.dma_start(out=NB_, in_=self.g_bg[li].rearrange("(j p) -> p j", p=128), allow_slow_non_contiguous=True),
             writes=[bcst], dma=True)
        S.op("dve", lambda e: e.tensor_scalar(out=NB_, in0=NB_, scalar1=-1.0, scalar2=None, op0=ALU.mult), reads=[bcst], writes=[bcst])
        S.op("pool", lambda e: e.memset(ONE1, 1.0), writes=[bcst])
        S.op("pool", lambda e: e.memset(M01, 1.0), writes=[bcst])
        S.op("pool", lambda e: e.memset(M01.rearrange("p (c t) -> p c t", t=64)[:, :, 0:1], 0.0), reads=[bcst], writes=[bcst])
        NTMP = 2
        T1 = [self.sb(512) for _ in range(NTMP)]
        CS = [self.sb(512) for _ in range(NTMP)]
        EB = [self.sb(512) for _ in range(NTMP)]
        EBi = [self.sb(512) for _ in range(NTMP)]
        DF = [self.sb(512) for _ in range(NTMP)]
        tb = [[self.buf("gla%s%d" % (nm, j)) for j in range(NTMP)] for nm in ("T1", "CS", "EB", "EBi", "DF")]
        it = 0
        for (t0, t1) in tg:
            meta_g = (t0 == 16)
            n = 16 if meta_g else (t1 - t0) * 128
            csz = 16 if meta_g else 64
            nch = n // csz
            c0 = 32 if meta_g else t0 * 2
            tsl = slice(t0 * 128, t0 * 128 + n)
            for j in range(8):
                k = it % NTMP
                it += 1
                bi = self.nextbank()
                ps = self.PS[:, bi, 0:n]
                S.op("pe", lambda e, ps=ps, j=j, tsl=tsl: e.matmul(ps, WG2[0:16, j * 128:(j + 1) * 128], GL[0:16, tsl], start=True, stop=True),
                     reads=[bcst, glb], writes=[self.pbank[bi]])
                t1_, cs_, eb_, ebi_, df_ = T1[k][:, 0:n], CS[k][:, 0:n], EB[k][:, 0:n], EBi[k][:, 0:n], DF[k][:, 0:n]
                S.op("act", lambda e, t1_=t1_, ps=ps, j=j: e.activation(out=t1_, in_=ps, func=AF.Exp, bias=NB_[:, j:j + 1], scale=-1.0),
                     reads=[self.pbank[bi], bcst], writes=[tb[0][k]])
                S.op("act", lambda e, t1_=t1_: e.activation(out=t1_, in_=t1_, func=AF.Ln, bias=ONE1, scale=1.0),
                     reads=[tb[0][k], bcst], writes=[tb[0][k]])
                S.op("dve", lambda e, cs_=cs_, t1_=t1_, n=n: e.tensor_tensor_scan(out=cs_, data0=M01[:, 0:n], data1=t1_, initial=0.0,
                                                                                 op0=ALU.mult, op1=ALU.add),
                     reads=[tb[0][k], bcst], writes=[tb[1][k]])
                S.op("act", lambda e, eb_=eb_, cs_=cs_: e.activation(out=eb_, in_=cs_, func=AF.Exp, scale=-1.0 / 16), reads=[tb[1][k]], writes=[tb[2][k]])
                S.op("act", lambda e, ebi_=ebi_, cs_=cs_: e.activation(out=ebi_, in_=cs_, func=AF.Exp, scale=1.0 / 16), reads=[tb[1][k]], writes=[tb[3][k]])
                for c in range(nch):
                    S.op("dve", lambda e, df_=df_, cs_=cs_, c=c, csz=csz: e.tensor_scalar(
                        out=df_[:, c * csz:(c + 1) * csz], in0=cs_[:, c * csz:(c + 1) * csz], scalar1=cs_[:, (c + 1) * csz - 1:(c + 1) * csz],
                        scalar2=None, op0=ALU.subtract), reads=[tb[1][k]], writes=[tb[4][k]])
                S.op("act", lambda e, df_=df_: e.activation(out=df_, in_=df_, func=AF.Exp, scale=1.0 / 16), reads=[tb[4][k]], writes=[tb[4][k]])
                S.op("act", lambda e, cs_=cs_, j=j, c0=c0, nch=nch, csz=csz: e.activation(
                    out=DEC[:, j, c0:c0 + nch], in_=cs_.rearrange("p (c t) -> p c t", t=csz)[:, :, csz - 1], func=AF.Exp, scale=-1.0 / 16),
                    reads=[tb[1][k]], writes=[decb])
                tiles_b = qkb[t0:t1]
                S.op("dve", lambda e, j=j, tsl=tsl, eb_=eb_: e.scalar_tensor_tensor(out=QK[:, j, tsl], in0=QK[:, j, tsl], scalar=1.0 / 16, in1=eb_,
                                                                                    op0=ALU.mult, op1=ALU.mult),
                     reads=[tb[2][k]] + tiles_b, writes=tiles_b)
                S.op("pool", lambda e, j=j, tsl=tsl, df_=df_: e.tensor_tensor(out=KS[:, j, tsl], in0=QK[:, 8 + j, tsl], in1=df_, op=ALU.mult),
                     reads=[tb[4][k]] + tiles_b, writes=ksb[t0:t1])
                S.op("pool", lambda e, j=j, tsl=tsl, ebi_=ebi_: e.tensor_tensor(out=QK[:, 8 + j, tsl], in0=QK[:, 8 + j, tsl], in1=ebi_, op=ALU.mult),
                     reads=[tb[3][k]] + tiles_b + ksb[t0:t1], writes=tiles_b)
        S.barrier()
        base3 = self.sb_off
        S32 = self.sb(8 * 512, F32, [8, 512])
        Sbf = self.sb(8 * 256, BF16, [8, 512])
        s32b = [self.buf("S32_%d" % j) for j in range(8)]
        sbfb = [self.buf("Sbf_%d" % j) for j in range(8)]
        MK = self.sb(256, F32, [4, 64])
        NG = self.sb(D)
        bmk = self.buf("glaMK")
        S.op("pool", lambda e: e.memset(S32, 0.0), writes=s32b)
        S.op("pool", lambda e: e.memset(Sbf, 0.0), writes=sbfb)
        S.op("pool", lambda e: e.memset(MK, 1.0), writes=[bmk])
        S.op("pool", lambda e: e.affine_select(out=MK, in_=MK, pattern=[[0, 4], [1, 64]], base=0, channel_multiplier=-1,
                                               compare_op=ALU.is_ge, fill=0.0), reads=[bmk], writes=[bmk])
        S.op("sp", lambda e: e.dma_start(out=NG, in_=self.g_ng[li:li + 1, :].partition_broadcast(128)), writes=[bmk], dma=True)
        Vc = [self.sb(1024, BF16) for _ in range(2)]
        Rc = [self.sb(1024, BF16) for _ in range(2)]
        vcb = [self.buf("glaVc%d" % j) for j in range(2)]
        rcb = [self.buf("glaRc%d" % j) for j in range(2)]
        KSc = [self.sb(512, BF16, [8, 128]) for _ in range(2)]
        kscb = [self.buf("glaKSc%d" % j) for j in range(2)]
        ATT = [self.sb(128, BF16, [4, 64]) for _ in range(2)]
        attb = [self.buf("glaATT%d" % j) for j in range(2)]
        YF = [self.sb(512) for _ in range(2)]
        yfb = [self.buf("glaYF%d" % j) for j in range(2)]
        YB = [self.sb(1024, BF16) for _ in range(2)]
        ybb = [self.buf("glaYB%d" % j) for j in range(2)]
        ST = [self.sb(6) for _ in range(4)]
        MV = [self.sb(2) for _ in range(4)]
        RS = [self.sb(1) for _ in range(4)]
        stb = [self.buf("glaST%d" % j) for j in range(4)]
        order = [(16, 0, 16)] + [(t, hf, 64) for t in range(16) for hf in range(2)]
        hctr = 0
        for n_, (tile, hf, C) in enumerate(order):
            r0 = tile * 128 + hf * 64
            ts = slice(r0, r0 + C)
            cidx = 32 if tile == 16 else tile * 2 + hf
            b2 = n_ % 2
            vc, rc, ksc, att, yb = Vc[b2], Rc[b2], KSc[b2], ATT[b2], YB[b2]
            S.op("sp", lambda e, vc=vc, ts=ts, C=C: e.dma_start(out=vc[0:C, :], in_=self.Vd[ts, :]), reads=[vdb], writes=[vcb[b2]], dma=True)
            S.op("sp", lambda e, rc=rc, ts=ts, C=C: e.dma_start(out=rc[0:C, :], in_=self.Rd[ts, :]), reads=[rdb], writes=[rcb[b2]], dma=True)
            bi = self.nextbank()
            pk = self.PS[:, bi, :].bitcast(BF16).rearrange("p (a b) -> p a b", a=8)

            def trk(e, pk=pk, ts=ts, C=C):
                for j in range(8):
                    ins = e.transpose(out=pk[0:C, j, :], in_=KS[:, j, ts], identity=self.identb)
                return ins
            S.op("pe", trk, reads=[ksb[tile], self.buf("identb")], writes=[self.pbank[bi]])
            S.op("act", lambda e, ksc=ksc, pk=pk, C=C: e.activation(out=ksc[0:C], in_=pk[0:C], func=AF.Copy), reads=[self.pbank[bi]], writes=[kscb[b2]])
            bi = self.nextbank()
            pa = self.PS[:, bi, 0:256].rearrange("p (h c) -> p h c", h=4)

            def mma(e, pa=pa, ts=ts, C=C):
                for h in range(4):
                    for dc in range(2):
                        ins = e.matmul(pa[0:C, h, 0:C], QK[:, 8 + h * 2 + dc, ts], QK[:, h * 2 + dc, ts], start=(dc == 0), stop=(dc == 1))
                return ins
            S.op("pe", mma, reads=[qkb[tile]], writes=[self.pbank[bi]])
            S.op("dve", lambda e, att=att, pa=pa, C=C: e.tensor_tensor(out=att[0:C, :, 0:C], in0=pa[0:C, :, 0:C], in1=MK[0:C, :, 0:C], op=ALU.mult),
                 reads=[self.pbank[bi], bmk], writes=[attb[b2]])
            for h in range(4):
                bo = self.nextbank()
                po = self.PS[:, bo, :]

                def mmo(e, po=po, h=h, ts=ts, C=C, att=att, vc=vc):
                    e.matmul(po[0:C, :], att[0:C, h, 0:C], vc[0:C, h * 512:(h + 1) * 512], start=True, stop=False)
                    for dc in range(2):
                        ins = e.matmul(po[0:C, :], QK[:, h * 2 + dc, ts], Sbf[:, h * 2 + dc, :], start=False, stop=(dc == 1))
                    return ins
                S.op("pe", mmo, reads=[attb[b2], vcb[b2], qkb[tile], sbfb[h * 2], sbfb[h * 2 + 1]], writes=[self.pbank[bo]])
                for dc in range(2):
                    j = h * 2 + dc
                    bs_ = self.nextbank()
                    pss = self.PS[:, bs_, :]
                    S.op("pe", lambda e, pss=pss, ksc=ksc, j=j, h=h, C=C, vc=vc: e.matmul(pss, ksc[0:C, j, :], vc[0:C, h * 512:(h + 1) * 512], start=True, stop=True),
                         reads=[kscb[b2], vcb[b2]], writes=[self.pbank[bs_]])
                    S.op("dve", lambda e, pss=pss, j=j, cidx=cidx: e.scalar_tensor_tensor(out=S32[:, j, :], in0=S32[:, j, :], scalar=DEC[:, j, cidx:cidx + 1],
                                                                                          in1=pss, op0=ALU.mult, op1=ALU.add),
                         reads=[self.pbank[bs_], decb, s32b[j]], writes=[s32b[j]])
                    if dc == 0:
                        S.op("act", lambda e, j=j: e.activation(out=Sbf[:, j, :], in_=S32[:, j, :], func=AF.Copy), reads=[s32b[j]], writes=[sbfb[j]])
                    else:
                        S.op("act", lambda e, j=j: e.activation(out=Sbf[:, j, :], in_=S32[:, j, :], func=AF.Copy), reads=[s32b[j]], writes=[sbfb[j]])
                k4 = hctr % 4
                k2 = hctr % 2
                hctr += 1
                st, mv, rs, yf = ST[k4], MV[k4], RS[k4], YF[k2]
                S.op("dve", lambda e, st=st, po=po, C=C: e.bn_stats(out=st[0:C, :], in_=po[0:C, :]), reads=[self.pbank[bo]], writes=[stb[k4]])
                S.op("dve", lambda e, st=st, mv=mv, C=C: e.bn_aggr(out=mv[0:C, :], in_=st[0:C, :]), reads=[stb[k4]], writes=[stb[k4]])
                S.op("act", lambda e, mv=mv, rs=rs, C=C: e.activation(out=rs[0:C, :], in_=mv[0:C, 1:2], func=AF.Sqrt, bias=self.eps_ap[0:C, :], scale=1.0),
                     reads=[stb[k4], self.buf("eps")], writes=[stb[k4]])
                S.op("dve", lambda e, rs=rs, C=C: e.reciprocal(out=rs[0:C, :], in_=rs[0:C, :]), reads=[stb[k4]], writes=[stb[k4]])
                S.op("dve", lambda e, mv=mv, rs=rs, st=st, C=C: e.scalar_tensor_tensor(out=st[0:C, 0:1], in0=mv[0:C, 0:1], scalar=-1.0, in1=rs[0:C, :],
                                                                                       op0=ALU.mult, op1=ALU.mult),
                     reads=[stb[k4]], writes=[stb[k4]])
                S.op("act", lambda e, yf=yf, po=po, st=st, rs=rs, C=C: e.activation(out=yf[0:C, :], in_=po[0:C, :], func=AF.Identity,
                                                                                    bias=st[0:C, 0:1], scale=rs[0:C, :]),
                     reads=[self.pbank[bo], stb[k4]], writes=[yfb[k2]])
                S.op("dve", lambda e, yf=yf, yb=yb, rc=rc, h=h, C=C: e.tensor_tensor(out=yb[0:C, h * 512:(h + 1) * 512], in0=yf[0:C, :],
                                                                                    in1=rc[0:C, h * 512:(h + 1) * 512], op=ALU.mult),
                     reads=[yfb[k2], rcb[b2]], writes=[ybb[b2]])
            bi = self.nextbank()
            py = self.PS[:, bi, :].bitcast(BF16).rearrange("p (a b) -> p a b", a=16)

            def try_(e, py=py, yb=yb, C=C):
                for kc in range(KC):
                    ins = e.transpose(out=py[:, kc, 0:C], in_=yb[0:C, kc * 128:(kc + 1) * 128], identity=self.identb[0:C, 0:C])
                return ins
            S.op("pe", try_, reads=[ybb[b2], self.buf("identb")], writes=[self.pbank[bi]])
            S.op("act", lambda e, py=py, ts=ts, C=C: e.activation(out=QK[:, :, ts], in_=py[:, :, 0:C], func=AF.Copy),
                 reads=[self.pbank[bi]], writes=[qkb[tile]])
        S.op("pool", lambda e: e.memset(QK[:, :, 2048 + NMETA:TP], 0.0), writes=[qkb[16]])
        self.outproj(QK, qkb, wo, base1)


def t5_bucket_np(dist):
    d = np.maximum(dist, 0)
    df = np.maximum(d, 1).astype(np.float32)
    large = 16 + (np.log(df / np.float32(16)) / np.float32(np.log(128 / 16)) * np.float32(16)).astype(np.int32)
    large = np.minimum(large, 31)
    return np.where(d < 16, d, large)


def bias_onehots():
    out = {}
    j = np.arange(128)

    def mk(dist, valid):
        nq, ns = dist.shape
        b = t5_bucket_np(dist)
        oh = np.zeros((33, nq, ns), np.float32)
        qq, ss = np.meshgrid(np.arange(nq), np.arange(ns), indexing="ij")
        oh[np.where(valid, b, 32), qq, ss] = 1.0
        return oh.reshape(33, nq * ns)
    d = j[:, None] - j[None, :]
    out["oh_cur"] = mk(d, d >= 0)
    d = 128 + j[:, None] - j[None, :]
    out["oh_prev"] = mk(d, d < 128)
    m = np.arange(16)
    d = NMETA + j[:, None] - m[None, :]
    out["oh_meta0"] = mk(d, np.ones_like(d, bool))
    d = NMETA + 128 + j[:, None] - m[None, :]
    out["oh_metac"] = mk(d, np.ones_like(d, bool))
    d = m[:, None] - m[None, :]
    out["oh_mm"] = mk(d, d >= 0)
    return out


def build_program(cfg):
    b = Builder(cfg)
    nc = b.build()
    return nc, b


FULL_PHASES = [
    ("init",), ("ln", 0, 0, "plain"),
    ("gla", 0), ("ln", 0, 0, "ln"), ("ffn", 0), ("ln", 0, 1, "ln"),
    ("gla", 1), ("ln", 1, 0, "ln"), ("moe", 0), ("ln", 1, 1, "ln"),
    ("kv",), ("bias",),
    ("swa", 0), ("ln", 2, 0, "ln"), ("ffn", 1), ("ln", 2, 1, "ln"),
    ("swa", 1), ("ln", 3, 0, "ln"), ("moe", 1), ("ln", 3, 1, "final"),
]

_WEIGHT_KEYS = ["meta_tokens", "rel_bias_table", "ln_gain", "ln_bias", "gla_w_in", "gla_w_gate2", "gla_b_gate",
                "gla_norm_gain", "gla_w_out", "kv_w_shared", "swa_w_q", "swa_sinks", "swa_w_out",
                "ffn_w_gate_up", "ffn_w_down", "moe_w_router", "moe_w_gate_up", "moe_w_down"]


def kernel(**inputs):
    x = np.asarray(inputs["x"], dtype=np.float32)
    nb = x.shape[0]
    nc, _ = build_program({"phases": FULL_PHASES})
    shared = {k: np.ascontiguousarray(np.asarray(inputs[k], dtype=np.float32)) for k in _WEIGHT_KEYS}
    shared.update(bias_onehots())
    in_maps = []
    for b in range(nb):
        m = dict(shared)
        m["x"] = np.ascontiguousarray(x[b])
        in_maps.append(m)
    res = run_bass_kernel_spmd(nc, in_maps, core_ids=list(range(nb)))
    return np.stack([np.asarray(r["out"], dtype=np.float32) for r in res.results], axis=0)
```

```python
import contextlib
import numpy as np
import concourse.bass as bass
import concourse.mybir as mybir
from concourse.bass_utils import run_bass_kernel_spmd

F32 = mybir.dt.float32
BF16 = mybir.dt.bfloat16
AF = mybir.ActivationFunctionType
ALU = mybir.AluOpType

D = 2048
NT = 17
TP = NT * 128
NMETA = 16
ALPHA = float(8 ** 0.25)
EPS = 1e-5
FF = 7168
NE = 8
CAPT = 5
CAP = CAPT * 128
KC = 16


class Buf:
    __slots__ = ("name", "w", "r")

    def __init__(self, name):
        self.name = name
        self.w = None
        self.r = []


class Op:
    __slots__ = ("eng", "fn", "deps", "needed", "sem", "val", "inc", "dma")


class Sched:
    ENG = ("pe", "act", "dve", "pool", "sp")

    def __init__(self, nc, es, n_dma_sems=20):
        self.nc = nc
        self.streams = {e: [] for e in self.ENG}
        self.csem = {e: es.enter_context(nc.semaphore("c_" + e)) for e in ("pe", "act", "dve", "pool")}
        self.dsem = {"sp": [es.enter_context(nc.semaphore("d_sp%d" % i)) for i in range(n_dma_sems)],
                     "act": [es.enter_context(nc.semaphore("d_act%d" % i)) for i in range(8)]}
        self.dctr = {"sp": 0, "act": 0}
        self.dlast = {}
        self.last = {e: None for e in self.ENG}

    def op(self, eng, fn, reads=(), writes=(), dma=False):
        o = Op()
        o.eng, o.fn, o.dma, o.needed = eng, fn, dma, False
        o.sem = None
        o.val = 0
        o.inc = 0
        deps = []
        for b in reads:
            if b.w is not None:
                deps.append(b.w)
        for b in writes:
            if b.w is not None:
                deps.append(b.w)
            deps.extend(b.r)
        if dma:
            pool = self.dsem[eng]
            i = self.dctr[eng] % len(pool)
            self.dctr[eng] += 1
            o.sem = pool[i]
            prev = self.dlast.get((eng, i))
            if prev is not None:
                deps.append(prev)
            self.dlast[(eng, i)] = o
            o.needed = True
        seen = set()
        od = []
        for d in deps:
            if id(d) in seen or d is o:
                continue
            seen.add(id(d))
            if d.eng == "pe" and eng == "pe" and not d.dma and not dma:
                continue
            od.append(d)
        o.deps = od
        for d in od:
            d.needed = True
        for b in reads:
            if not dma:
                b.r = [x for x in b.r if x.dma or x.eng != eng]
            b.r.append(o)
        for b in writes:
            b.w = o
            b.r = []
        self.streams[eng].append(o)
        self.last[eng] = o
        return o

    def barrier(self):
        tails = [o for o in self.last.values() if o is not None]
        tails += [o for o in self.dlast.values()]
        bb = Buf("barrier")
        for e in self.ENG:
            o = Op()
            o.eng, o.fn, o.dma, o.needed = e, None, False, False
            o.sem, o.val, o.inc = None, 0, 0
            o.deps = [t for t in tails if not (t.eng == e and not t.dma and e == "pe")]
            for d in o.deps:
                d.needed = True
            self.streams[e].append(o)

    def assign(self):
        for e, ops in self.streams.items():
            c = 0
            dc = {}
            for o in ops:
                if o.fn is None:
                    continue
                if o.dma:
                    k = id(o.sem)
                    dc[k] = dc.get(k, 0) + 16
                    o.val, o.inc = dc[k], 16
                elif o.needed:
                    c += 1
                    o.val, o.inc, o.sem = c, 1, self.csem[e]

    def emit(self, name, e):
        waited = {}
        for o in self.streams[name]:
            for d in o.deps:
                k = id(d.sem)
                if waited.get(k, 0) < d.val:
                    e.wait_ge(d.sem, d.val)
                    waited[k] = d.val
            if o.fn is None:
                continue
            ins = o.fn(e)
            if o.needed:
                ins.then_inc(o.sem, o.inc)


class Builder:
    def __init__(self, cfg):
        self.cfg = cfg
        self.nc = bass.Bass("TRN2", target_bir_lowering=False)
        self.es = contextlib.ExitStack()
        self.bufs = {}

    def buf(self, name):
        b = self.bufs.get(name)
        if b is None:
            b = self.bufs[name] = Buf(name)
        return b

    def din(self, name, shape, dt=F32):
        return self.nc.dram_tensor(name, list(shape), dt, kind="ExternalInput").ap()

    def dout(self, name, shape, dt=F32):
        return self.nc.dram_tensor(name, list(shape), dt, kind="ExternalOutput").ap()

    def dscr(self, name, shape, dt=F32):
        kind = "ExternalOutput" if name in self.cfg.get("expose", ()) else "Internal"
        return self.nc.dram_tensor(name, list(shape), dt, kind=kind).ap()

    def sb_reset(self, base=None):
        self.sb_off = self.sb_persist if base is None else base

    def sb(self, words, dt=F32, shape=None):
        words = int(words)
        a = self.sb_off
        self.sb_off += words
        assert self.sb_off <= self.SBW, ("SBUF overflow", self.sb_off, self.SBW)
        v = self.SB[:, a:a + words]
        if dt != F32:
            v = v.bitcast(dt)
        if shape is not None:
            names = " ".join("d%d" % i for i in range(len(shape)))
            kw = {"d%d" % i: int(s) for i, s in enumerate(shape)}
            v = v.rearrange("p (%s) -> p %s" % (names, names), **kw)
        return v

    def build(self):
        nc, es, cfg = self.nc, self.es, self.cfg
        self.SBW = cfg.get("sbw", 53000)
        self.SB = es.enter_context(nc.sbuf_tensor("SB", [128, self.SBW], F32))
        self.PS = es.enter_context(nc.psum_tensor("PS", [128, 8, 512], F32))
        self.S = Sched(nc, es)
        self.pbank = [self.buf("psum%d" % i) for i in range(8)]
        S = self.S

        self.x = self.din("x", [2048, D])
        self.meta = self.din("meta_tokens", [NMETA, D])
        self.ln_gain = self.din("ln_gain", [4, 2, D])
        self.ln_bias = self.din("ln_bias", [4, 2, D])
        self.out = self.dout("out", [2048, D])
        self.hres = self.dscr("hres", [TP, D])
        self.hT = self.dscr("hT", [NT, 128, KC * 128], BF16)
        self.fd = self.dscr("fd", [TP, D])
        need = cfg["phases"]
        if any(p[0] == "ffn" for p in need):
            self.w_gu = self.din("ffn_w_gate_up", [2, D, 2 * FF])
            self.w_dn = self.din("ffn_w_down", [2, FF, D])
        if any(p[0] == "gla" for p in need):
            self.g_win = self.din("gla_w_in", [2, D, 6160])
            self.g_wg2 = self.din("gla_w_gate2", [2, 16, 1024])
            self.g_bg = self.din("gla_b_gate", [2, 1024])
            self.g_ng = self.din("gla_norm_gain", [2, D])
            self.g_wout = self.din("gla_w_out", [2, D, D])
        if any(p[0] == "swa" for p in need):
            self.w_q = self.din("swa_w_q", [2, D, D])
            self.w_o = self.din("swa_w_out", [2, D, D])
            self.sinks = self.din("swa_sinks", [2, 32])
        if any(p[0] == "moe" for p in need):
            self.m_rt = self.din("moe_w_router", [2, D, NE])
            self.m_gu = self.din("moe_w_gate_up", [2, NE, D, 2 * FF])
            self.m_dn = self.din("moe_w_down", [2, NE, FF, D])
            self.XS = self.dscr("XS", [NE, CAPT, 128, KC * 128], BF16)
            self.FA = self.dscr("FA", [NE * CAP, D], BF16)
            self.SELT = self.dscr("SELT", [NT, 128, NE * CAPT * 128], BF16)

        self.sb_off = 0
        self.identb = self.sb(64, BF16, [128])
        self.identf = self.sb(128, F32, [128])
        self.gs = self.sb(NE * CAPT, F32, [NE * CAPT])
        self.eps_ap = self.sb(1)
        self.sb_persist = self.sb_off
        cb = self.buf("consts")

        S.op("pool", lambda e: e.memset(self.identf, 0.0), writes=[cb])
        S.op("pool", lambda e: e.affine_select(out=self.identf, in_=self.identf, pattern=[[-1, 128]], base=0,
                                               channel_multiplier=1, compare_op=ALU.not_equal, fill=1.0),
             reads=[cb], writes=[cb])
        S.op("pool", lambda e: e.memset(self.eps_ap, EPS), writes=[self.buf("eps")])
        S.op("pool", lambda e: e.tensor_copy(self.identb, self.identf), reads=[cb], writes=[self.buf("identb")])

        for ph in need:
            S.barrier()
            self.sb_reset()
            getattr(self, "ph_" + ph[0])(*ph[1:])
        S.barrier()

        S.assign()
        with nc.Block() as block:
            @block.tensor
            def _(e):
                S.emit("pe", e)

            @block.scalar
            def _(e):
                S.emit("act", e)

            @block.vector
            def _(e):
                S.emit("dve", e)

            @block.gpsimd
            def _(e):
                S.emit("pool", e)

            @block.sync
            def _(e):
                S.emit("sp", e)
        return nc

    def ph_init(self):
        S = self.S
        hb = [self.buf("hres%d" % i) for i in range(NT)]
        for i in range(16):
            S.op("sp", lambda e, i=i: e.dma_start(out=self.hres[i * 128:(i + 1) * 128, :],
                                                   in_=self.x[i * 128:(i + 1) * 128, :]),
                 writes=[hb[i]], dma=True)
        z = self.sb(D, F32)
        zb = self.buf("ztile")
        S.op("pool", lambda e: e.memset(z, 0.0), writes=[zb])
        S.op("sp", lambda e: e.dma_start(out=z[0:NMETA, :], in_=self.meta[:, :]), reads=[zb], writes=[zb], dma=True)
        S.op("sp", lambda e: e.dma_start(out=self.hres[2048:2176, :], in_=z), reads=[zb], writes=[hb[16]], dma=True)

    def ph_ln(self, li, which, mode):
        S = self.S
        NB = 4
        A = [self.sb(D) for _ in range(NB)]
        Fq = [self.sb(D) for _ in range(NB)]
        Yb = [self.sb(D // 2, BF16) for _ in range(NB)]
        HT = [self.sb(KC * 64, BF16, [KC, 128]) for _ in range(NB)]
        Gb = self.sb(D)
        Bb = self.sb(D)
        st = [self.sb(24, F32, [4, 6]) for _ in range(NB)]
        mv = [self.sb(2) for _ in range(NB)]
        sd = [self.sb(1) for _ in range(NB)]
        rs = [self.sb(1) for _ in range(NB)]
        bA = [self.buf("lnA%d" % j) for j in range(NB)]
        bF = [self.buf("lnF%d" % j) for j in range(NB)]
        bY = [self.buf("lnY%d" % j) for j in range(NB)]
        bH = [self.buf("lnH%d" % j) for j in range(NB)]
        bs = [self.buf("lnS%d" % j) for j in range(NB)]
        bg = self.buf("lnG")
        if mode != "plain":
            S.op("sp", lambda e: e.dma_start(out=Gb, in_=self.ln_gain[li, which:which + 1, :].partition_broadcast(128)),
                 writes=[bg], dma=True)
            S.op("sp", lambda e: e.dma_start(out=Bb, in_=self.ln_bias[li, which:which + 1, :].partition_broadcast(128)),
                 writes=[bg], dma=True)
        hb = [self.buf("hres%d" % i) for i in range(NT)]
        htb = [self.buf("hT%d" % i) for i in range(NT)]
        fb = [self.buf("fd%d" % i) for i in range(NT)]
        for i in range(NT):
            j = i % NB
            a, f, yb, ht = A[j], Fq[j], Yb[j], HT[j]
            rows = slice(i * 128, (i + 1) * 128)
            S.op("sp", lambda e, a=a, rows=rows: e.dma_start(out=a, in_=self.hres[rows, :]),
                 reads=[hb[i]], writes=[bA[j]], dma=True)
            if mode != "plain":
                S.op("sp", lambda e, f=f, rows=rows: e.dma_start(out=f, in_=self.fd[rows, :]),
                     reads=[fb[i]], writes=[bF[j]], dma=True)
                S.op("dve", lambda e, a=a, f=f: e.scalar_tensor_tensor(out=a, in0=a, scalar=ALPHA, in1=f,
                                                                       op0=ALU.mult, op1=ALU.add),
                     reads=[bF[j], bA[j]], writes=[bA[j]])
                for q in range(4):
                    S.op("dve", lambda e, a=a, q=q, s=st[j]: e.bn_stats(out=s[:, q, :], in_=a[:, q * 512:(q + 1) * 512]),
                         reads=[bA[j]], writes=[bs[j]])
                S.op("dve", lambda e, s=st[j], m=mv[j]: e.bn_aggr(out=m, in_=s), reads=[bs[j]], writes=[bs[j]])
                S.op("act", lambda e, m=mv[j], d=sd[j]: e.activation(out=d, in_=m[:, 1:2], func=AF.Sqrt, bias=self.eps_ap, scale=1.0),
                     reads=[bs[j], self.buf("eps")], writes=[bs[j]])
                S.op("dve", lambda e, d=sd[j], r=rs[j]: e.reciprocal(out=r, in_=d), reads=[bs[j]], writes=[bs[j]])
                S.op("dve", lambda e, a=a, m=mv[j]: e.scalar_tensor_tensor(out=a, in0=a, scalar=m[:, 0:1], in1=Gb,
                                                                            op0=ALU.subtract, op1=ALU.mult),
                     reads=[bs[j], bA[j], bg], writes=[bA[j]])
                S.op("dve", lambda e, a=a, r=rs[j]: e.scalar_tensor_tensor(out=a, in0=a, scalar=r, in1=Bb,
                                                                            op0=ALU.mult, op1=ALU.add),
                     reads=[bs[j], bA[j], bg], writes=[bA[j]])
                if mode == "final":
                    if i < 16:
                        S.op("act", lambda e, a=a, rows=rows: e.dma_start(out=self.out[rows, :], in_=a),
                             reads=[bA[j]], writes=[self.buf("out%d" % i)], dma=True)
                    continue
                S.op("act", lambda e, a=a, rows=rows: e.dma_start(out=self.hres[rows, :], in_=a),
                     reads=[bA[j]], writes=[hb[i]], dma=True)
            S.op("act", lambda e, a=a, yb=yb: e.activation(out=yb, in_=a, func=AF.Copy), reads=[bA[j]], writes=[bY[j]])
            for half in range(2):
                bank = self.pbank[(2 * i + half) % 8]
                pv = self.PS[:, (2 * i + half) % 8, :].bitcast(BF16).rearrange("p (a b) -> p a b", a=8)

                def tr(e, yb=yb, pv=pv, half=half):
                    for q in range(8):
                        kc = half * 8 + q
                        ins = e.transpose(out=pv[:, q, :], in_=yb[:, kc * 128:(kc + 1) * 128], identity=self.identb)
                    return ins
                S.op("pe", tr, reads=[bY[j], self.buf("identb")], writes=[bank])
                eng = "act"
                if eng == "dve":
                    S.op("dve", lambda e, ht=ht, pv=pv, half=half: e.tensor_copy(ht[:, half * 8:(half + 1) * 8, :], pv),
                         reads=[bank], writes=[bH[j]])
                else:
                    S.op("act", lambda e, ht=ht, pv=pv, half=half: e.activation(out=ht[:, half * 8:(half + 1) * 8, :], in_=pv, func=AF.Copy),
                         reads=[bank], writes=[bH[j]])
            S.op("act", lambda e, ht=ht, i=i: e.dma_start(out=self.hT[i], in_=ht.rearrange("p a b -> p (a b)")),
                 reads=[bH[j]], writes=[htb[i]], dma=True)

    def ffn_pass(self, XT, xbs, nt, wgu, wdn, sink, R):
        S = self.S
        GT, gtb = R["GT"], R["gtb"]
        groups = [(0, min(3, nt))] + ([(3, nt)] if nt > 3 else [])
        wguv = wgu.rearrange("(kc p) f -> p kc f", p=128)
        wdnv = wdn.rearrange("(hc p) f -> p hc f", p=128)
        ctr = R["ctr"]
        tiles = []
        for hp in range(FF // 256):
            tiles.append((wguv[:, :, hp * 256:(hp + 1) * 256], (KC, 256)))
            tiles.append((wguv[:, :, FF + hp * 256:FF + (hp + 1) * 256], (KC, 256)))
        for fp in range(D // 256):
            for q in range(4):
                tiles.append((wdnv[:, q * 14:(q + 1) * 14, fp * 256:(fp + 1) * 256], (14, 256)))
        issued = [0]
        handles = {}

        def load_cast(j):
            src_ap, shape3 = tiles[j]
            k = ctr[0] % 2
            ctr[0] += 1
            stg = R["stg"][k][:, 0:shape3[0] * shape3[1]].rearrange("p (a b) -> p a b", a=shape3[0])
            sbf = R["stgb"][k]
            k2 = ctr[1] % 3
            ctr[1] += 1
            wb = R["wbf"][k2][:, 0:shape3[0] * shape3[1]].rearrange("p (a b) -> p a b", a=shape3[0])
            wbb = R["wbfb"][k2]
            S.op("sp", lambda e: e.dma_start(out=stg, in_=src_ap), writes=[sbf], dma=True)
            if j % 2 == 0:
                S.op("act", lambda e: e.activation(out=wb, in_=stg, func=AF.Copy), reads=[sbf], writes=[wbb])
            else:
                S.op("dve", lambda e: e.tensor_copy(wb, stg), reads=[sbf], writes=[wbb])
            handles[j] = (wb, wbb)

        def get(j):
            while issued[0] <= min(j + 2, len(tiles) - 1):
                load_cast(issued[0])
                issued[0] += 1
            return handles.pop(j)

        def mm(e, w, p, t0, t1, h2):
            for kc in range(KC):
                ins = e.matmul(p, w[:, kc, h2 * 128:(h2 + 1) * 128], XT[:, t0:t1, kc, :],
                               start=(kc == 0), stop=(kc == KC - 1))
            return ins
        for hp in range(FF // 256):
            for part in range(2):
                w, wbb = get(hp * 2 + part)
                for h2 in range(2):
                    for gi, (t0, t1) in enumerate(groups):
                        n = (t1 - t0) * 128
                        bi = part * 4 + h2 * 2 + gi
                        p = self.PS[:, bi, 0:n]
                        S.op("pe", lambda e, w=w, p=p, t0=t0, t1=t1, h2=h2: mm(e, w, p, t0, t1, h2),
                             reads=[wbb] + xbs[t0:t1], writes=[self.pbank[bi]])
            for h2 in range(2):
                hc = hp * 2 + h2
                for gi, (t0, t1) in enumerate(groups):
                    n = (t1 - t0) * 128
                    bG = h2 * 2 + gi
                    bU = 4 + bG
                    pG = self.PS[:, bG, 0:n]
                    pU = self.PS[:, bU, 0:n]
                    k = ctr[2] % 2
                    ctr[2] += 1
                    sg = R["sil"][k][:, 0:n]
                    sgb = R["silb"][k]
                    S.op("act", lambda e, sg=sg, pG=pG: e.activation(out=sg, in_=pG, func=AF.Silu),
                         reads=[self.pbank[bG]], writes=[sgb])
                    S.op("dve", lambda e, sg=sg, pU=pU, hc=hc, t0=t0, t1=t1: e.tensor_tensor(
                        out=GT[:, hc, t0 * 128:t1 * 128], in0=sg, in1=pU, op=ALU.mult),
                        reads=[sgb, self.pbank[bU]], writes=[gtb])
        base = 2 * (FF // 256)
        for fp in range(D // 256):
            for q in range(4):
                w, wb_ = get(base + fp * 4 + q)
                for f2 in range(2):
                    for gi, (t0, t1) in enumerate(groups):
                        n = (t1 - t0) * 128
                        bi = f2 * 2 + gi
                        p = self.PS[:, bi, 0:n]

                        def mmb(e, w=w, p=p, t0=t0, t1=t1, f2=f2, q=q):
                            for h in range(14):
                                ins = e.matmul(p, w[:, h, f2 * 128:(f2 + 1) * 128], GT[:, q * 14 + h, t0 * 128:t1 * 128],
                                               start=(q == 0 and h == 0), stop=(q == 3 and h == 13))
                            return ins
                        S.op("pe", mmb, reads=[wb_, gtb], writes=[self.pbank[bi]])
            k = ctr[3] % 2
            ctr[3] += 1
            FT = R["FT"][k]
            ftb = R["ftb"][k]
            for f2 in range(2):
                for gi, (t0, t1) in enumerate(groups):
                    n = (t1 - t0) * 128
                    bi = f2 * 2 + gi
                    p = self.PS[:, bi, 0:n]
                    if (f2 + gi) % 2 == 0:
                        S.op("act", lambda e, p=p, f2=f2, t0=t0, t1=t1, FT=FT: e.activation(
                            out=FT[:, f2, t0 * 128:t1 * 128], in_=p, func=AF.Copy),
                            reads=[self.pbank[bi]], writes=[ftb])
                    else:
                        S.op("dve", lambda e, p=p, f2=f2, t0=t0, t1=t1, FT=FT: e.tensor_copy(FT[:, f2, t0 * 128:t1 * 128], p),
                             reads=[self.pbank[bi]], writes=[ftb])
            for t in range(nt):
                bi = 4 + (ctr[4] % 4)
                ctr[4] += 1
                pv = self.PS[:, bi, 0:256]

                def trb(e, pv=pv, FT=FT, t=t):
                    for f2 in range(2):
                        ins = e.transpose(out=pv[:, f2 * 128:(f2 + 1) * 128], in_=FT[:, f2, t * 128:(t + 1) * 128],
                                          identity=self.identf)
                    return ins
                S.op("pe", trb, reads=[ftb, self.buf("consts")], writes=[self.pbank[bi]])
                sink(t, fp, pv, self.pbank[bi])

    def ffn_resources(self, ntmax):
        R = {}
        R["GT"] = self.sb(56 * ntmax * 64, BF16, [56, ntmax * 128])
        R["gtb"] = self.buf("GT")
        R["stg"] = [self.sb(4096) for _ in range(2)]
        R["stgb"] = [self.buf("stg%d" % i) for i in range(2)]
        R["wbf"] = [self.sb(2048, BF16) for _ in range(3)]
        R["wbfb"] = [self.buf("wbf%d" % i) for i in range(3)]
        R["sil"] = [self.sb(384) for _ in range(2)]
        R["silb"] = [self.buf("sil%d" % i) for i in range(2)]
        R["FT"] = [self.sb(2 * ntmax * 128, F32, [2, ntmax * 128]) for _ in range(2)]
        R["ftb"] = [self.buf("FT%d" % i) for i in range(2)]
        R["OT"] = [self.sb(256) for _ in range(4)]
        R["otb"] = [self.buf("OT%d" % i) for i in range(4)]
        R["ctr"] = [0] * 8
        return R

    def ph_ffn(self, wi):
        S = self.S
        passes = [(0, 6), (6, 12), (12, 17)]
        R = self.ffn_resources(6)
        XT = self.sb(6 * KC * 64, BF16, [6, KC, 128])
        xbs = [self.buf("XT%d" % t) for t in range(6)]
        htb = [self.buf("hT%d" % i) for i in range(NT)]
        fb = [self.buf("fd%d" % i) for i in range(NT)]
        for (a, b) in passes:
            nt = b - a
            for t in range(nt):
                S.op("sp", lambda e, t=t, a=a: e.dma_start(out=XT[:, t, :, :].rearrange("p a b -> p (a b)"), in_=self.hT[a + t]),
                     reads=[htb[a + t]], writes=[xbs[t]], dma=True)

            def sink(t, fp, pv, bankbuf, a=a):
                k = R["ctr"][5] % 4
                R["ctr"][5] += 1
                ot, otb = R["OT"][k], R["otb"][k]
                S.op("dve", lambda e: e.tensor_copy(ot, pv), reads=[bankbuf], writes=[otb])
                S.op("sp", lambda e: e.dma_start(out=self.fd[(a + t) * 128:(a + t + 1) * 128, fp * 256:(fp + 1) * 256], in_=ot),
                     reads=[otb], writes=[fb[a + t]], dma=True)
            self.ffn_pass(XT, xbs, nt, self.w_gu[wi], self.w_dn[wi], sink, R)


    def nextbank(self):
        c = getattr(self, "_bankctr", 0)
        self._bankctr = c + 1
        return c % 8

    def ph_moe(self, wi):
        S = self.S
        htb = [self.buf("hT%d" % i) for i in range(NT)]
        hb = [self.buf("hres%d" % i) for i in range(NT)]
        fb = [self.buf("fd%d" % i) for i in range(NT)]
        WRf = self.sb(128, F32, [KC, 8])
        WRb = self.sb(64, BF16, [KC, 8])
        LG = self.sb(136, F32, [NT, 8])
        M8 = self.sb(136, F32, [NT, 8])
        MASK = self.sb(136, F32, [NT, 8])
        MASK1 = self.sb(136, F32, [NT, 8])
        GATE = self.sb(136, F32, [NT, 8])
        MASKb = self.sb(68, BF16, [NT, 8])
        GATEb = self.sb(68, BF16, [NT, 8])
        RANK = self.sb(136, F32, [NT, 8])
        D1, E1, DEN, G1, G2, DG = [self.sb(NT) for _ in range(6)]
        VAL = self.sb(1)
        ONESf = self.sb(128)
        TRIf = self.sb(128)
        ONESb = self.sb(64, BF16)
        TRIb = self.sb(64, BF16)
        IOTA = self.sb(CAP)
        bw, bl, bm = self.buf("moeW"), self.buf("moeLG"), self.buf("moeM")
        bc = self.buf("moeC")
        S.op("sp", lambda e: e.dma_start(out=WRf, in_=self.m_rt[wi].rearrange("(kc p) e -> p kc e", p=128)), writes=[bw], dma=True)
        S.op("act", lambda e: e.activation(out=WRb, in_=WRf, func=AF.Copy), reads=[bw], writes=[bw])
        S.op("pool", lambda e: e.memset(ONESf, 1.0), writes=[bc])
        S.op("pool", lambda e: e.affine_select(out=TRIf, in_=ONESf, pattern=[[1, 128]], base=-1, channel_multiplier=-1,
                                               compare_op=ALU.is_ge, fill=0.0), reads=[bc], writes=[bc])
        S.op("pool", lambda e: e.tensor_copy(ONESb, ONESf), reads=[bc], writes=[bc])
        S.op("pool", lambda e: e.tensor_copy(TRIb, TRIf), reads=[bc], writes=[bc])
        S.op("pool", lambda e: e.iota(IOTA, pattern=[[1, CAP]], base=0, channel_multiplier=0,
                                      allow_small_or_imprecise_dtypes=True), writes=[bc])
        S.op("pool", lambda e: e.memset(VAL, 0.0), writes=[bc])
        S.op("pool", lambda e: e.memset(VAL[0:NMETA, :], 1.0), reads=[bc], writes=[bc])
        HTi = [self.sb(1024, BF16, [KC, 128]) for _ in range(2)]
        hbuf = [self.buf("moeHT%d" % j) for j in range(2)]
        for i in range(NT):
            j = i % 2
            S.op("sp", lambda e, i=i, j=j: e.dma_start(out=HTi[j].rearrange("p a b -> p (a b)"), in_=self.hT[i]),
                 reads=[htb[i]], writes=[hbuf[j]], dma=True)
            bi = self.nextbank()
            ps = self.PS[:, bi, 0:8]

            def mml(e, j=j, ps=ps):
                for kc in range(KC):
                    ins = e.matmul(ps, HTi[j][:, kc, :], WRb[:, kc, :], start=(kc == 0), stop=(kc == KC - 1))
                return ins
            S.op("pe", mml, reads=[hbuf[j], bw], writes=[self.pbank[bi]])
            S.op("act", lambda e, i=i, ps=ps: e.activation(out=LG[:, i, :], in_=ps, func=AF.Copy),
                 reads=[self.pbank[bi]], writes=[bl])
        for i in range(NT):
            S.op("dve", lambda e, i=i: e.max(out=M8[:, i, :], in_=LG[:, i, :]), reads=[bl], writes=[bm])
        for i in range(NT):
            S.op("dve", lambda e, i=i: e.tensor_scalar(out=MASK[:, i, :], in0=LG[:, i, :], scalar1=M8[:, i, 1:2], scalar2=None,
                                                        op0=ALU.is_ge), reads=[bl, bm], writes=[self.buf("moeMASK")])
            S.op("dve", lambda e, i=i: e.tensor_scalar(out=MASK1[:, i, :], in0=LG[:, i, :], scalar1=M8[:, i, 0:1], scalar2=None,
                                                        op0=ALU.is_equal), reads=[bl, bm], writes=[self.buf("moeMASK1")])
        bk, bk1, bgt = self.buf("moeMASK"), self.buf("moeMASK1"), self.buf("moeG")
        S.op("dve", lambda e: e.tensor_scalar(out=MASK[:, NT - 1, :], in0=MASK[:, NT - 1, :], scalar1=VAL, scalar2=None, op0=ALU.mult),
             reads=[bk, bc], writes=[bk])
        S.op("dve", lambda e: e.tensor_tensor(out=D1, in0=M8[:, :, 1], in1=M8[:, :, 0], op=ALU.subtract), reads=[bm], writes=[bgt])
        S.op("act", lambda e: e.activation(out=E1, in_=D1, func=AF.Exp), reads=[bgt], writes=[bgt])
        S.op("dve", lambda e: e.tensor_scalar(out=DEN, in0=E1, scalar1=1.0, scalar2=None, op0=ALU.add), reads=[bgt], writes=[bgt])
        S.op("dve", lambda e: e.reciprocal(out=G1, in_=DEN), reads=[bgt], writes=[bgt])
        S.op("dve", lambda e: e.tensor_tensor(out=G2, in0=E1, in1=G1, op=ALU.mult), reads=[bgt], writes=[bgt])
        S.op("dve", lambda e: e.tensor_tensor(out=DG, in0=G1, in1=G2, op=ALU.subtract), reads=[bgt], writes=[bgt])
        bga = self.buf("moeGATE")
        for i in range(NT):
            S.op("dve", lambda e, i=i: e.tensor_scalar(out=GATE[:, i, :], in0=MASK[:, i, :], scalar1=G2[:, i:i + 1], scalar2=None,
                                                        op0=ALU.mult), reads=[bk, bgt], writes=[bga])
            S.op("dve", lambda e, i=i: e.scalar_tensor_tensor(out=GATE[:, i, :], in0=MASK1[:, i, :], scalar=DG[:, i:i + 1],
                                                               in1=GATE[:, i, :], op0=ALU.mult, op1=ALU.add),
                 reads=[bk1, bgt, bga], writes=[bga])
        S.op("act", lambda e: e.activation(out=MASKb, in_=MASK, func=AF.Copy), reads=[bk], writes=[self.buf("moeMASKb")])
        S.op("act", lambda e: e.activation(out=GATEb, in_=GATE, func=AF.Copy), reads=[bga], writes=[self.buf("moeGATEb")])
        bkb, bgb, brk = self.buf("moeMASKb"), self.buf("moeGATEb"), self.buf("moeRANK")
        for i in range(NT):
            bi = self.nextbank()
            ps = self.PS[:, bi, 0:8]

            def mmr(e, i=i, ps=ps):
                for j in range(i):
                    e.matmul(ps, ONESb, MASKb[:, j, :], start=(j == 0), stop=False)
                return e.matmul(ps, TRIb, MASKb[:, i, :], start=(i == 0), stop=True)
            S.op("pe", mmr, reads=[bkb, bc], writes=[self.pbank[bi]])
            S.op("act", lambda e, i=i, ps=ps: e.activation(out=RANK[:, i, :], in_=ps, func=AF.Copy),
                 reads=[self.pbank[bi]], writes=[brk])
        if self.cfg.get("dbg_route"):
            dbg = self.dout("dbg_route", [128, 4 * 136])
            for n_, t_ in enumerate((LG, MASK, GATE, RANK)):
                S.op("sp", lambda e, n_=n_, t_=t_: e.dma_start(out=dbg[:, n_ * 136:(n_ + 1) * 136], in_=t_.rearrange("p a b -> p (a b)")),
                     reads=[bl, bk, bga, brk], dma=True)
        base_after_route = self.sb_off
        HTOK = self.sb(NT * 1024, BF16, [NT, D])
        btok = self.buf("HTOK")
        ld = [self.sb(D) for _ in range(2)]
        ldb = [self.buf("moeLD%d" % j) for j in range(2)]
        for i in range(NT):
            j = i % 2
            S.op("sp", lambda e, i=i, j=j: e.dma_start(out=ld[j], in_=self.hres[i * 128:(i + 1) * 128, :]),
                 reads=[hb[i]], writes=[ldb[j]], dma=True)
            if i % 2 == 0:
                S.op("act", lambda e, i=i, j=j: e.activation(out=HTOK[:, i, :], in_=ld[j], func=AF.Copy), reads=[ldb[j]], writes=[btok])
            else:
                S.op("pool", lambda e, i=i, j=j: e.tensor_copy(HTOK[:, i, :], ld[j]), reads=[ldb[j]], writes=[btok])
        SEL = [self.sb(NT * CAP // 2, BF16, [NT, CAP]) for _ in range(2)]
        selb = [self.buf("SEL%d" % j) for j in range(2)]
        XE = [self.sb(CAPT * KC * 64, BF16, [CAPT, KC, 128]) for _ in range(2)]
        xeb = [self.buf("XE%d" % j) for j in range(2)]
        STG = [self.sb(CAPT * 64, BF16, [CAPT, 128]) for _ in range(3)]
        stgb = [self.buf("STG%d" % j) for j in range(3)]
        gsb = self.buf("gs")
        groups = [(0, 3), (3, CAPT)]
        sctr = 0
        for ex in range(NE):
            j = ex % 2
            sel = SEL[j]
            for i in range(NT):
                S.op("dve", lambda e, i=i, sel=sel, ex=ex: e.tensor_scalar(
                    out=sel[:, i, :], in0=IOTA, scalar1=RANK[:, i, ex:ex + 1], scalar2=MASK[:, i, ex:ex + 1],
                    op0=ALU.is_equal, op1=ALU.mult), reads=[brk, bk, bc], writes=[selb[j]])
            xe = XE[j]
            for kc in range(KC):
                for (t0, t1) in groups:
                    n = (t1 - t0) * 128
                    bi = self.nextbank()
                    ps = self.PS[:, bi, 0:n]

                    def mmd(e, kc=kc, t0=t0, t1=t1, ps=ps, sel=sel):
                        for i in range(t0, NT):
                            ins = e.matmul(ps, HTOK[:, i, kc * 128:(kc + 1) * 128], sel[:, i, t0 * 128:t1 * 128],
                                           start=(i == t0), stop=(i == NT - 1))
                        return ins
                    S.op("pe", mmd, reads=[btok, selb[j]], writes=[self.pbank[bi]])
                    dst = xe[:, t0:t1, kc, :]
                    psv = ps.rearrange("p (a b) -> p a b", b=128)
                    if kc % 2 == 0:
                        S.op("act", lambda e, dst=dst, psv=psv: e.activation(out=dst, in_=psv, func=AF.Copy),
                             reads=[self.pbank[bi]], writes=[xeb[j]])
                    else:
                        S.op("dve", lambda e, dst=dst, psv=psv: e.tensor_copy(dst, psv), reads=[self.pbank[bi]], writes=[xeb[j]])
            for t in range(CAPT):
                S.op("sp", lambda e, t=t, ex=ex, xe=xe: e.dma_start(out=self.XS[ex, t], in_=xe[:, t, :, :].rearrange("p a b -> p (a b)")),
                     reads=[xeb[j]], writes=[self.buf("XS%d" % ex)], dma=True)
            for sc in range(CAPT):
                bi = self.nextbank()
                ps = self.PS[:, bi, 0:1]

                def mmg(e, sc=sc, ps=ps, sel=sel, ex=ex):
                    for i in range(sc, NT):
                        ins = e.matmul(ps, sel[:, i, sc * 128:(sc + 1) * 128], GATEb[:, i, ex:ex + 1],
                                       start=(i == sc), stop=(i == NT - 1))
                    return ins
                S.op("pe", mmg, reads=[selb[j], bgb], writes=[self.pbank[bi]])
                S.op("act", lambda e, ps=ps, ex=ex, sc=sc: e.activation(out=self.gs[:, ex * CAPT + sc:ex * CAPT + sc + 1], in_=ps, func=AF.Copy),
                     reads=[self.pbank[bi]], writes=[gsb])
            for i in range(NT):
                bi = self.nextbank()
                pv = self.PS[:, bi, 0:CAPT * 64].bitcast(BF16).rearrange("p (a b) -> p a b", a=CAPT)

                def trs(e, i=i, pv=pv, sel=sel):
                    for sc in range(CAPT):
                        ins = e.transpose(out=pv[:, sc, :], in_=sel[:, i, sc * 128:(sc + 1) * 128], identity=self.identb)
                    return ins
                S.op("pe", trs, reads=[selb[j], self.buf("identb")], writes=[self.pbank[bi]])
                k = sctr % 3
                sctr += 1
                stg = STG[k]
                if i % 2 == 0:
                    S.op("dve", lambda e, stg=stg, pv=pv: e.tensor_copy(stg, pv), reads=[self.pbank[bi]], writes=[stgb[k]])
                else:
                    S.op("act", lambda e, stg=stg, pv=pv: e.activation(out=stg, in_=pv, func=AF.Copy), reads=[self.pbank[bi]], writes=[stgb[k]])
                S.op("sp", lambda e, stg=stg, i=i, ex=ex: e.dma_start(
                    out=self.SELT[i, :, ex * CAP:(ex + 1) * CAP], in_=stg.rearrange("p a b -> p (a b)")),
                    reads=[stgb[k]], writes=[self.buf("SELT%d" % i)], dma=True)
        S.barrier()
        self.sb_reset()
        R = self.ffn_resources(CAPT)
        XT = self.sb(CAPT * KC * 64, BF16, [CAPT, KC, 128])
        xbs = [self.buf("XT%d" % t) for t in range(CAPT)]
        OTb = [self.sb(128, BF16) for _ in range(4)]
        fab = self.buf("FA")
        for ex in range(NE):
            for t in range(CAPT):
                S.op("sp", lambda e, t=t, ex=ex: e.dma_start(out=XT[:, t, :, :].rearrange("p a b -> p (a b)"), in_=self.XS[ex, t]),
                     reads=[self.buf("XS%d" % ex)], writes=[xbs[t]], dma=True)

            def sink(t, fp, pv, bankbuf, ex=ex):
                k = R["ctr"][5] % 4
                R["ctr"][5] += 1
                ot, otb = OTb[k], R["otb"][k]
                g = self.gs[:, ex * CAPT + t:ex * CAPT + t + 1]
                S.op("dve", lambda e: e.tensor_scalar(out=ot, in0=pv, scalar1=g, scalar2=None, op0=ALU.mult),
                     reads=[bankbuf, gsb], writes=[otb])
                r0 = ex * CAP + t * 128
                S.op("sp", lambda e: e.dma_start(out=self.FA[r0:r0 + 128, fp * 256:(fp + 1) * 256], in_=ot),
                     reads=[otb], writes=[fab], dma=True)
            self.ffn_pass(XT, xbs, CAPT, self.m_gu[wi, ex], self.m_dn[wi, ex], sink, R)
        S.barrier()
        self.sb_reset()
        NC_ = NE * CAPT
        FAh = self.sb(NC_ * 512, BF16, [NC_, 1024])
        fahb = self.buf("FAh")
        STi = [self.sb(NC_ * 64, BF16, [NC_, 128]) for _ in range(2)]
        stib = [self.buf("STi%d" % j) for j in range(2)]
        OC = [self.sb(1024) for _ in range(2)]
        ocb = [self.buf("OC%d" % j) for j in range(2)]
        FAv = self.FA.rearrange("(c p) f -> p c f", p=128)
        cc = 0
        for h in range(2):
            for ex in range(NE):
                S.op("sp", lambda e, ex=ex, h=h: e.dma_start(out=FAh[:, ex * CAPT:(ex + 1) * CAPT, :],
                                                              in_=FAv[:, ex * CAPT:(ex + 1) * CAPT, h * 1024:(h + 1) * 1024]),
                     reads=[fab], writes=[fahb], dma=True)
            for i in range(NT):
                j = cc % 2
                cc += 1
                S.op("sp", lambda e, i=i, j=j: e.dma_start(out=STi[j].rearrange("p a b -> p (a b)"), in_=self.SELT[i]),
                     reads=[self.buf("SELT%d" % i)], writes=[stib[j]], dma=True)
                for nch in range(2):
                    bi = self.nextbank()
                    ps = self.PS[:, bi, :]

                    def mmc(e, j=j, nch=nch, ps=ps, i=i):
                        cl = [c for c in range(NC_) if (c % CAPT) <= i]
                        for n_, c in enumerate(cl):
                            ins = e.matmul(ps, STi[j][:, c, :], FAh[:, c, nch * 512:(nch + 1) * 512],
                                           start=(n_ == 0), stop=(n_ == len(cl) - 1))
                        return ins
                    S.op("pe", mmc, reads=[stib[j], fahb], writes=[self.pbank[bi]])
                    if nch == 0:
                        S.op("act", lambda e, j=j, ps=ps: e.activation(out=OC[j][:, 0:512], in_=ps, func=AF.Copy),
                             reads=[self.pbank[bi]], writes=[ocb[j]])
                    else:
                        S.op("dve", lambda e, j=j, ps=ps: e.tensor_copy(OC[j][:, 512:1024], ps),
                             reads=[self.pbank[bi]], writes=[ocb[j]])
                S.op("act", lambda e, i=i, j=j, h=h: e.dma_start(out=self.fd[i * 128:(i + 1) * 128, h * 1024:(h + 1) * 1024], in_=OC[j]),
                     reads=[ocb[j]], writes=[fb[i]], dma=True)


    def ph_kv(self):
        S = self.S
        htb = [self.buf("hT%d" % i) for i in range(NT)]
        wkv = self.din("kv_w_shared", [D, 512])
        self.kT2 = self.dscr("kT2", [128, 4 * TP], BF16)
        self.Vaug = self.dscr("Vaug", [NT, 128, 4 * 66], BF16)
        WKf = self.sb(KC * 512, F32, [KC, 512])
        WK2 = self.sb(KC * 4 * 64, BF16, [KC, 4, 128])
        WVb = self.sb(KC * 128, BF16, [KC, 256])
        XTall = self.sb(NT * 1024, BF16, [NT, KC, 128])
        KT = self.sb(4 * TP // 2, BF16, [4, TP])
        VA = [self.sb(4 * 33, BF16, [4, 66]) for _ in range(2)]
        bw, bw2, bx, bkt = self.buf("kvW"), self.buf("kvW2"), self.buf("kvX"), self.buf("kvKT")
        vab = [self.buf("kvVA%d" % j) for j in range(2)]
        S.op("sp", lambda e: e.dma_start(out=WKf, in_=wkv.rearrange("(kc p) f -> p kc f", p=128)), writes=[bw], dma=True)
        for g in range(4):
            for hf in range(2):
                eng = "act" if hf == 0 else "pool"
                if eng == "act":
                    S.op("act", lambda e, g=g, hf=hf: e.activation(out=WK2[:, :, g, hf * 64:(hf + 1) * 64], in_=WKf[:, :, g * 64:(g + 1) * 64], func=AF.Copy),
                         reads=[bw], writes=[bw2])
                else:
                    S.op("pool", lambda e, g=g, hf=hf: e.tensor_copy(WK2[:, :, g, hf * 64:(hf + 1) * 64], WKf[:, :, g * 64:(g + 1) * 64]),
                         reads=[bw], writes=[bw2])
        S.op("act", lambda e: e.activation(out=WVb, in_=WKf[:, :, 256:512], func=AF.Copy), reads=[bw], writes=[bw2])
        for i in range(NT):
            S.op("sp", lambda e, i=i: e.dma_start(out=XTall[:, i, :, :].rearrange("p a b -> p (a b)"), in_=self.hT[i]),
                 reads=[htb[i]], writes=[bx], dma=True)
        tg = [(0, 4), (4, 8), (8, 12), (12, 16), (16, 17)]
        for (t0, t1) in tg:
            n = (t1 - t0) * 128
            for g in range(4):
                bi = self.nextbank()
                ps = self.PS[:, bi, 0:n]

                def mmk(e, g=g, t0=t0, t1=t1, ps=ps):
                    for kc in range(KC):
                        ins = e.matmul(ps, WK2[:, kc, g, :], XTall[:, t0:t1, kc, :], start=(kc == 0), stop=(kc == KC - 1))
                    return ins
                S.op("pe", mmk, reads=[bw2, bx], writes=[self.pbank[bi]])
                if g % 2 == 0:
                    S.op("act", lambda e, g=g, t0=t0, t1=t1, ps=ps: e.activation(out=KT[:, g, t0 * 128:t1 * 128], in_=ps, func=AF.Copy),
                         reads=[self.pbank[bi]], writes=[bkt])
                else:
                    S.op("dve", lambda e, g=g, t0=t0, t1=t1, ps=ps: e.tensor_copy(KT[:, g, t0 * 128:t1 * 128], ps),
                         reads=[self.pbank[bi]], writes=[bkt])
        S.op("sp", lambda e: e.dma_start(out=self.kT2, in_=KT.rearrange("p a b -> p (a b)")), reads=[bkt], writes=[self.buf("kT2d")], dma=True)
        for i in range(NT):
            j = i % 2
            bi = self.nextbank()
            ps = self.PS[:, bi, 0:256]

            def mmv(e, i=i, ps=ps):
                for kc in range(KC):
                    ins = e.matmul(ps, XTall[:, i, kc, :], WVb[:, kc, :], start=(kc == 0), stop=(kc == KC - 1))
                return ins
            S.op("pe", mmv, reads=[bw2, bx], writes=[self.pbank[bi]])
            S.op("pool", lambda e, j=j: e.memset(VA[j][:, :, 64:66], 1.0), writes=[vab[j]])
            S.op("act", lambda e, j=j, ps=ps: e.activation(out=VA[j][:, :, 0:64], in_=ps.rearrange("p (a b) -> p a b", a=4), func=AF.Copy),
                 reads=[self.pbank[bi], vab[j]], writes=[vab[j]])
            S.op("sp", lambda e, i=i, j=j: e.dma_start(out=self.Vaug[i], in_=VA[j].rearrange("p a b -> p (a b)")),
                 reads=[vab[j]], writes=[self.buf("Vaugd")], dma=True)

    def ph_bias(self):
        S = self.S
        tab = self.din("rel_bias_table", [32, 32])
        ohs = {"cur": (128, 128), "prev": (128, 128), "meta0": (128, 16), "metac": (128, 16), "mm": (16, 16)}
        self.bias_d = {}
        T33 = self.sb(32)
        bt = self.buf("biasT")
        S.op("pool", lambda e: e.memset(T33, -30000.0), writes=[bt])
        S.op("sp", lambda e: e.dma_start(out=T33[0:32, :], in_=tab), reads=[bt], writes=[bt], dma=True)
        OH = [self.sb(128 * 128, F32, [128, 128]) for _ in range(2)]
        ohb = [self.buf("OH%d" % j) for j in range(2)]
        BT = [self.sb(32 * 128, F32, [32, 128]) for _ in range(2)]
        btb = [self.buf("BT%d" % j) for j in range(2)]
        for n_, (name, (nq, ns)) in enumerate(ohs.items()):
            j = n_ % 2
            src = self.din("oh_" + name, [33, nq * ns])
            dst = self.dscr("bias_" + name, [128, 32 * 128])
            self.bias_d[name] = dst
            oh = OH[j][:, 0:nq, 0:ns] if False else OH[j].rearrange("p a b -> p (a b)")[:, 0:nq * ns].rearrange("p (a b) -> p a b", a=nq)
            S.op("sp", lambda e, oh=oh, src=src: e.dma_start(out=oh[0:33].rearrange("p a b -> p (a b)"), in_=src), writes=[ohb[j]], dma=True)
            btile = BT[j]
            S.op("pool", lambda e, btile=btile: e.memset(btile, 0.0), writes=[btb[j]])
            for q0 in range(0, nq, 16):
                bi = self.nextbank()
                ps = self.PS[:, bi, :].rearrange("p (q h) -> p q h", q=16)

                def mmb(e, q0=q0, ps=ps, oh=oh, ns=ns):
                    for q in range(16):
                        ins = e.matmul(ps[0:ns, q, :], oh[0:33, q0 + q, :], T33[0:33, :], start=True, stop=True)
                    return ins
                S.op("pe", mmb, reads=[ohb[j], bt], writes=[self.pbank[bi]])
                S.op("dve", lambda e, q0=q0, ps=ps, btile=btile, ns=ns: e.tensor_copy(
                    btile[0:ns, :, q0:q0 + 16], ps[0:ns, :, :].rearrange("p q h -> p h q")),
                    reads=[self.pbank[bi]], writes=[btb[j]])
            S.op("sp", lambda e, dst=dst, btile=btile: e.dma_start(out=dst, in_=btile.rearrange("p a b -> p (a b)")),
                 reads=[btb[j]], writes=[self.buf("biasd")], dma=True)

    def ph_swa(self, jb):
        S = self.S
        htb = [self.buf("hT%d" % i) for i in range(NT)]
        fb = [self.buf("fd%d" % i) for i in range(NT)]
        wq = self.w_q[jb].rearrange("(kc p) f -> p kc f", p=128)
        wo = self.w_o[jb].rearrange("(kc p) f -> p kc f", p=128)
        QA = self.sb(KC * TP // 2, BF16, [KC, TP])
        qab = [self.buf("QA%d" % i) for i in range(NT)]
        base1 = self.sb_off
        XTall = self.sb(NT * 1024, BF16, [NT, KC, 128])
        bx = self.buf("swaX")
        stg = [self.sb(4096, F32, [KC, 256]) for _ in range(2)]
        stgb = [self.buf("swaStg%d" % j) for j in range(2)]
        wbf = [self.sb(2048, BF16, [KC, 256]) for _ in range(3)]
        wbfb = [self.buf("swaW%d" % j) for j in range(3)]
        for i in range(NT):
            S.op("sp", lambda e, i=i: e.dma_start(out=XTall[:, i, :, :].rearrange("p a b -> p (a b)"), in_=self.hT[i]),
                 reads=[htb[i]], writes=[bx], dma=True)
        tg = [(0, 4), (4, 8), (8, 12), (12, 16), (16, 17)]
        for cp in range(8):
            j, j2 = cp % 2, cp % 3
            S.op("sp", lambda e, cp=cp, j=j: e.dma_start(out=stg[j], in_=wq[:, :, cp * 256:(cp + 1) * 256]), writes=[stgb[j]], dma=True)
            if cp % 2 == 0:
                S.op("act", lambda e, j=j, j2=j2: e.activation(out=wbf[j2], in_=stg[j], func=AF.Copy), reads=[stgb[j]], writes=[wbfb[j2]])
            else:
                S.op("dve", lambda e, j=j, j2=j2: e.tensor_copy(wbf[j2], stg[j]), reads=[stgb[j]], writes=[wbfb[j2]])
            for c2 in range(2):
                c = cp * 2 + c2
                for (t0, t1) in tg:
                    n = (t1 - t0) * 128
                    bi = self.nextbank()
                    ps = self.PS[:, bi, 0:n]

                    def mmq(e, j2=j2, c2=c2, t0=t0, t1=t1, ps=ps):
                        for kc in range(KC):
                            ins = e.matmul(ps, wbf[j2][:, kc, c2 * 128:(c2 + 1) * 128], XTall[:, t0:t1, kc, :],
                                           start=(kc == 0), stop=(kc == KC - 1))
                        return ins
                    S.op("pe", mmq, reads=[wbfb[j2], bx], writes=[self.pbank[bi]])
                    if (t0 // 4) % 2 == 0:
                        S.op("act", lambda e, c=c, t0=t0, t1=t1, ps=ps: e.activation(out=QA[:, c, t0 * 128:t1 * 128], in_=ps, func=AF.Copy),
                             reads=[self.pbank[bi]], writes=qab[t0:t1])
                    else:
                        S.op("dve", lambda e, c=c, t0=t0, t1=t1, ps=ps: e.tensor_copy(QA[:, c, t0 * 128:t1 * 128], ps),
                             reads=[self.pbank[bi]], writes=qab[t0:t1])
        S.barrier()
        self.sb_reset(base1)
        KTh = [self.sb(4 * TP // 2, BF16, [4, TP]) for _ in range(2)]
        VA = self.sb(NT * 4 * 33, BF16, [NT, 4, 66])
        Bc = self.sb(4096, F32, [32, 128])
        Bp = self.sb(4096, F32, [32, 128])
        Bm0 = self.sb(4096, F32, [32, 128])
        Bmc = self.sb(4096, F32, [32, 128])
        Bmm = self.sb(512, F32, [32, 16])
        SK = self.sb(32)
        SKe = self.sb(32)
        bkv, bbias, bsk = self.buf("swaKV"), self.buf("swaBias"), self.buf("swaSK")
        for hf_ in range(2):
            S.op("sp", lambda e, hf_=hf_: e.dma_start(out=KTh[hf_].rearrange("p a b -> p (a b)"), in_=self.kT2),
                 reads=[self.buf("kT2d")], writes=[bkv], dma=True)
        S.op("pool", lambda e: e.memset(KTh[0][64:128], 0.0), reads=[bkv], writes=[bkv])
        S.op("pool", lambda e: e.memset(KTh[1][0:64], 0.0), reads=[bkv], writes=[bkv])
        S.op("sp", lambda e: e.dma_start(out=VA.rearrange("p t a b -> p t (a b)"), in_=self.Vaug.rearrange("t p f -> p t f")),
             reads=[self.buf("Vaugd")], writes=[bkv], dma=True)
        for t_, nm in ((Bc, "cur"), (Bp, "prev"), (Bm0, "meta0"), (Bmc, "metac")):
            S.op("sp", lambda e, t_=t_, nm=nm: e.dma_start(out=t_.rearrange("p a b -> p (a b)"), in_=self.bias_d[nm]),
                 reads=[self.buf("biasd")], writes=[bbias], dma=True)
        S.op("sp", lambda e: e.dma_start(out=Bmm, in_=self.bias_d["mm"].rearrange("p (h q) -> p h q", h=32)[:, :, 0:16]),
             reads=[self.buf("biasd")], writes=[bbias], dma=True)
        S.op("sp", lambda e: e.dma_start(out=SK, in_=self.sinks[jb:jb + 1, :].partition_broadcast(128)), writes=[bsk], dma=True)
        S.op("act", lambda e: e.activation(out=SKe, in_=SK, func=AF.Exp), reads=[bsk], writes=[bsk])
        TMP = [self.sb(512) for _ in range(4)]
        tmpb = [self.buf("swaTMP%d" % j) for j in range(4)]
        EX = [self.sb(256, BF16) for _ in range(7)]
        exb = [self.buf("swaEX%d" % j) for j in range(7)]
        DEN = [self.sb(4) for _ in range(4)]
        denb = [self.buf("swaDEN%d" % j) for j in range(4)]
        AO = [self.sb(1024, BF16) for _ in range(2)]
        aob = [self.buf("swaAO%d" % j) for j in range(2)]
        cnt = [0, 0, 0]
        blocks = list(range(16)) + [16]
        pending = [None]
        for blk in blocks:
            meta_q = (blk == 16)
            nq = NMETA if meta_q else 128
            qs = slice(blk * 128, blk * 128 + nq)
            ao = AO[blk % 2]
            aobuf = aob[blk % 2]
            if meta_q:
                S.op("pool", lambda e, ao=ao: e.memset(ao, 0.0), writes=[aobuf])
            for hg in range(8):
                g = hg // 2
                pieces = []
                if meta_q:
                    pieces.append((slice(2048, 2064), 16, Bmm, 16))
                else:
                    pieces.append((slice(blk * 128, blk * 128 + 128), blk, Bc, 128))
                    if blk > 0:
                        pieces.append((slice((blk - 1) * 128, blk * 128), blk - 1, Bp, 128))
                    pieces.append((slice(2048, 2064), 16, Bm0 if blk == 0 else Bmc, 16))
                exs = []
                for (ks, vt, btile, ns) in pieces:
                    bi = self.nextbank()
                    ps = self.PS[:, bi, :].rearrange("p (h q) -> p h q", h=4)

                    def mms(e, ks=ks, ns=ns, ps=ps, hg=hg, g=g, qs=qs, nq=nq):
                        for hh in range(4):
                            h = hg * 4 + hh
                            kc, hf = h // 2, h % 2
                            ins = e.matmul(ps[0:ns, hh, 0:nq], KTh[hf][:, g, ks], QA[:, kc, qs], start=True, stop=True)
                        return ins
                    S.op("pe", mms, reads=[bkv, qab[blk]], writes=[self.pbank[bi]])
                    k = cnt[0] % 4
                    cnt[0] += 1
                    tmp = TMP[k].rearrange("p (h q) -> p h q", h=4)
                    S.op("dve", lambda e, tmp=tmp, ps=ps, ns=ns, nq=nq, btile=btile, hg=hg: e.scalar_tensor_tensor(
                        out=tmp[0:ns, :, 0:nq], in0=ps[0:ns, :, 0:nq], scalar=0.125, in1=btile[0:ns, hg * 4:(hg + 1) * 4, 0:nq],
                        op0=ALU.mult, op1=ALU.add), reads=[self.pbank[bi], bbias], writes=[tmpb[k]])
                    k2 = cnt[1] % 7
                    cnt[1] += 1
                    ex = EX[k2].rearrange("p (h q) -> p h q", h=4)
                    S.op("act", lambda e, ex=ex, tmp=tmp, ns=ns, nq=nq: e.activation(out=ex[0:ns, :, 0:nq], in_=tmp[0:ns, :, 0:nq], func=AF.Exp),
                         reads=[tmpb[k]], writes=[exb[k2]])
                    exs.append((ex, exb[k2], vt, ns))
                bi = self.nextbank()
                po = self.PS[:, bi, :].rearrange("p (h q) -> p h q", h=4)

                def mmo(e, exs=exs, po=po, g=g, nq=nq):
                    for hh in range(4):
                        for n_, (ex, _, vt, ns) in enumerate(exs):
                            ins = e.matmul(po[0:nq, hh, 0:65], ex[0:ns, hh, 0:nq], VA[0:ns, vt, g, 0:65],
                                           start=(n_ == 0), stop=(n_ == len(exs) - 1))
                    return ins
                S.op("pe", mmo, reads=[bkv] + [x[1] for x in exs], writes=[self.pbank[bi]])
                def back(bi=bi, po=po, hg=hg, nq=nq, ao=ao, aobuf=aobuf):
                    k3 = cnt[2] % 4
                    cnt[2] += 1
                    den = DEN[k3]
                    S.op("dve", lambda e, den=den, po=po, hg=hg, nq=nq: e.tensor_tensor(
                        out=den[0:nq, :], in0=po[0:nq, :, 64], in1=SKe[0:nq, hg * 4:(hg + 1) * 4], op=ALU.add),
                        reads=[self.pbank[bi], bsk], writes=[denb[k3]])
                    S.op("dve", lambda e, den=den, nq=nq: e.reciprocal(out=den[0:nq, :], in_=den[0:nq, :]), reads=[denb[k3]], writes=[denb[k3]])
                    for hh in range(4):
                        h = hg * 4 + hh
                        if hh % 2 == 0:
                            S.op("act", lambda e, ao=ao, po=po, den=den, h=h, hh=hh, nq=nq: e.activation(
                                out=ao[0:nq, h * 64:(h + 1) * 64], in_=po[0:nq, hh, 0:64], func=AF.Copy, scale=den[0:nq, hh:hh + 1]),
                                reads=[self.pbank[bi], denb[k3]], writes=[aobuf])
                        else:
                            S.op("dve", lambda e, ao=ao, po=po, den=den, h=h, hh=hh, nq=nq: e.tensor_scalar(
                                out=ao[0:nq, h * 64:(h + 1) * 64], in0=po[0:nq, hh, 0:64], scalar1=den[0:nq, hh:hh + 1], scalar2=None, op0=ALU.mult),
                                reads=[self.pbank[bi], denb[k3]], writes=[aobuf])
                if pending[0] is not None:
                    pending[0]()
                pending[0] = back
            if pending[0] is not None:
                pending[0]()
                pending[0] = None
            for half in range(2):
                bi = self.nextbank()
                pv = self.PS[:, bi, :].bitcast(BF16).rearrange("p (a b) -> p a b", a=8)

                def tr(e, ao=ao, pv=pv, half=half):
                    for q in range(8):
                        kc = half * 8 + q
                        ins = e.transpose(out=pv[:, q, :], in_=ao[:, kc * 128:(kc + 1) * 128], identity=self.identb)
                    return ins
                S.op("pe", tr, reads=[aobuf, self.buf("identb")], writes=[self.pbank[bi]])
                dst = QA[:, half * 8:(half + 1) * 8, blk * 128:(blk + 1) * 128]
                if half == 0:
                    S.op("dve", lambda e, dst=dst, pv=pv: e.tensor_copy(dst, pv), reads=[self.pbank[bi]], writes=[qab[blk]])
                else:
                    S.op("act", lambda e, dst=dst, pv=pv: e.activation(out=dst, in_=pv, func=AF.Copy), reads=[self.pbank[bi]], writes=[qab[blk]])
        self.outproj(QA, qab, wo, base1)

    def outproj(self, QA, qab, wo, base1):
        S = self.S
        fb = [self.buf("fd%d" % i) for i in range(NT)]
        S.barrier()
        self.sb_reset(base1)
        WO = self.sb(KC * 1024, BF16, [KC, D])
        wob = self.buf("swaWO")
        sgO = [self.sb(4096, F32, [KC, 256]) for _ in range(2)]
        sgO_b = [self.buf("swaStgO%d" % j) for j in range(2)]
        OT = [self.sb(D) for _ in range(2)]
        otb = [self.buf("swaOT%d" % j) for j in range(2)]
        for cp in range(8):
            j = cp % 2
            S.op("sp", lambda e, cp=cp, j=j: e.dma_start(out=sgO[j], in_=wo[:, :, cp * 256:(cp + 1) * 256]), writes=[sgO_b[j]], dma=True)
            if cp % 2 == 0:
                S.op("dve", lambda e, j=j, cp=cp: e.tensor_copy(WO[:, :, cp * 256:(cp + 1) * 256], sgO[j]), reads=[sgO_b[j]], writes=[wob])
            else:
                S.op("act", lambda e, j=j, cp=cp: e.activation(out=WO[:, :, cp * 256:(cp + 1) * 256], in_=sgO[j], func=AF.Copy), reads=[sgO_b[j]], writes=[wob])
        for i in range(NT):
            j = i % 2
            for n in range(4):
                bi = self.nextbank()
                ps = self.PS[:, bi, :]

                def mmw(e, i=i, n=n, ps=ps):
                    for kc in range(KC):
                        ins = e.matmul(ps, QA[:, kc, i * 128:(i + 1) * 128], WO[:, kc, n * 512:(n + 1) * 512],
                                       start=(kc == 0), stop=(kc == KC - 1))
                    return ins
                S.op("pe", mmw, reads=[wob, qab[i]], writes=[self.pbank[bi]])
                if n % 2 == 0:
                    S.op("act", lambda e, j=j, n=n, ps=ps: e.activation(out=OT[j][:, n * 512:(n + 1) * 512], in_=ps, func=AF.Copy),
                         reads=[self.pbank[bi]], writes=[otb[j]])
                else:
                    S.op("dve", lambda e, j=j, n=n, ps=ps: e.tensor_copy(OT[j][:, n * 512:(n + 1) * 512], ps),
                         reads=[self.pbank[bi]], writes=[otb[j]])
            S.op("sp", lambda e, i=i, j=j: e.dma_start(out=self.fd[i * 128:(i + 1) * 128, :], in_=OT[j]),
                 reads=[otb[j]], writes=[fb[i]], dma=True)


    def ph_gla(self, li):
        S = self.S
        htb = [self.buf("hT%d" % i) for i in range(NT)]
        w_in = self.g_win[li].rearrange("(kc p) f -> p kc f", p=128)
        wo = self.g_wout[li].rearrange("(kc p) f -> p kc f", p=128)
        if not hasattr(self, "Vd"):
            self.Vd = self.dscr("Vd", [TP, D], BF16)
            self.Rd = self.dscr("Rd", [TP, D], BF16)
        QK = self.sb(KC * TP // 2, BF16, [KC, TP])
        qkb = [self.buf("QK%d" % i) for i in range(NT)]
        base1 = self.sb_off
        GL = self.sb(TP)
        glb = self.buf("GL")
        base2 = self.sb_off
        XTall = self.sb(NT * 1024, BF16, [NT, KC, 128])
        bx = self.buf("glaX")
        g1stg = [self.sb(4096, F32, [KC, 256]) for _ in range(2)]
        g1stgb = [self.buf("glaStg%d" % j) for j in range(2)]
        g1w = [self.sb(2048, BF16, [KC, 256]) for _ in range(2)]
        g1wb = [self.buf("glaW%d" % j) for j in range(2)]
        VO = [self.sb(128, BF16) for _ in range(4)]
        vob = [self.buf("glaVO%d" % j) for j in range(4)]
        RT = [self.sb(256) for _ in range(2)]
        rtb = [self.buf("glaRT%d" % j) for j in range(2)]
        NG1 = self.sb(D)
        ngb1 = self.buf("glaNG1")
        S.op("sp", lambda e: e.dma_start(out=NG1, in_=self.g_ng[li:li + 1, :].partition_broadcast(128)), writes=[ngb1], dma=True)
        for i in range(NT):
            S.op("sp", lambda e, i=i: e.dma_start(out=XTall[:, i, :, :].rearrange("p a b -> p (a b)"), in_=self.hT[i]),
                 reads=[htb[i]], writes=[bx], dma=True)
        tg = [(0, 4), (4, 8), (8, 12), (12, 16), (16, 17)]
        vctr = [0]
        vdb, rdb = self.buf("Vd"), self.buf("Rd")
        def g1_load(ct):
            j = ct % 2
            ncol = 256 if ct < 24 else 16
            stg_v = g1stg[j][:, :, 0:ncol]
            w_v = g1w[j][:, :, 0:ncol]
            S.op("sp", lambda e: e.dma_start(out=stg_v, in_=w_in[:, :, ct * 256:ct * 256 + ncol]),
                 writes=[g1stgb[j]], dma=True)
            if ct % 2 == 0:
                S.op("act", lambda e: e.activation(out=w_v, in_=stg_v, func=AF.Copy), reads=[g1stgb[j]], writes=[g1wb[j]])
            else:
                S.op("dve", lambda e: e.tensor_copy(w_v, stg_v), reads=[g1stgb[j]], writes=[g1wb[j]])
        g1_load(0)
        for ct in range(25):
            j, j2 = ct % 2, ct % 2
            ncol = 256 if ct < 24 else 16
            w_v = g1w[j2][:, :, 0:ncol]
            if ct + 1 < 25:
                g1_load(ct + 1)
            if ct < 8 or ct == 24:
                for c2 in range(2 if ct < 24 else 1):
                    mcols = 128 if ct < 24 else 16
                    for (t0, t1) in tg:
                        n = (t1 - t0) * 128
                        bi = self.nextbank()
                        ps = self.PS[0:mcols, bi, 0:n]

                        def mmq(e, w_v=w_v, c2=c2, t0=t0, t1=t1, ps=ps, mcols=mcols):
                            for kc in range(KC):
                                ins = e.matmul(ps, w_v[:, kc, c2 * 128:c2 * 128 + mcols], XTall[:, t0:t1, kc, :],
                                               start=(kc == 0), stop=(kc == KC - 1))
                            return ins
                        S.op("pe", mmq, reads=[g1wb[j2], bx], writes=[self.pbank[bi]])
                        if ct == 24:
                            S.op("act", lambda e, t0=t0, t1=t1, ps=ps: e.activation(out=GL[0:16, t0 * 128:t1 * 128], in_=ps, func=AF.Copy),
                                 reads=[self.pbank[bi]], writes=[glb])
                        else:
                            c = ct * 2 + c2
                            if (t0 // 4) % 2 == 0:
                                S.op("act", lambda e, c=c, t0=t0, t1=t1, ps=ps: e.activation(out=QK[:, c, t0 * 128:t1 * 128], in_=ps, func=AF.Copy),
                                     reads=[self.pbank[bi]], writes=qkb[t0:t1])
                            else:
                                S.op("dve", lambda e, c=c, t0=t0, t1=t1, ps=ps: e.tensor_copy(QK[:, c, t0 * 128:t1 * 128], ps),
                                     reads=[self.pbank[bi]], writes=qkb[t0:t1])
            else:
                is_r = ct >= 16
                col0 = (ct - 8) * 256 if not is_r else (ct - 16) * 256
                dstd = self.Rd if is_r else self.Vd
                dbuf = rdb if is_r else vdb
                for i in range(NT):
                    bi = self.nextbank()
                    ps = self.PS[:, bi, 0:256]

                    def mmv(e, w_v=w_v, i=i, ps=ps):
                        for kc in range(KC):
                            ins = e.matmul(ps, XTall[:, i, kc, :], w_v[:, kc, :], start=(kc == 0), stop=(kc == KC - 1))
                        return ins
                    S.op("pe", mmv, reads=[g1wb[j2], bx], writes=[self.pbank[bi]])
                    k = vctr[0] % 4
                    vctr[0] += 1
                    vo = VO[k]
                    if is_r:
                        k5 = vctr[0] % 2
                        S.op("act", lambda e, k5=k5, ps=ps: e.activation(out=RT[k5], in_=ps, func=AF.Silu), reads=[self.pbank[bi]], writes=[rtb[k5]])
                        S.op("dve", lambda e, vo=vo, k5=k5, col0=col0: e.tensor_tensor(out=vo, in0=RT[k5], in1=NG1[:, col0:col0 + 256], op=ALU.mult),
                             reads=[rtb[k5], ngb1], writes=[vob[k]])
                    else:
                        S.op("dve", lambda e, vo=vo, ps=ps: e.tensor_copy(vo, ps), reads=[self.pbank[bi]], writes=[vob[k]])
                    S.op("sp", lambda e, vo=vo, i=i, col0=col0, dstd=dstd: e.dma_start(out=dstd[i * 128:(i + 1) * 128, col0:col0 + 256], in_=vo),
                         reads=[vob[k]], writes=[dbuf], dma=True)
        S.barrier()
        self.sb_reset(base2)
        KS = self.sb(8 * TP // 2, BF16, [8, TP])
        ksb = [self.buf("KS%d" % i) for i in range(NT)]
        DEC = self.sb(8 * 34, F32, [8, 34])
        decb = self.buf("DEC")
        WG2 = self.sb(1024)
        NB_ = self.sb(8)
        ONE1 = self.sb(1)
        M01 = self.sb(512)
        bcst = self.buf("glaC")
        S.op("sp", lambda e: e.dma_start(out=WG2[0:16, :], in_=self.g_wg2[li]), writes=[bcst], dma=True)
        S.op("sp", lambda e: e.dma_start(out=NB_, in_=self.g_bg[li].rearrange("(j p) -> p j", p=128), allow_slow_non_contiguous=True),
             writes=[bcst], dma=True)
        S.op("dve", lambda e: e.tensor_scalar(out=NB_, in0=NB_, scalar1=-1.0, scalar2=None, op0=ALU.mult), reads=[bcst], writes=[bcst])
        S.op("pool", lambda e: e.memset(ONE1, 1.0), writes=[bcst])
        S.op("pool", lambda e: e.memset(M01, 1.0), writes=[bcst])
        S.op("pool", lambda e: e.memset(M01.rearrange("p (c t) -> p c t", t=64)[:, :, 0:1], 0.0), reads=[bcst], writes=[bcst])
        NTMP = 2
        T1 = [self.sb(512) for _ in range(NTMP)]
        CS = [self.sb(512) for _ in range(NTMP)]
        EB = [self.sb(512) for _ in range(NTMP)]
        EBi = [self.sb(512) for _ in range(NTMP)]
        DF = [self.sb(512) for _ in range(NTMP)]
        tb = [[self.buf("gla%s%d" % (nm, j)) for j in range(NTMP)] for nm in ("T1", "CS", "EB", "EBi", "DF")]
        it = 0
        for (t0, t1) in tg:
            meta_g = (t0 == 16)
            n = 16 if meta_g else (t1 - t0) * 128
            csz = 16 if meta_g else 64
            nch = n // csz
            c0 = 32 if meta_g else t0 * 2
            tsl = slice(t0 * 128, t0 * 128 + n)
            for j in range(8):
                k = it % NTMP
                it += 1
                bi = self.nextbank()
                ps = self.PS[:, bi, 0:n]
                S.op("pe", lambda e, ps=ps, j=j, tsl=tsl: e.matmul(ps, WG2[0:16, j * 128:(j + 1) * 128], GL[0:16, tsl], start=True, stop=True),
                     reads=[bcst, glb], writes=[self.pbank[bi]])
                t1_, cs_, eb_, ebi_, df_ = T1[k][:, 0:n], CS[k][:, 0:n], EB[k][:, 0:n], EBi[k][:, 0:n], DF[k][:, 0:n]
                S.op("act", lambda e, t1_=t1_, ps=ps, j=j: e.activation(out=t1_, in_=ps, func=AF.Exp, bias=NB_[:, j:j + 1], scale=-1.0),
                     reads=[self.pbank[bi], bcst], writes=[tb[0][k]])
                S.op("act", lambda e, t1_=t1_: e.activation(out=t1_, in_=t1_, func=AF.Ln, bias=ONE1, scale=1.0),
                     reads=[tb[0][k], bcst], writes=[tb[0][k]])
                S.op("dve", lambda e, cs_=cs_, t1_=t1_, n=n: e.tensor_tensor_scan(out=cs_, data0=M01[:, 0:n], data1=t1_, initial=0.0,
                                                                                 op0=ALU.mult, op1=ALU.add),
                     reads=[tb[0][k], bcst], writes=[tb[1][k]])
                S.op("act", lambda e, eb_=eb_, cs_=cs_: e.activation(out=eb_, in_=cs_, func=AF.Exp, scale=-1.0 / 16), reads=[tb[1][k]], writes=[tb[2][k]])
                S.op("act", lambda e, ebi_=ebi_, cs_=cs_: e.activation(out=ebi_, in_=cs_, func=AF.Exp, scale=1.0 / 16), reads=[tb[1][k]], writes=[tb[3][k]])
                for c in range(nch):
                    S.op("dve", lambda e, df_=df_, cs_=cs_, c=c, csz=csz: e.tensor_scalar(
                        out=df_[:, c * csz:(c + 1) * csz], in0=cs_[:, c * csz:(c + 1) * csz], scalar1=cs_[:, (c + 1) * csz - 1:(c + 1) * csz],
                        scalar2=None, op0=ALU.subtract), reads=[tb[1][k]], writes=[tb[4][k]])
                S.op("act", lambda e, df_=df_: e.activation(out=df_, in_=df_, func=AF.Exp, scale=1.0 / 16), reads=[tb[4][k]], writes=[tb[4][k]])
                S.op("act", lambda e, cs_=cs_, j=j, c0=c0, nch=nch, csz=csz: e.activation(
                    out=DEC[:, j, c0:c0 + nch], in_=cs_.rearrange("p (c t) -> p c t", t=csz)[:, :, csz - 1], func=AF.Exp, scale=-1.0 / 16),
                    reads=[tb[1][k]], writes=[decb])
                tiles_b = qkb[t0:t1]
                S.op("dve", lambda e, j=j, tsl=tsl, eb_=eb_: e.scalar_tensor_tensor(out=QK[:, j, tsl], in0=QK[:, j, tsl], scalar=1.0 / 16, in1=eb_,
                                                                                    op0=ALU.mult, op1=ALU.mult),
                     reads=[tb[2][k]] + tiles_b, writes=tiles_b)
                S.op("pool", lambda e, j=j, tsl=tsl, df_=df_: e.tensor_tensor(out=KS[:, j, tsl], in0=QK[:, 8 + j, tsl], in1=df_, op=ALU.mult),
                     reads=[tb[4][k]] + tiles_b, writes=ksb[t0:t1])
                S.op("pool", lambda e, j=j, tsl=tsl, ebi_=ebi_: e.tensor_tensor(out=QK[:, 8 + j, tsl], in0=QK[:, 8 + j, tsl], in1=ebi_, op=ALU.mult),
                     reads=[tb[3][k]] + tiles_b + ksb[t0:t1], writes=tiles_b)
        S.barrier()
        base3 = self.sb_off
        S32 = self.sb(8 * 512, F32, [8, 512])
        Sbf = self.sb(8 * 256, BF16, [8, 512])
        s32b = [self.buf("S32_%d" % j) for j in range(8)]
        sbfb = [self.buf("Sbf_%d" % j) for j in range(8)]
        MK = self.sb(256, F32, [4, 64])
        NG = self.sb(D)
        bmk = self.buf("glaMK")
        S.op("pool", lambda e: e.memset(S32, 0.0), writes=s32b)
        S.op("pool", lambda e: e.memset(Sbf, 0.0), writes=sbfb)
        S.op("pool", lambda e: e.memset(MK, 1.0), writes=[bmk])
        S.op("pool", lambda e: e.affine_select(out=MK, in_=MK, pattern=[[0, 4], [1, 64]], base=0, channel_multiplier=-1,
                                               compare_op=ALU.is_ge, fill=0.0), reads=[bmk], writes=[bmk])
        S.op("sp", lambda e: e.dma_start(out=NG, in_=self.g_ng[li:li + 1, :].partition_broadcast(128)), writes=[bmk], dma=True)
        Vc = [self.sb(1024, BF16) for _ in range(2)]
        Rc = [self.sb(1024, BF16) for _ in range(2)]
        vcb = [self.buf("glaVc%d" % j) for j in range(2)]
        rcb = [self.buf("glaRc%d" % j) for j in range(2)]
        KSc = [self.sb(512, BF16, [8, 128]) for _ in range(2)]
        kscb = [self.buf("glaKSc%d" % j) for j in range(2)]
        ATT = [self.sb(128, BF16, [4, 64]) for _ in range(2)]
        attb = [self.buf("glaATT%d" % j) for j in range(2)]
        YF = [self.sb(512) for _ in range(2)]
        yfb = [self.buf("glaYF%d" % j) for j in range(2)]
        YB = [self.sb(1024, BF16) for _ in range(2)]
        ybb = [self.buf("glaYB%d" % j) for j in range(2)]
        ST = [self.sb(6) for _ in range(4)]
        MV = [self.sb(2) for _ in range(4)]
        RS = [self.sb(1) for _ in range(4)]
        stb = [self.buf("glaST%d" % j) for j in range(4)]
        order = [(16, 0, 16)] + [(t, hf, 64) for t in range(16) for hf in range(2)]
        hctr = 0
        for n_, (tile, hf, C) in enumerate(order):
            r0 = tile * 128 + hf * 64
            ts = slice(r0, r0 + C)
            cidx = 32 if tile == 16 else tile * 2 + hf
            b2 = n_ % 2
            vc, rc, ksc, att, yb = Vc[b2], Rc[b2], KSc[b2], ATT[b2], YB[b2]
            S.op("sp", lambda e, vc=vc, ts=ts, C=C: e.dma_start(out=vc[0:C, :], in_=self.Vd[ts, :]), reads=[vdb], writes=[vcb[b2]], dma=True)
            S.op("sp", lambda e, rc=rc, ts=ts, C=C: e.dma_start(out=rc[0:C, :], in_=self.Rd[ts, :]), reads=[rdb], writes=[rcb[b2]], dma=True)
            bi = self.nextbank()
            pk = self.PS[:, bi, :].bitcast(BF16).rearrange("p (a b) -> p a b", a=8)

            def trk(e, pk=pk, ts=ts, C=C):
                for j in range(8):
                    ins = e.transpose(out=pk[0:C, j, :], in_=KS[:, j, ts], identity=self.identb)
                return ins
            S.op("pe", trk, reads=[ksb[tile], self.buf("identb")], writes=[self.pbank[bi]])
            S.op("act", lambda e, ksc=ksc, pk=pk, C=C: e.activation(out=ksc[0:C], in_=pk[0:C], func=AF.Copy), reads=[self.pbank[bi]], writes=[kscb[b2]])
            bi = self.nextbank()
            pa = self.PS[:, bi, 0:256].rearrange("p (h c) -> p h c", h=4)

            def mma(e, pa=pa, ts=ts, C=C):
                for h in range(4):
                    for dc in range(2):
                        ins = e.matmul(pa[0:C, h, 0:C], QK[:, 8 + h * 2 + dc, ts], QK[:, h * 2 + dc, ts], start=(dc == 0), stop=(dc == 1))
                return ins
            S.op("pe", mma, reads=[qkb[tile]], writes=[self.pbank[bi]])
            S.op("dve", lambda e, att=att, pa=pa, C=C: e.tensor_tensor(out=att[0:C, :, 0:C], in0=pa[0:C, :, 0:C], in1=MK[0:C, :, 0:C], op=ALU.mult),
                 reads=[self.pbank[bi], bmk], writes=[attb[b2]])
            for h in range(4):
                bo = self.nextbank()
                po = self.PS[:, bo, :]

                def mmo(e, po=po, h=h, ts=ts, C=C, att=att, vc=vc):
                    e.matmul(po[0:C, :], att[0:C, h, 0:C], vc[0:C, h * 512:(h + 1) * 512], start=True, stop=False)
                    for dc in range(2):
                        ins = e.matmul(po[0:C, :], QK[:, h * 2 + dc, ts], Sbf[:, h * 2 + dc, :], start=False, stop=(dc == 1))
                    return ins
                S.op("pe", mmo, reads=[attb[b2], vcb[b2], qkb[tile], sbfb[h * 2], sbfb[h * 2 + 1]], writes=[self.pbank[bo]])
                for dc in range(2):
                    j = h * 2 + dc
                    bs_ = self.nextbank()
                    pss = self.PS[:, bs_, :]
                    S.op("pe", lambda e, pss=pss, ksc=ksc, j=j, h=h, C=C, vc=vc: e.matmul(pss, ksc[0:C, j, :], vc[0:C, h * 512:(h + 1) * 512], start=True, stop=True),
                         reads=[kscb[b2], vcb[b2]], writes=[self.pbank[bs_]])
                    S.op("dve", lambda e, pss=pss, j=j, cidx=cidx: e.scalar_tensor_tensor(out=S32[:, j, :], in0=S32[:, j, :], scalar=DEC[:, j, cidx:cidx + 1],
                                                                                          in1=pss, op0=ALU.mult, op1=ALU.add),
                         reads=[self.pbank[bs_], decb, s32b[j]], writes=[s32b[j]])
                    if dc == 0:
                        S.op("act", lambda e, j=j: e.activation(out=Sbf[:, j, :], in_=S32[:, j, :], func=AF.Copy), reads=[s32b[j]], writes=[sbfb[j]])
                    else:
                        S.op("act", lambda e, j=j: e.activation(out=Sbf[:, j, :], in_=S32[:, j, :], func=AF.Copy), reads=[s32b[j]], writes=[sbfb[j]])
                k4 = hctr % 4
                k2 = hctr % 2
                hctr += 1
                st, mv, rs, yf = ST[k4], MV[k4], RS[k4], YF[k2]
                S.op("dve", lambda e, st=st, po=po, C=C: e.bn_stats(out=st[0:C, :], in_=po[0:C, :]), reads=[self.pbank[bo]], writes=[stb[k4]])
                S.op("dve", lambda e, st=st, mv=mv, C=C: e.bn_aggr(out=mv[0:C, :], in_=st[0:C, :]), reads=[stb[k4]], writes=[stb[k4]])
                S.op("act", lambda e, mv=mv, rs=rs, C=C: e.activation(out=rs[0:C, :], in_=mv[0:C, 1:2], func=AF.Sqrt, bias=self.eps_ap[0:C, :], scale=1.0),
                     reads=[stb[k4], self.buf("eps")], writes=[stb[k4]])
                S.op("dve", lambda e, rs=rs, C=C: e.reciprocal(out=rs[0:C, :], in_=rs[0:C, :]), reads=[stb[k4]], writes=[stb[k4]])
                S.op("dve", lambda e, mv=mv, rs=rs, st=st, C=C: e.scalar_tensor_tensor(out=st[0:C, 0:1], in0=mv[0:C, 0:1], scalar=-1.0, in1=rs[0:C, :],
                                                                                       op0=ALU.mult, op1=ALU.mult),
                     reads=[stb[k4]], writes=[stb[k4]])
                S.op("act", lambda e, yf=yf, po=po, st=st, rs=rs, C=C: e.activation(out=yf[0:C, :], in_=po[0:C, :], func=AF.Identity,
                                                                                    bias=st[0:C, 0:1], scale=rs[0:C, :]),
                     reads=[self.pbank[bo], stb[k4]], writes=[yfb[k2]])
                S.op("dve", lambda e, yf=yf, yb=yb, rc=rc, h=h, C=C: e.tensor_tensor(out=yb[0:C, h * 512:(h + 1) * 512], in0=yf[0:C, :],
                                                                                    in1=rc[0:C, h * 512:(h + 1) * 512], op=ALU.mult),
                     reads=[yfb[k2], rcb[b2]], writes=[ybb[b2]])
            bi = self.nextbank()
            py = self.PS[:, bi, :].bitcast(BF16).rearrange("p (a b) -> p a b", a=16)

            def try_(e, py=py, yb=yb, C=C):
                for kc in range(KC):
                    ins = e.transpose(out=py[:, kc, 0:C], in_=yb[0:C, kc * 128:(kc + 1) * 128], identity=self.identb[0:C, 0:C])
                return ins
            S.op("pe", try_, reads=[ybb[b2], self.buf("identb")], writes=[self.pbank[bi]])
            S.op("act", lambda e, py=py, ts=ts, C=C: e.activation(out=QK[:, :, ts], in_=py[:, :, 0:C], func=AF.Copy),
                 reads=[self.pbank[bi]], writes=[qkb[tile]])
        S.op("pool", lambda e: e.memset(QK[:, :, 2048 + NMETA:TP], 0.0), writes=[qkb[16]])
        self.outproj(QK, qkb, wo, base1)


def t5_bucket_np(dist):
    d = np.maximum(dist, 0)
    df = np.maximum(d, 1).astype(np.float32)
    large = 16 + (np.log(df / np.float32(16)) / np.float32(np.log(128 / 16)) * np.float32(16)).astype(np.int32)
    large = np.minimum(large, 31)
    return np.where(d < 16, d, large)


def bias_onehots():
    out = {}
    j = np.arange(128)

    def mk(dist, valid):
        nq, ns = dist.shape
        b = t5_bucket_np(dist)
        oh = np.zeros((33, nq, ns), np.float32)
        qq, ss = np.meshgrid(np.arange(nq), np.arange(ns), indexing="ij")
        oh[np.where(valid, b, 32), qq, ss] = 1.0
        return oh.reshape(33, nq * ns)
    d = j[:, None] - j[None, :]
    out["oh_cur"] = mk(d, d >= 0)
    d = 128 + j[:, None] - j[None, :]
    out["oh_prev"] = mk(d, d < 128)
    m = np.arange(16)
    d = NMETA + j[:, None] - m[None, :]
    out["oh_meta0"] = mk(d, np.ones_like(d, bool))
    d = NMETA + 128 + j[:, None] - m[None, :]
    out["oh_metac"] = mk(d, np.ones_like(d, bool))
    d = m[:, None] - m[None, :]
    out["oh_mm"] = mk(d, d >= 0)
    return out


def build_program(cfg):
    b = Builder(cfg)
    nc = b.build()
    return nc, b


FULL_PHASES = [
    ("init",), ("ln", 0, 0, "plain"),
    ("gla", 0), ("ln", 0, 0, "ln"), ("ffn", 0), ("ln", 0, 1, "ln"),
    ("gla", 1), ("ln", 1, 0, "ln"), ("moe", 0), ("ln", 1, 1, "ln"),
    ("kv",), ("bias",),
    ("swa", 0), ("ln", 2, 0, "ln"), ("ffn", 1), ("ln", 2, 1, "ln"),
    ("swa", 1), ("ln", 3, 0, "ln"), ("moe", 1), ("ln", 3, 1, "final"),
]

_WEIGHT_KEYS = ["meta_tokens", "rel_bias_table", "ln_gain", "ln_bias", "gla_w_in", "gla_w_gate2", "gla_b_gate",
                "gla_norm_gain", "gla_w_out", "kv_w_shared", "swa_w_q", "swa_sinks", "swa_w_out",
                "ffn_w_gate_up", "ffn_w_down", "moe_w_router", "moe_w_gate_up", "moe_w_down"]


def kernel(**inputs):
    x = np.asarray(inputs["x"], dtype=np.float32)
    nb = x.shape[0]
    nc, _ = build_program({"phases": FULL_PHASES})
    shared = {k: np.ascontiguousarray(np.asarray(inputs[k], dtype=np.float32)) for k in _WEIGHT_KEYS}
    shared.update(bias_onehots())
    in_maps = []
    for b in range(nb):
        m = dict(shared)
        m["x"] = np.ascontiguousarray(x[b])
        in_maps.append(m)
    res = run_bass_kernel_spmd(nc, in_maps, core_ids=list(range(nb)))
    return np.stack([np.asarray(r["out"], dtype=np.float32) for r in res.results], axis=0)
```
